# Optimizing a Trainium2 kernel written in Bass

```python
import math
import jax, jax.numpy as jnp
from jax import lax
import numpy as np

D_MODEL = 1024
BATCH = 4
SEQ = 8192
DEPTH = 1

N_META = 16
BLOCK_Q = 128
EPS = 1e-6

MLA_HEADS = 8
MLA_Q_RANK = 256
MLA_KV_RANK = 256
MLA_NOPE_DIM = 64
MLA_ROPE_DIM = 32
MLA_V_DIM = 64
MLA_QK_DIM = MLA_NOPE_DIM + MLA_ROPE_DIM
ROPE_THETA = 10000.0

DIFF_HEADS = 4
DIFF_HEAD_DIM = 64
DIFF_V_DIM = 2 * DIFF_HEAD_DIM
DIFF_QK_WIDTH = DIFF_HEADS * 2 * DIFF_HEAD_DIM
DIFF_V_WIDTH = DIFF_HEADS * DIFF_V_DIM

NUM_BUCKETS = 32
MAX_DISTANCE = 128

_S1 = MLA_Q_RANK
_S2 = _S1 + MLA_KV_RANK
_S3 = _S2 + MLA_ROPE_DIM
_S4 = _S3 + DIFF_QK_WIDTH
_S5 = _S4 + DIFF_QK_WIDTH
IN_WIDTH = _S5 + DIFF_V_WIDTH
IN_SPLITS = (_S1, _S2, _S3, _S4, _S5)
MIX_WIDTH = MLA_HEADS * MLA_V_DIM + DIFF_V_WIDTH

PEER_HEADS = 8
PEER_N_KEYS = 128
PEER_N_EXPERTS = PEER_N_KEYS * PEER_N_KEYS
PEER_TOPK = 16
PEER_QUERY_DIM = 256
PEER_SUBKEY_DIM = PEER_QUERY_DIM // 2
PEER_CHUNK = 128

kernel_name = "hymba_mla_diffattn_peer_layer"


def _rms_norm(x, gain):
    xf = x.astype(jnp.float32)
    y = xf * lax.rsqrt(jnp.mean(xf * xf, axis=-1, keepdims=True) + EPS)
    return (y * gain.astype(jnp.float32)).astype(x.dtype)


def _rope(t, pos):
    half = t.shape[-1] // 2
    inv_freq = ROPE_THETA ** (-jnp.arange(half, dtype=jnp.float32) / half)
    ang = pos.astype(jnp.float32)[:, None] * inv_freq[None, :]
    cos = jnp.cos(ang)[:, None, :].astype(t.dtype)
    sin = jnp.sin(ang)[:, None, :].astype(t.dtype)
    t1, t2 = t[..., :half], t[..., half:]
    return jnp.concatenate([t1 * cos - t2 * sin, t1 * sin + t2 * cos], axis=-1)


def _t5_bucket(dist):
    n = jnp.maximum(dist, 0)
    max_exact = NUM_BUCKETS // 2
    nf = jnp.maximum(n, 1).astype(jnp.float32)
    large = max_exact + (jnp.log(nf / max_exact) / math.log(MAX_DISTANCE / max_exact)
                         * (NUM_BUCKETS - max_exact)).astype(jnp.int32)
    large = jnp.minimum(large, NUM_BUCKETS - 1)
    return jnp.where(n < max_exact, n, large)


def _causal_softmax(logits, q_pos, k_pos):
    mask = k_pos[None, :] <= q_pos[:, None]
    logits = jnp.where(mask, logits.astype(jnp.float32), -1e30)
    return jax.nn.softmax(logits, axis=-1)


def _to_blocks(t):
    b, l = t.shape[0], t.shape[1]
    t = t.reshape((b, l // BLOCK_Q, BLOCK_Q) + t.shape[2:])
    return jnp.moveaxis(t, 1, 0)


def _from_blocks(t):
    t = jnp.moveaxis(t, 0, 1)
    return t.reshape((t.shape[0], t.shape[1] * t.shape[2]) + t.shape[3:])


def _mla(c_q, c_kv, k_rope, q_norm, w_uq, kv_norm, w_ukv, qk_norm_q, qk_norm_k, pos):
    b, l, _ = c_q.shape
    q = (_rms_norm(c_q, q_norm) @ w_uq).reshape(b, l, MLA_HEADS, MLA_QK_DIM)
    kv = (_rms_norm(c_kv, kv_norm) @ w_ukv).reshape(b, l, MLA_HEADS, MLA_NOPE_DIM + MLA_V_DIM)
    k_nope, v = kv[..., :MLA_NOPE_DIM], kv[..., MLA_NOPE_DIM:]
    k_r = jnp.broadcast_to(k_rope[:, :, None, :], (b, l, MLA_HEADS, MLA_ROPE_DIM))
    k = jnp.concatenate([k_nope, k_r], axis=-1)
    q = _rms_norm(q, qk_norm_q)
    k = _rms_norm(k, qk_norm_k)
    q = jnp.concatenate([q[..., :MLA_NOPE_DIM], _rope(q[..., MLA_NOPE_DIM:], pos)], axis=-1)
    k = jnp.concatenate([k[..., :MLA_NOPE_DIM], _rope(k[..., MLA_NOPE_DIM:], pos)], axis=-1)
    scale = MLA_QK_DIM ** -0.5

    def block(args):
        q_start, qb = args
        q_pos = q_start + jnp.arange(BLOCK_Q, dtype=jnp.int32)
        s = jnp.einsum('bqhd,bkhd->bhqk', qb, k) * scale
        p = _causal_softmax(s, q_pos, pos)
        return jnp.einsum('bhqk,bkhd->bqhd', p.astype(v.dtype), v)

    starts = jnp.arange(l // BLOCK_Q, dtype=jnp.int32) * BLOCK_Q
    out = _from_blocks(lax.map(block, (starts, _to_blocks(q))))
    return out.reshape(b, l, MLA_HEADS * MLA_V_DIM)


def _diff_attn(q, k, v, q_norm, k_norm, lam_q1, lam_k1, lam_q2, lam_k2, subln, rel_bias, pos, lambda_init):
    b, l, _ = q.shape
    q = _rms_norm(q.reshape(b, l, DIFF_HEADS, 2, DIFF_HEAD_DIM), q_norm)
    k = _rms_norm(k.reshape(b, l, DIFF_HEADS, 2, DIFF_HEAD_DIM), k_norm)
    v = v.reshape(b, l, DIFF_HEADS, DIFF_V_DIM)
    lam = (jnp.exp(jnp.sum(lam_q1.astype(jnp.float32) * lam_k1.astype(jnp.float32)))
           - jnp.exp(jnp.sum(lam_q2.astype(jnp.float32) * lam_k2.astype(jnp.float32)))
           + lambda_init)
    k1, k2 = k[..., 0, :], k[..., 1, :]
    bias_table = rel_bias.astype(jnp.float32)
    scale = DIFF_HEAD_DIM ** -0.5

    def block(args):
        q_start, qb = args
        q_pos = q_start + jnp.arange(BLOCK_Q, dtype=jnp.int32)
        bias = bias_table[_t5_bucket(q_pos[:, None] - pos[None, :])]
        bias = jnp.transpose(bias, (2, 0, 1))[None]
        s1 = jnp.einsum('bqhd,bkhd->bhqk', qb[..., 0, :], k1).astype(jnp.float32) * scale + bias
        s2 = jnp.einsum('bqhd,bkhd->bhqk', qb[..., 1, :], k2).astype(jnp.float32) * scale + bias
        a = _causal_softmax(s1, q_pos, pos) - lam * _causal_softmax(s2, q_pos, pos)
        return jnp.einsum('bhqk,bkhd->bqhd', a.astype(v.dtype), v)

    starts = jnp.arange(l // BLOCK_Q, dtype=jnp.int32) * BLOCK_Q
    out = _from_blocks(lax.map(block, (starts, _to_blocks(q))))
    out = _rms_norm(out, subln) * (1.0 - lambda_init)
    return out.reshape(b, l, DIFF_V_WIDTH)


def _peer(h, w_query, sub_keys, u, v):
    b, l, d = h.shape
    tokens = h.reshape(b * l // PEER_CHUNK, PEER_CHUNK, d)

    def chunk(xc):
        q = (xc @ w_query).reshape(PEER_CHUNK, PEER_HEADS, 2, PEER_SUBKEY_DIM)
        s = jnp.einsum('chpd,hpnd->chpn', q, sub_keys).astype(jnp.float32)
        s1, i1 = lax.top_k(s[:, :, 0], PEER_TOPK)
        s2, i2 = lax.top_k(s[:, :, 1], PEER_TOPK)
        cand = (s1[..., :, None] + s2[..., None, :]).reshape(PEER_CHUNK, PEER_HEADS, PEER_TOPK * PEER_TOPK)
        best, flat = lax.top_k(cand, PEER_TOPK)
        e1 = jnp.take_along_axis(i1, flat // PEER_TOPK, axis=-1)
        e2 = jnp.take_along_axis(i2, flat % PEER_TOPK, axis=-1)
        experts = e1 * PEER_N_KEYS + e2
        g = jax.nn.softmax(best, axis=-1)
        u_sel = jnp.take(u, experts, axis=0)
        act = jax.nn.gelu(jnp.einsum('cd,chkd->chk', xc, u_sel).astype(jnp.float32), approximate=False)
        v_sel = jnp.take(v, experts, axis=0)
        return jnp.einsum('chk,chkd->cd', (g * act).astype(v.dtype), v_sel)

    return lax.map(chunk, tokens).reshape(b, l, d)


def setup_inputs(seed: int = 0) -> dict:
    key = jax.random.key(seed)
    ks = jax.random.split(key, 24)
    f32 = jnp.float32

    def nrm(k, shape, scale):
        return jax.random.normal(k, shape, f32) * scale

    def gain(k, shape):
        return 1.0 + 0.02 * jax.random.normal(k, shape, f32)

    return {
        "x": nrm(ks[0], (BATCH, SEQ, D_MODEL), 1.0),
        "meta_tokens": nrm(ks[1], (N_META, D_MODEL), 1.0),
        "rel_bias": nrm(ks[2], (NUM_BUCKETS, DIFF_HEADS), 0.5),
        "attn_norm": gain(ks[3], (DEPTH, D_MODEL)),
        "w_in": nrm(ks[4], (DEPTH, D_MODEL, IN_WIDTH), D_MODEL ** -0.5),
        "mla_q_norm": gain(ks[5], (DEPTH, MLA_Q_RANK)),
        "mla_w_uq": nrm(ks[6], (DEPTH, MLA_Q_RANK, MLA_HEADS * MLA_QK_DIM), MLA_Q_RANK ** -0.5),
        "mla_kv_norm": gain(ks[7], (DEPTH, MLA_KV_RANK)),
        "mla_w_ukv": nrm(ks[8], (DEPTH, MLA_KV_RANK, MLA_HEADS * (MLA_NOPE_DIM + MLA_V_DIM)), MLA_KV_RANK ** -0.5),
        "mla_qk_norm_q": gain(ks[9], (DEPTH, MLA_QK_DIM)),
        "mla_qk_norm_k": gain(ks[10], (DEPTH, MLA_QK_DIM)),
        "diff_q_norm": gain(ks[11], (DEPTH, DIFF_HEAD_DIM)),
        "diff_k_norm": gain(ks[12], (DEPTH, DIFF_HEAD_DIM)),
        "diff_lambda_q1": nrm(ks[13], (DEPTH, DIFF_HEAD_DIM), 0.1),
        "diff_lambda_k1": nrm(ks[14], (DEPTH, DIFF_HEAD_DIM), 0.1),
        "diff_lambda_q2": nrm(ks[15], (DEPTH, DIFF_HEAD_DIM), 0.1),
        "diff_lambda_k2": nrm(ks[16], (DEPTH, DIFF_HEAD_DIM), 0.1),
        "diff_subln": gain(ks[17], (DEPTH, DIFF_V_DIM)),
        "w_out": nrm(ks[18], (DEPTH, MIX_WIDTH, D_MODEL), MIX_WIDTH ** -0.5),
        "ffn_norm": gain(ks[19], (DEPTH, D_MODEL)),
        "peer_w_query": nrm(ks[20], (DEPTH, D_MODEL, PEER_HEADS * PEER_QUERY_DIM), D_MODEL ** -0.5),
        "peer_sub_keys": nrm(ks[21], (DEPTH, PEER_HEADS, 2, PEER_N_KEYS, PEER_SUBKEY_DIM), PEER_SUBKEY_DIM ** -0.5),
        "peer_u": nrm(ks[22], (DEPTH, PEER_N_EXPERTS, D_MODEL), D_MODEL ** -0.5),
        "peer_v": nrm(ks[23], (DEPTH, PEER_N_EXPERTS, D_MODEL), PEER_HEADS ** -0.5),
    }


def reference(x, meta_tokens, rel_bias, attn_norm, w_in, mla_q_norm, mla_w_uq, mla_kv_norm, mla_w_ukv,
              mla_qk_norm_q, mla_qk_norm_k, diff_q_norm, diff_k_norm, diff_lambda_q1, diff_lambda_k1,
              diff_lambda_q2, diff_lambda_k2, diff_subln, w_out, ffn_norm, peer_w_query, peer_sub_keys,
              peer_u, peer_v):
    b, s, d = x.shape
    l_real = N_META + s
    l_pad = -(-l_real // BLOCK_Q) * BLOCK_Q
    meta = jnp.broadcast_to(meta_tokens[None].astype(x.dtype), (b, N_META, d))
    h = jnp.concatenate([meta, x, jnp.zeros((b, l_pad - l_real, d), x.dtype)], axis=1)
    pos = jnp.arange(l_pad, dtype=jnp.int32)
    for layer in range(DEPTH):
        lambda_init = 0.8 - 0.6 * math.exp(-0.3 * layer)
        n = _rms_norm(h, attn_norm[layer])
        proj = n @ w_in[layer]
        c_q, c_kv, k_rope, dq, dk, dv = jnp.split(proj, IN_SPLITS, axis=-1)
        y_mla = _mla(c_q, c_kv, k_rope, mla_q_norm[layer], mla_w_uq[layer], mla_kv_norm[layer],
                     mla_w_ukv[layer], mla_qk_norm_q[layer], mla_qk_norm_k[layer], pos)
        y_diff = _diff_attn(dq, dk, dv, diff_q_norm[layer], diff_k_norm[layer], diff_lambda_q1[layer],
                            diff_lambda_k1[layer], diff_lambda_q2[layer], diff_lambda_k2[layer],
                            diff_subln[layer], rel_bias, pos, lambda_init)
        h = h + jnp.concatenate([y_mla, y_diff], axis=-1) @ w_out[layer]
        h = h + _peer(_rms_norm(h, ffn_norm[layer]), peer_w_query[layer], peer_sub_keys[layer],
                      peer_u[layer], peer_v[layer])
    return h[:, N_META:l_real]
```

```python
import math
from contextlib import ExitStack

import numpy as np
import concourse.bass as bass
import concourse.mybir as mybir
from concourse.bass_utils import run_bass_kernel_spmd

F32 = mybir.dt.float32
BF16 = mybir.dt.bfloat16
U32 = mybir.dt.uint32
AF = mybir.ActivationFunctionType
ALU = mybir.AluOpType
AX = mybir.AxisListType

D = 1024
NMETA = 16
EPS = 1e-6
LAMBDA_INIT = 0.2
NEG = -30000.0
IN_W = 2080

VEC_LAYOUT = [("attn_norm", 1024), ("ffn_norm", 1024), ("q_norm", 256), ("kv_norm", 256),
              ("gq", 96), ("gk", 96), ("gdq", 64), ("gdk", 64), ("lq1", 64), ("lk1", 64),
              ("lq2", 64), ("lk2", 64), ("subln", 128), ("b31", 4)]
VOFF = {}
_o = 0
for _n, _l in VEC_LAYOUT:
    VOFF[_n] = (_o, _o + _l)
    _o += _l
NVEC = _o


class Buf:
    __slots__ = ("w", "r")

    def __init__(self):
        self.w = None
        self.r = {}


class Tile:
    __slots__ = ("t", "b")

    def __init__(self, t):
        self.t = t
        self.b = Buf()


class Sched:
    ENG = ("pe", "act", "dve", "pool", "sp")

    def __init__(self, nc, es):
        self.nc = nc
        self.es = es
        self.sems = []
        self.owner = []
        self.prog = {e: [] for e in self.ENG}
        self.cnt = {e: 0 for e in self.ENG}
        self.esem = {}
        self.seen = {e: {} for e in self.ENG}
        for e in self.ENG:
            self._new_epoch(e)
        self.defer = False
        self.pending = []
        self.dq = {}
        for e, k in (("sp", 16), ("pool", 16), ("act", 4)):
            self.dq[e] = {"sems": [self._sem(None) for _ in range(k)], "cnt": [0] * k, "n": 0}

    def _sem(self, owner):
        s = self.es.enter_context(self.nc.semaphore(f"sm{len(self.sems)}"))
        self.sems.append(s)
        self.owner.append(owner)
        return len(self.sems) - 1

    def _new_epoch(self, e):
        self.esem[e] = self._sem(e)
        self.cnt[e] = 0

    def _waits(self, e, reads, writes):
        need = {}

        def add(s, v, war=False):
            if self.owner[s] == e and e == "pe":
                return
            if need.get(s, 0) < v:
                need[s] = v

        for b in reads:
            if b.w is not None:
                add(*b.w)
        for b in writes:
            if b.w is not None:
                add(*b.w)
            for s, v in b.r.items():
                add(s, v, True)
        out = []
        seen = self.seen[e]
        for s, v in need.items():
            if seen.get(s, 0) >= v:
                continue
            seen[s] = v
            out.append((s, v))
        return out

    def _book(self, ev, reads, writes):
        s, v = ev
        for b in reads:
            if b.r.get(s, 0) < v:
                b.r[s] = v
        for b in writes:
            b.w = ev
            b.r = {}

    def op(self, e, fn, reads=(), writes=(), dur=0.3):
        reads = [x.b if isinstance(x, Tile) else x for x in reads]
        writes = [x.b if isinstance(x, Tile) else x for x in writes]
        if self.defer:
            self.pending.append(("op", e, fn, reads, writes, dur, dur))
            return
        w = self._waits(e, reads, writes)
        if self.cnt[e] >= 60000:
            self._new_epoch(e)
        self.cnt[e] += 1
        ev = (self.esem[e], self.cnt[e])
        self.prog[e].append((w, fn, ev[0], 1))
        self._book(ev, reads, writes)

    def dma(self, e, fn, reads=(), writes=(), dur=3.0):
        reads = [x.b if isinstance(x, Tile) else x for x in reads]
        writes = [x.b if isinstance(x, Tile) else x for x in writes]
        if self.defer:
            self.pending.append(("dma", e, fn, reads, writes, 1.0 if e == "pool" else 0.35, dur))
            return
        q = self.dq[e]
        k = q["n"] % len(q["sems"])
        q["n"] += 1
        s = q["sems"][k]
        w = self._waits(e, reads, writes)
        c = q["cnt"][k]
        if c > 0 and self.seen[e].get(s, 0) < c:
            self.seen[e][s] = c
            w.append((s, c))
        q["cnt"][k] = c + 16
        ev = (s, c + 16)
        self.prog[e].append((w, fn, s, 16))
        self._book(ev, reads, writes)

    def reorder(self):
        import heapq
        ops = self.pending
        self.pending = []
        self.defer = False
        n = len(ops)
        lastw, readers = {}, {}
        deps = [None] * n
        succ = [[] for _ in range(n)]
        for i, (_, e, fn, reads, writes, busy, lat) in enumerate(ops):
            d = set()
            for b in reads:
                if id(b) in lastw:
                    d.add(lastw[id(b)])
            for b in writes:
                if id(b) in lastw:
                    d.add(lastw[id(b)])
                d.update(readers.get(id(b), ()))
            d.discard(i)
            for b in reads:
                readers.setdefault(id(b), []).append(i)
            for b in writes:
                lastw[id(b)] = i
                readers[id(b)] = []
            deps[i] = len(d)
            for j in d:
                succ[j].append(i)
        ready_t = [0.0] * n
        heaps = {e: [] for e in self.ENG}
        for i in range(n):
            if deps[i] == 0:
                heapq.heappush(heaps[ops[i][1]], (0.0, i))
        free = {e: 0.0 for e in self.ENG}
        order = []
        done = 0
        while done < n:
            best = None
            for e in self.ENG:
                h = heaps[e]
                if not h:
                    continue
                rt, i = h[0]
                st = max(rt, free[e])
                if best is None or (st, i) < (best[0], best[2]):
                    best = (st, e, i)
            st, e, i = best
            h = heaps[e]
            cand = []
            while h and h[0][0] <= st:
                cand.append(heapq.heappop(h))
            cand.sort(key=lambda x: x[1])
            rt, i = cand[0]
            for c in cand[1:]:
                heapq.heappush(h, c)
            busy, lat = ops[i][5], ops[i][6]
            free[e] = st + busy
            fin = st + lat
            order.append((st, i))
            done += 1
            for j in succ[i]:
                deps[j] -= 1
                t_ = fin + (0.15 if ops[j][1] == e else 0.9)
                if t_ > ready_t[j]:
                    ready_t[j] = t_
                if deps[j] == 0:
                    heapq.heappush(heaps[ops[j][1]], (ready_t[j], j))
        order.sort()
        for _, i in order:
            kind, e, fn, reads, writes, busy, lat = ops[i]
            if kind == "op":
                self.op(e, fn, reads, writes)
            else:
                self.dma(e, fn, reads, writes)

    def barrier(self):
        if self.pending:
            self.reorder()
        evs = []
        for e in self.ENG:
            if self.cnt[e] > 0:
                evs.append((self.esem[e], self.cnt[e]))
        for q in self.dq.values():
            for s, c in zip(q["sems"], q["cnt"]):
                if c > 0:
                    evs.append((s, c))
        for e in self.ENG:
            w = []
            for s, v in evs:
                if self.owner[s] == e:
                    continue
                if self.seen[e].get(s, 0) >= v:
                    continue
                self.seen[e][s] = v
                w.append((s, v))
            if w:
                self.prog[e].append((w, None, None, 0))

    def _run(self, lst, eng):
        for w, fn, s, inc in lst:
            for sw, v in w:
                eng.wait_ge(self.sems[sw], v)
            if fn is not None:
                fn(eng).then_inc(self.sems[s], inc)

    def flush(self):
        progs = self.prog
        self.prog = {e: [] for e in self.ENG}
        with self.nc.Block() as block:
            @block.tensor
            def _(eng):
                self._run(progs["pe"], eng)

            @block.scalar
            def _(eng):
                self._run(progs["act"], eng)

            @block.vector
            def _(eng):
                self._run(progs["dve"], eng)

            @block.gpsimd
            def _(eng):
                self._run(progs["pool"], eng)

            @block.sync
            def _(eng):
                self._run(progs["sp"], eng)


def bc(ap, shape):
    return ap.to_broadcast(list(shape))


def build(NOWN, debug=False, phases="ABC", skip_c2=False):
    NKB = 2 * NOWN
    nc = bass.Bass("TRN2", target_bir_lowering=False)

    def din(name, shape, dt=F32):
        return nc.dram_tensor(name, list(shape), dt, kind="ExternalInput")

    skind = "ExternalOutput" if debug else "Internal"

    def dsc(name, shape, dt):
        return nc.dram_tensor(name, list(shape), dt, kind=skind)

    hseq = din("hseq", [NKB, 128, D])
    hown = din("hown", [NOWN, 128, D])
    csk = din("csk", [NKB, 128, 32])
    csq = din("csq", [NOWN, 128, 32])
    w_in = din("w_in", [8, 128, IN_W])
    w_uq = din("w_uq", [2, 128, 768])
    w_ukv = din("w_ukv", [2, 128, 1024])
    w_out = din("w_out", [8, 128, 1024])
    w_query = din("w_query", [8, 128, 2048])
    skT = din("skT", [16, 128, 128])
    uT_d = din("peer_uT", [8, 128, 16384])
    v_d = din("peer_v", [16384, D])
    UB = dsc("UB", [32, 128, 8, 512], BF16)
    VB = dsc("VB", [32, 128, 4, 1024], BF16)
    XT = dsc("XT", [NOWN, 128, 8, 128], BF16)
    dUB, dVB = ([Buf() for _ in range(32)] for _ in range(2))
    dXT = [Buf() for _ in range(NOWN)]
    vecs = din("vecs", [1, NVEC])
    maskm = din("maskm", [2, 128, 128])
    biasd = din("biasd", [3, 4, 128, 128])
    y = nc.dram_tensor("y", [NOWN, 128, D], F32, kind="ExternalOutput")

    KVW = 1024 + 520 + 512 + 516
    KVD = dsc("KV", [NKB, 128, KVW], BF16)

    class _Sub:
        def __init__(self, c0, shape, p=128):
            self.c0, self.shape, self.p = c0, shape, p

        def __getitem__(self, blk):
            a, b = self.shape
            return KVD[blk, 0:self.p, self.c0:self.c0 + a * b].rearrange("p (a b) -> p a b", a=a)

    KTm, Vm, KTd, Vd = _Sub(0, (8, 128), 96), _Sub(1024, (8, 65)), _Sub(1544, (4, 128)), _Sub(2056, (4, 129))
    QTm = dsc("QTm", [NOWN, 96, 8, 128], BF16)
    QTd = dsc("QTd", [NOWN, 128, 4, 128], BF16)
    H2 = dsc("H2", [NOWN, 128, D], F32)
    dKTm, dVm, dKTd, dVd = ([Buf() for _ in range(NKB)] for _ in range(4))
    dQTm, dQTd, dH2 = ([Buf() for _ in range(NOWN)] for _ in range(3))

    with ExitStack() as ges:
        S = Sched(nc, ges)
        ncnt = [0]

        def sb(es, shape, dt=F32):
            ncnt[0] += 1
            return Tile(es.enter_context(nc.sbuf_tensor(f"t{ncnt[0]}", list(shape), dt)))

        def ps(es, shape, dt=F32):
            ncnt[0] += 1
            per_bank = 512 if dt == F32 else 1024
            n = int(np.prod(shape[1:]))
            nb_ = -(-n // per_bank)
            t = es.enter_context(nc.psum_tensor(f"p{ncnt[0]}", [128, nb_ * per_bank], dt))
            v = t[:, 0:n]
            if len(shape) == 3:
                v = v.rearrange("p (a b) -> p a b", a=shape[1])
            return Tile(v)

        def phase(name):
            if name in phases:
                with ExitStack() as pes:
                    yield pes

        def nel(ap):
            n = 1
            for s_ in ap.shape[1:]:
                n *= int(s_)
            return n

        def act(fn, r, w, dur=0.3):
            S.op("act", fn, r, w, dur)

        def dve(fn, r, w, dur=0.3):
            S.op("dve", fn, r, w, dur)

        def pe(fn, r, w, dur=0.15):
            S.op("pe", fn, r, w, dur)

        def A(out, in_, func, r, w, **kw):
            act(lambda e: e.activation(out=out, in_=in_, func=func, **kw), r, w, 0.25 + nel(out) * 0.00075)

        def TT(out, in0, in1, op, r, w, eng="dve"):
            S.op(eng, lambda e: e.tensor_tensor(out=out, in0=in0, in1=in1, op=op), r, w, 0.12 + nel(out) * 0.0016)

        def TS(out, in0, s1, s2, op0, op1, r, w, eng="dve"):
            if op1 is None:
                S.op(eng, lambda e: e.tensor_scalar(out=out, in0=in0, scalar1=s1, scalar2=None, op0=op0), r, w,
                     0.12 + nel(out) * 0.001)
            else:
                S.op(eng, lambda e: e.tensor_scalar(out=out, in0=in0, scalar1=s1, scalar2=s2, op0=op0, op1=op1), r, w,
                     0.12 + nel(out) * 0.001)

        def STT(out, in0, sc, in1, op0, op1, r, w, accum=None):
            if accum is None:
                dve(lambda e: e.scalar_tensor_tensor(out=out, in0=in0, scalar=sc, in1=in1, op0=op0, op1=op1), r, w,
                    0.12 + nel(out) * 0.0016)
            else:
                dve(lambda e: e.scalar_tensor_tensor(out=out, in0=in0, scalar=sc, in1=in1, op0=op0, op1=op1,
                                                     accum_out=accum), r, w, 0.25 + nel(out) * 0.0016)

        def RED(out, in_, r, w):
            dve(lambda e: e.tensor_reduce(out=out, in_=in_, axis=AX.X, op=ALU.add), r, w, 0.12 + nel(in_) * 0.00105)

        def CP(eng, out, in_, r, w):
            if eng == "act":
                act(lambda e: e.copy(out=out, in_=in_), r, w, 0.25 + nel(out) * 0.00075)
            else:
                S.op(eng, lambda e: e.tensor_copy(out=out, in_=in_), r, w,
                     (0.12 + nel(out) * 0.00105) if eng == "dve" else (0.3 + nel(out) * 0.0006))

        def MM(out, lhsT, rhs, start, stop, r, w):
            pe(lambda e: e.matmul(out, lhsT, rhs, start=start, stop=stop), r, w, 0.1 + nel(rhs) * 0.00052)

        def TR(out, in_, ident, r, w):
            pe(lambda e: e.transpose(out, in_, ident), r, w, 0.2)

        def DMA(eng, out, in_, r, w):
            S.dma(eng, lambda e: e.dma_start(out=out, in_=in_), r, w, 2.5 + nel(out) * 128 * 4 / 1.0e5)

        vec = sb(ges, [128, NVEC])
        ident = sb(ges, [128, 128])
        identb = sb(ges, [128, 128], BF16)
        epsc = sb(ges, [128, 1])
        gqs = sb(ges, [128, 96])
        gdqs = sb(ges, [128, 64])
        subl8 = sb(ges, [128, 128])
        neglam = sb(ges, [128, 1])
        ltmp = sb(ges, [128, 64])
        lsc = sb(ges, [128, 4])

        def V(name):
            a, b = VOFF[name]
            return vec.t[:, a:b]

        DMA("sp", vec.t[:, :], vecs[0:1, :].to_broadcast([128, NVEC]), [], [vec])
        S.op("pool", lambda e: e.memset(ident.t[:, :], 0.0), [], [ident])
        S.op("pool", lambda e: e.affine_select(out=ident.t[:, :], in_=ident.t[:, :], pattern=[[-1, 128]],
                                               compare_op=ALU.not_equal, fill=1.0, base=0, channel_multiplier=1),
             [ident], [ident])
        CP("dve", identb.t[:, :], ident.t[:, :], [ident], [identb])
        S.op("pool", lambda e: e.memset(epsc.t[:, :], EPS), [], [epsc])
        TS(gqs.t[:, :], V("gq"), 96.0 ** -0.5, None, ALU.mult, None, [vec], [gqs])
        TS(gdqs.t[:, :], V("gdq"), 0.125, None, ALU.mult, None, [vec], [gdqs])
        TS(subl8.t[:, :], V("subln"), 1.0 - LAMBDA_INIT, None, ALU.mult, None, [vec], [subl8])
        STT(ltmp.t[:, :], V("lq1"), 1.0, V("lk1"), ALU.mult, ALU.mult, [vec], [ltmp, lsc], accum=lsc.t[:, 0:1])
        STT(ltmp.t[:, :], V("lq2"), 1.0, V("lk2"), ALU.mult, ALU.mult, [vec], [ltmp, lsc], accum=lsc.t[:, 1:2])
        A(lsc.t[:, 2:4], lsc.t[:, 0:2], AF.Exp, [lsc], [lsc])
        TT(neglam.t[:, :], lsc.t[:, 3:4], lsc.t[:, 2:3], ALU.subtract, [lsc], [neglam])
        TS(neglam.t[:, :], neglam.t[:, :], -LAMBDA_INIT, None, ALU.add, None, [neglam], [neglam])

        def load_w_bf16(es, dram, nchunk, ncol, stage_pool):
            wt = sb(es, [128, nchunk, ncol], BF16)
            for c in range(nchunk):
                st = stage_pool[c % len(stage_pool)]
                DMA("sp", st.t[:, 0:ncol], dram[c], [], [st])
                if c % 2 == 0:
                    CP("dve", wt.t[:, c, :], st.t[:, 0:ncol], [st], [wt])
                else:
                    CP("pool", wt.t[:, c, :], st.t[:, 0:ncol], [st], [wt])
            return wt

        for es in phase("A"):
            S.defer = True
            stage = [sb(es, [128, IN_W]) for _ in range(2)]
            w_in_b = load_w_bf16(es, w_in, 8, IN_W, stage)
            w_uq_b = load_w_bf16(es, w_uq, 2, 768, stage)
            w_ukv_b = load_w_bf16(es, w_ukv, 2, 1024, stage)

            NB_ = 2
            hb = [sb(es, [128, D]) for _ in range(NB_)]
            cs = [sb(es, [128, 32]) for _ in range(NB_)]
            junkA = [sb(es, [128, D]) for _ in range(NB_)]
            st4 = [sb(es, [128, 8]) for _ in range(NB_)]
            nb = [sb(es, [128, D], BF16) for _ in range(NB_)]
            nT = [sb(es, [128, 8, 128], BF16) for _ in range(NB_)]
            cn = [sb(es, [128, 256], BF16) for _ in range(NB_)]
            cT = [sb(es, [128, 2, 128], BF16) for _ in range(NB_)]
            kvs = [sb(es, [128, 1024]) for _ in range(NB_)]
            dks = [sb(es, [128, 512]) for _ in range(NB_)]
            pas = [sb(es, [128, 288]) for _ in range(NB_)]
            sqA = [sb(es, [128, 1024]) for _ in range(NB_)]
            s8 = [sb(es, [128, 32]) for _ in range(NB_)]
            krg = [sb(es, [128, 8, 32]) for _ in range(NB_)]
            rr = [sb(es, [128, 8, 32]) for _ in range(NB_)]
            kk = [sb(es, [128, 8, 96], BF16) for _ in range(NB_)]
            kd = [sb(es, [128, 512], BF16) for _ in range(NB_)]
            tmp8A = [sb(es, [128, 8, 64]) for _ in range(NB_)]
            ktm = [sb(es, [128, 8, 128], BF16) for _ in range(NB_)]
            ktd = [sb(es, [128, 4, 128], BF16) for _ in range(NB_)]
            vv = [sb(es, [128, 8, 65], BF16) for _ in range(NB_)]
            vd = [sb(es, [128, 4, 129], BF16) for _ in range(NB_)]
            for t in ktm:
                S.op("pool", lambda e, t=t: e.memset(t.t[:, :, :], 0.0), [], [t])
            for t in vv:
                S.op("pool", lambda e, t=t: e.memset(t.t[:, :, 64:65], 1.0), [], [t])
            for t in vd:
                S.op("pool", lambda e, t=t: e.memset(t.t[:, :, 128:129], 1.0), [], [t])

            TP = ps(es, [128, 8, 128], BF16)
            TK = ps(es, [128, 16, 128], BF16)
            PA = ps(es, [128, 512])
            PB = ps(es, [128, 512])
            PC = ps(es, [128, 512])
            PD = ps(es, [128, 1024])

            def rstd_of(out, ss, dim, rbufs, wbuf, tmp):
                A(tmp, ss, AF.Ln, rbufs + [epsc], [wbuf], scale=1.0 / dim, bias=epsc.t[:, 0:1])
                A(out, tmp, AF.Exp, [wbuf], [wbuf], scale=-0.5)

            def p1(kside, blk, it):
                k = it % NB_
                junk, sq, tmp8 = junkA[k], sqA[k], tmp8A[k]
                H, C = hb[k], cs[k]
                src = hseq[blk] if kside else hown[blk]
                DMA("sp", H.t[:, :], src, [], [H])
                DMA("sp", C.t[:, :], (csk if kside else csq)[blk], [], [C])
                st = st4[k]
                A(junk.t[:, :], H.t[:, :], AF.Square, [H], [junk, st], accum_out=st.t[:, 0:1])
                rstd_of(st.t[:, 2:3], st.t[:, 0:1], D, [st], st, st.t[:, 1:2])
                STT(nb[k].t[:, :], H.t[:, :], st.t[:, 2:3], V("attn_norm"), ALU.mult, ALU.mult, [H, st, vec], [nb[k]])
                for c in range(8):
                    TR(TP.t[:, c, :], nb[k].t[:, c * 128:(c + 1) * 128], identb.t[:, :], [nb[k], identb], [TP])
                CP("act", nT[k].t[:, :, :], TP.t[:, :, :], [TP], [nT[k]])
                if kside:
                    groups = [(PA, 288, 256), (PB, 512, 1056), (PC, 512, 1568)]
                else:
                    groups = [(PA, 256, 0), (PB, 512, 544)]
                for (pt, n, c0) in groups:
                    for c in range(8):
                        MM(pt.t[:, 0:n], nT[k].t[:, c, :], w_in_b.t[:, c, c0:c0 + n], c == 0, c == 7,
                           [nT[k], w_in_b], [pt])
                npa = 288 if kside else 256
                CP("act", pas[k].t[:, 0:npa], PA.t[:, 0:npa], [PA], [pas[k]])
                CP("act", dks[k].t[:, :], PB.t[:, :], [PB], [dks[k]])
                if kside:
                    CP("dve", vd[k].t[:, :, 0:128], PC.t[:, :].rearrange("p (h e) -> p h e", h=4), [PC], [vd[k]])
                    DMA("sp", Vd[blk], vd[k].t[:, :, :], [vd[k]], [dVd[blk]])

            def p2(kside, blk, it):
                k = it % NB_
                junk, sq, tmp8 = junkA[k], sqA[k], tmp8A[k]
                C = cs[k]
                st = st4[k]
                gname = "kv_norm" if kside else "q_norm"
                A(junk.t[:, 0:256], pas[k].t[:, 0:256], AF.Square, [pas[k]], [junk, st], accum_out=st.t[:, 3:4])
                rstd_of(st.t[:, 5:6], st.t[:, 3:4], 256, [st], st, st.t[:, 4:5])
                STT(cn[k].t[:, :], pas[k].t[:, 0:256], st.t[:, 5:6], V(gname), ALU.mult, ALU.mult, [pas[k], st, vec], [cn[k]])
                for c in range(2):
                    TR(TK.t[:, 12 + c, :], cn[k].t[:, c * 128:(c + 1) * 128], identb.t[:, :], [cn[k], identb], [TK])
                CP("act", cT[k].t[:, :, :], TK.t[:, 12:14, :], [TK], [cT[k]])
                wup = w_ukv_b if kside else w_uq_b
                nup = 1024 if kside else 768
                for h0 in range(0, nup, 512):
                    n = min(512, nup - h0)
                    for c in range(2):
                        MM(PD.t[:, h0:h0 + n], cT[k].t[:, c, :], wup.t[:, c, h0:h0 + n], c == 0, c == 1,
                           [cT[k], wup], [PD])
                KV = kvs[k]
                CP("act", KV.t[:, 0:nup], PD.t[:, 0:nup], [PD], [KV])
                s = s8[k]
                if kside:
                    kv3 = KV.t[:, :].rearrange("p (h e) -> p h e", h=8)
                    TT(sq.t[:, 0:512].rearrange("p (h e) -> p h e", h=8), kv3[:, :, 0:64], kv3[:, :, 0:64], ALU.mult,
                       [KV], [sq])
                    RED(s.t[:, 0:8], sq.t[:, 0:512].rearrange("p (h e) -> p h e", h=8), [sq], [s])
                    CP("act", krg[k].t[:, 0, :], pas[k].t[:, 256:288], [pas[k]], [krg[k]])
                    A(junk.t[:, 0:32], krg[k].t[:, 0, :], AF.Square, [krg[k]], [junk, st], accum_out=st.t[:, 6:7])
                    TS(s.t[:, 0:8], s.t[:, 0:8], st.t[:, 6:7], None, ALU.add, None, [s, st], [s])
                    rstd_of(s.t[:, 16:24], s.t[:, 0:8], 96, [s], s, s.t[:, 8:16])
                    K3 = kk[k].t
                    TT(tmp8.t[:, :, :], kv3[:, :, 0:64], bc(s.t[:, 16:24].unsqueeze(2), [128, 8, 64]), ALU.mult,
                       [KV, s], [tmp8])
                    TT(K3[:, :, 0:64], tmp8.t[:, :, :], bc(V("gk")[:, 0:64].unsqueeze(1), [128, 8, 64]), ALU.mult,
                       [tmp8, vec], [kk[k]])
                    t0 = krg[k].t[:, 1, :]
                    TT(t0, krg[k].t[:, 0, :], V("gk")[:, 64:96], ALU.mult, [krg[k], vec], [krg[k]])
                    co, si = C.t[:, 0:16], C.t[:, 16:32]
                    r1, r2 = rr[k].t[:, 0, 0:16], rr[k].t[:, 0, 16:32]
                    a1, a2 = rr[k].t[:, 1, 0:16], rr[k].t[:, 1, 16:32]
                    TT(a1, t0[:, 0:16], co, ALU.mult, [krg[k], C], [rr[k]])
                    TT(a2, t0[:, 16:32], si, ALU.mult, [krg[k], C], [rr[k]])
                    TT(r1, a1, a2, ALU.subtract, [rr[k]], [rr[k]])
                    TT(a1, t0[:, 0:16], si, ALU.mult, [krg[k], C, rr[k]], [rr[k]])
                    TT(a2, t0[:, 16:32], co, ALU.mult, [krg[k], C, rr[k]], [rr[k]])
                    TT(r2, a1, a2, ALU.add, [rr[k]], [rr[k]])
                    TT(K3[:, :, 64:96], bc(rr[k].t[:, 0:1, :], [128, 8, 32]), bc(s.t[:, 16:24].unsqueeze(2), [128, 8, 32]),
                       ALU.mult, [rr[k], s], [kk[k]])
                    for h in range(8):
                        TR(TK.t[0:96, h, :], K3[:, h, :], identb.t[:, :], [kk[k], identb], [TK])
                    CP("act", ktm[k].t[0:96, :, :], TK.t[0:96, 0:8, :], [TK], [ktm[k]])
                    DMA("sp", KVD[blk, :, 0:1024], ktm[k].t[:, :, :].rearrange("p a b -> p (a b)"), [ktm[k]], [dKTm[blk]])
                    CP("pool", vv[k].t[:, :, 0:64], kv3[:, :, 64:128], [KV], [vv[k]])
                    DMA("sp", Vm[blk], vv[k].t[:, :, :], [vv[k]], [dVm[blk]])
                    d3 = dks[k].t[:, :].rearrange("p (g e) -> p g e", g=8)
                    TT(sq.t[:, 512:1024].rearrange("p (g e) -> p g e", g=8), d3, d3, ALU.mult, [dks[k]], [sq])
                    RED(s.t[:, 24:32], sq.t[:, 512:1024].rearrange("p (g e) -> p g e", g=8), [sq], [s])
                    rstd_of(s.t[:, 24:32], s.t[:, 24:32], 64, [s], s, s.t[:, 8:16])
                    TT(tmp8.t[:, :, :], d3, bc(s.t[:, 24:32].unsqueeze(2), [128, 8, 64]), ALU.mult, [dks[k], s], [tmp8])
                    TT(kd[k].t[:, :].rearrange("p (g e) -> p g e", g=8), tmp8.t[:, :, :],
                       bc(V("gdk").unsqueeze(1), [128, 8, 64]), ALU.mult, [tmp8, vec], [kd[k]])
                    for h in range(4):
                        TR(TK.t[:, 8 + h, :], kd[k].t[:, h * 128:(h + 1) * 128], identb.t[:, :], [kd[k], identb], [TK])
                    CP("act", ktd[k].t[:, :, :], TK.t[:, 8:12, :], [TK], [ktd[k]])
                    DMA("sp", KTd[blk], ktd[k].t[:, :, :], [ktd[k]], [dKTd[blk]])
                else:
                    q3 = KV.t[:, 0:768].rearrange("p (h e) -> p h e", h=8)
                    TT(sq.t[:, 0:768].rearrange("p (h e) -> p h e", h=8), q3, q3, ALU.mult, [KV], [sq])
                    RED(s.t[:, 0:8], sq.t[:, 0:768].rearrange("p (h e) -> p h e", h=8), [sq], [s])
                    rstd_of(s.t[:, 16:24], s.t[:, 0:8], 96, [s], s, s.t[:, 8:16])
                    Q3 = kk[k].t
                    TT(tmp8.t[:, :, :], q3[:, :, 0:64], bc(s.t[:, 16:24].unsqueeze(2), [128, 8, 64]), ALU.mult,
                       [KV, s], [tmp8])
                    TT(Q3[:, :, 0:64], tmp8.t[:, :, :], bc(gqs.t[:, 0:64].unsqueeze(1), [128, 8, 64]), ALU.mult,
                       [tmp8, gqs], [kk[k]])
                    tq = krg[k].t
                    TT(tq[:, :, :], q3[:, :, 64:96], bc(s.t[:, 16:24].unsqueeze(2), [128, 8, 32]), ALU.mult,
                       [KV, s], [krg[k]])
                    TT(tq[:, :, :], tq[:, :, :], bc(gqs.t[:, 64:96].unsqueeze(1), [128, 8, 32]), ALU.mult,
                       [krg[k], gqs], [krg[k]])
                    co = bc(C.t[:, 0:16].unsqueeze(1), [128, 8, 16])
                    si = bc(C.t[:, 16:32].unsqueeze(1), [128, 8, 16])
                    a1, a2 = rr[k].t[:, :, 0:16], rr[k].t[:, :, 16:32]
                    TT(a1, tq[:, :, 0:16], co, ALU.mult, [krg[k], C], [rr[k]])
                    TT(a2, tq[:, :, 16:32], si, ALU.mult, [krg[k], C], [rr[k]])
                    TT(Q3[:, :, 64:80], a1, a2, ALU.subtract, [rr[k]], [kk[k]])
                    TT(a1, tq[:, :, 0:16], si, ALU.mult, [krg[k], C, kk[k]], [rr[k]])
                    TT(a2, tq[:, :, 16:32], co, ALU.mult, [krg[k], C, kk[k]], [rr[k]])
                    TT(Q3[:, :, 80:96], a1, a2, ALU.add, [rr[k]], [kk[k]])
                    for h in range(8):
                        TR(TK.t[0:96, h, :], Q3[:, h, :], identb.t[:, :], [kk[k], identb], [TK])
                    CP("act", ktm[k].t[0:96, :, :], TK.t[0:96, 0:8, :], [TK], [ktm[k]])
                    DMA("sp", QTm[blk], ktm[k].t[0:96, :, :], [ktm[k]], [dQTm[blk]])
                    d3 = dks[k].t[:, :].rearrange("p (g e) -> p g e", g=8)
                    TT(sq.t[:, 512:1024].rearrange("p (g e) -> p g e", g=8), d3, d3, ALU.mult, [dks[k]], [sq])
                    RED(s.t[:, 24:32], sq.t[:, 512:1024].rearrange("p (g e) -> p g e", g=8), [sq], [s])
                    rstd_of(s.t[:, 24:32], s.t[:, 24:32], 64, [s], s, s.t[:, 8:16])
                    TT(tmp8.t[:, :, :], d3, bc(s.t[:, 24:32].unsqueeze(2), [128, 8, 64]), ALU.mult, [dks[k], s], [tmp8])
                    TT(kd[k].t[:, :].rearrange("p (g e) -> p g e", g=8), tmp8.t[:, :, :],
                       bc(gdqs.t[:, :].unsqueeze(1), [128, 8, 64]), ALU.mult, [tmp8, gdqs], [kd[k]])
                    for h in range(4):
                        TR(TK.t[:, 8 + h, :], kd[k].t[:, h * 128:(h + 1) * 128], identb.t[:, :], [kd[k], identb], [TK])
                    CP("act", ktd[k].t[:, :, :], TK.t[:, 8:12, :], [TK], [ktd[k]])
                    DMA("sp", QTd[blk], ktd[k].t[:, :, :], [ktd[k]], [dQTd[blk]])

            seq = [(True, blk) for blk in range(NKB)] + [(False, blk) for blk in range(NOWN)]
            p1(seq[0][0], seq[0][1], 0)
            for n in range(len(seq)):
                if n + 1 < len(seq):
                    p1(seq[n + 1][0], seq[n + 1][1], n + 1)
                p2(seq[n][0], seq[n][1], n)
            S.barrier()
            S.flush()

        for es in phase("B"):
            S.defer = True
            stage = [sb(es, [128, 1024]) for _ in range(2)]
            w_out_b = load_w_bf16(es, w_out, 8, 1024, stage)
            mk = sb(es, [128, 2, 128])
            bd = sb(es, [128, 12, 128])
            b31c = V("b31")
            for t in range(2):
                DMA("sp", mk.t[:, t, :], maskm[t], [], [mk])
            for t in range(3):
                for h in range(4):
                    DMA("sp", bd.t[:, t * 4 + h, :], biasd[t, h], [], [bd])
            for t in range(3):
                for h in range(4):
                    TS(bd.t[:, t * 4 + h, :], bd.t[:, t * 4 + h, :], b31c[:, h:h + 1], None, ALU.subtract, None,
                       [bd, vec], [bd])
            mkb = sb(es, [128, 2, 128], BF16)
            bdh = sb(es, [128, 12, 128], BF16)
            bdl = sb(es, [128, 12, 128], BF16)
            bdr = sb(es, [128, 12, 128])
            CP("dve", mkb.t[:, :, :], mk.t[:, :, :], [mk], [mkb])
            CP("dve", bdh.t[:, :, :], bd.t[:, :, :], [bd], [bdh])
            TT(bdr.t[:, :, :], bd.t[:, :, :], bdh.t[:, :, :], ALU.subtract, [bd, bdh], [bdr])
            CP("dve", bdl.t[:, :, :], bdr.t[:, :, :], [bdr], [bdl])
            NKV = 6
            kvt = [sb(es, [128, KVW], BF16) for _ in range(NKV)]
            qtm = [sb(es, [128, 8, 128], BF16) for _ in range(2)]
            qtd = [sb(es, [128, 4, 128], BF16) for _ in range(2)]
            ptm = [sb(es, [128, 4, 128], BF16) for _ in range(4)]
            ptd = [sb(es, [128, 4, 128], BF16) for _ in range(4)]
            hb = [sb(es, [128, D]) for _ in range(2)]
            mixb = sb(es, [128, D], BF16)
            mixT = sb(es, [128, 8, 128], BF16)
            h2 = [sb(es, [128, D]) for _ in range(2)]
            rec = sb(es, [128, 16])
            od = sb(es, [128, 8, 128])
            odd = sb(es, [128, 4, 128])
            sq = sb(es, [128, 4, 128])
            s4 = sb(es, [128, 12])

            AM = [ps(es, [128, 4, 65]) for _ in range(2)]
            AD = [ps(es, [128, 3, 129]), ps(es, [128, 3, 129]), ps(es, [128, 2, 129])]
            SS = [ps(es, [128, 4, 128]) for _ in range(3)]
            sidx = [0]

            def next_s():
                t = SS[sidx[0] % 3]
                sidx[0] += 1
                return t

            if "C" in phases:
                cst = [sb(es, [128, 4096]) for _ in range(2)]
                cbf = [sb(es, [128, 4096], BF16) for _ in range(2)]
                for gq in range(32):
                    for which in range(2):
                        j = (gq * 2 + which) % 2
                        if which == 0:
                            src_ap = uT_d[:, :, gq * 512:(gq + 1) * 512].rearrange("k d e -> d k e")
                            dst_ap, dbuf = UB[gq].rearrange("d k e -> d (k e)"), dUB[gq]
                            stv = cst[j].t[:, :].rearrange("p (k e) -> p k e", k=8)
                        else:
                            src_ap = v_d[gq * 512:(gq + 1) * 512, :].rearrange("(c e) d -> e c d", c=4)
                            dst_ap, dbuf = VB[gq].rearrange("e c d -> e (c d)"), dVB[gq]
                            stv = cst[j].t[:, :].rearrange("p (c d) -> p c d", c=4)
                        DMA("pool", stv, src_ap, [], [cst[j]])
                        CP("pool", cbf[j].t[:, :], cst[j].t[:, :], [cst[j]], [cbf[j]])
                        DMA("pool", dst_ap, cbf[j].t[:, :], [cbf[j]], [dbuf])
            pcount = [0]
            kvit = 0
            for i in range(NOWN):
                Qm, Qd = qtm[i % 2], qtd[i % 2]
                DMA("sp", Qm.t[0:96, :, :], QTm[i], [dQTm[i]], [Qm])
                DMA("sp", Qd.t[:, :, :], QTd[i], [dQTd[i]], [Qd])
                H = hb[i % 2]
                DMA("sp", H.t[:, :], hown[i], [], [H])
                last = 2 * i + 1
                pend = []
                for kb in range(0, last + 1):
                    kq = kvit % NKV
                    kvit += 1
                    kvb = kvt[kq]
                    DMA("sp", kvb.t[:, :], KVD[kb], [dKTm[kb], dVm[kb], dKTd[kb], dVd[kb]], [kvb])

                    def _v(c0, a, b, kvb=kvb):
                        t_ = Tile(kvb.t[:, c0:c0 + a * b].rearrange("p (a b) -> p a b", a=a))
                        t_.b = kvb.b
                        return t_

                    Km, Vmt, Kd, Vdt = _v(0, 8, 128), _v(1024, 8, 65), _v(1544, 4, 128), _v(2056, 4, 129)
                    t = kb - 2 * i
                    first, fin = (kb == 0), (kb == last)
                    def mk_mla(half, t=t, first=first, fin=fin, Km=Km, Vmt=Vmt):
                        box = {}

                        def fS():
                            St = next_s()
                            for hh in range(4):
                                h = half * 4 + hh
                                sp_ = t in (0, 1)
                                MM(St.t[:, hh, :], Km.t[0:96, h, :], Qm.t[0:96, h, :], True, not sp_, [Km, Qm], [St])
                                if sp_:
                                    MM(St.t[:, hh, :], identb.t[:, :], mkb.t[:, t, :], False, True, [identb, mkb], [St])
                            P = ptm[pcount[0] % 4]
                            pcount[0] += 1
                            A(P.t[:, :, :], St.t[:, :, :], AF.Exp, [St], [P])
                            box["P"] = P

                        def fPV():
                            P = box["P"]
                            for hh in range(4):
                                h = half * 4 + hh
                                MM(AM[half].t[:, hh, :], P.t[:, hh, :], Vmt.t[:, h, :], first and hh == 0, fin and hh == 3,
                                   [P, Vmt], [AM[half]])
                        return fS, fPV

                    def mk_diff(t=t, first=first, fin=fin, Kd=Kd, Vdt=Vdt):
                        box = {}

                        def fS():
                            sp_ = t in (-1, 0, 1)
                            Sm = [next_s(), next_s()]
                            for h in range(4):
                                for m in range(2):
                                    MM(Sm[m].t[:, h, :], Kd.t[m * 64:(m + 1) * 64, h, :], Qd.t[m * 64:(m + 1) * 64, h, :],
                                       True, not sp_, [Kd, Qd], [Sm[m]])
                                    if sp_:
                                        MM(Sm[m].t[:, h, :], identb.t[:, :], bdh.t[:, (t + 1) * 4 + h, :], False, False,
                                           [identb, bdh], [Sm[m]])
                                        MM(Sm[m].t[:, h, :], identb.t[:, :], bdl.t[:, (t + 1) * 4 + h, :], False, True,
                                           [identb, bdl], [Sm[m]])
                            Pm = []
                            for m in range(2):
                                P = ptd[pcount[0] % 4]
                                pcount[0] += 1
                                A(P.t[:, :, :], Sm[m].t[:, :, :], AF.Exp, [Sm[m]], [P])
                                Pm.append(P)
                            box["Pm"] = Pm

                        def fPV():
                            Pm = box["Pm"]
                            for h in range(4):
                                for m in range(2):
                                    g = h * 2 + m
                                    MM(AD[g // 3].t[:, g % 3, :], Pm[m].t[:, h, :], Vdt.t[:, h, :], first and g in (0, 3, 6),
                                       fin and g in (2, 5, 7), [Pm[m], Vdt], [AD[g // 3]])
                        return fS, fPV

                    for stg in (mk_mla(0), mk_mla(1), mk_diff()):
                        stg[0]()
                        if pend:
                            pend.pop(0)()
                        pend.append(stg[1])
                while pend:
                    pend.pop(0)()
                for half in range(2):
                    dve(lambda e, half=half: e.reciprocal(out=rec.t[:, half * 4:half * 4 + 4],
                                                          in_=AM[half].t[:, :, 64]), [AM[half]], [rec])
                    TT(mixb.t[:, half * 256:(half + 1) * 256].rearrange("p (h e) -> p h e", h=4), AM[half].t[:, :, 0:64],
                       bc(rec.t[:, half * 4:half * 4 + 4].unsqueeze(2), [128, 4, 64]), ALU.mult, [AM[half], rec], [mixb])
                for a, n0, n in ((0, 0, 3), (1, 3, 3), (2, 6, 2)):
                    dve(lambda e, a=a, n0=n0, n=n: e.reciprocal(out=rec.t[:, 8 + n0:8 + n0 + n], in_=AD[a].t[:, :, 128]),
                        [AD[a]], [rec])
                    TT(od.t[:, n0:n0 + n, :], AD[a].t[:, :, 0:128], bc(rec.t[:, 8 + n0:8 + n0 + n].unsqueeze(2), [128, n, 128]),
                       ALU.mult, [AD[a], rec], [od])
                o4 = od.t[:, :, :].rearrange("p (h m) e -> p h m e", m=2)
                STT(odd.t[:, :, :], o4[:, :, 1, :], neglam.t[:, 0:1], o4[:, :, 0, :], ALU.mult, ALU.add, [od, neglam], [odd])
                TT(sq.t[:, :, :], odd.t[:, :, :], odd.t[:, :, :], ALU.mult, [odd], [sq])
                RED(s4.t[:, 0:4], sq.t[:, :, :], [sq], [s4])
                A(s4.t[:, 4:8], s4.t[:, 0:4], AF.Ln, [s4, epsc], [s4], scale=1.0 / 128, bias=epsc.t[:, 0:1])
                A(s4.t[:, 8:12], s4.t[:, 4:8], AF.Exp, [s4], [s4], scale=-0.5)
                TT(sq.t[:, :, :], odd.t[:, :, :], bc(s4.t[:, 8:12].unsqueeze(2), [128, 4, 128]), ALU.mult, [odd, s4], [sq])
                TT(mixb.t[:, 512:1024].rearrange("p (h e) -> p h e", h=4), sq.t[:, :, :],
                   bc(subl8.t[:, :].unsqueeze(1), [128, 4, 128]), ALU.mult, [sq, subl8], [mixb])
                Ta = next_s()
                Tv = Ta.t[:, :, :].rearrange("p a b -> p (a b)").bitcast(BF16).rearrange("p (c q) -> p c q", c=8)
                for c in range(8):
                    TR(Tv[:, c, :], mixb.t[:, c * 128:(c + 1) * 128], identb.t[:, :], [mixb, identb], [Ta])
                CP("act", mixT.t[:, :, :], Tv, [Ta], [mixT])
                Hh = h2[i % 2]
                for half in range(2):
                    Po = next_s()
                    Pf = Po.t[:, :, :].rearrange("p a b -> p (a b)")
                    for c in range(8):
                        MM(Pf, mixT.t[:, c, :], w_out_b.t[:, c, half * 512:(half + 1) * 512], c == 0, c == 7,
                           [mixT, w_out_b], [Po])
                    TT(Hh.t[:, half * 512:(half + 1) * 512], Pf, H.t[:, half * 512:(half + 1) * 512], ALU.add,
                       [Po, H], [Hh])
                DMA("sp", H2[i], Hh.t[:, :], [Hh], [dH2[i]])
            S.barrier()
            S.flush()

        for es in phase("C"):
          T = NOWN * 128
          E1T = sb(es, [128, T], BF16)
          E2T = sb(es, [128, T], BF16)
          GGT = sb(es, [128, T], BF16)
          iotaf = sb(es, [128, 128])
          S.op("pool", lambda e: e.iota(iotaf.t[:, :], pattern=[[1, 128]], base=0, channel_multiplier=0,
                                        allow_small_or_imprecise_dtypes=True), [], [iotaf])
          with ExitStack() as es1:
            es_outer = es
            es = es1
            S.defer = True
            stage = [sb(es, [128, 2048]) for _ in range(2)]
            w_q_b = load_w_bf16(es, w_query, 8, 2048, stage)
            skb = sb(es, [128, 16, 128], BF16)
            for c in range(16):
                st = stage[c % 2]
                DMA("sp", st.t[:, 0:128], skT[c], [], [st])
                CP("dve", skb.t[:, c, :], st.t[:, 0:128], [st], [skb])
            thr = sb(es, [128, 15])
            io16 = sb(es, [128, 16])
            for m in range(15):
                S.op("pool", lambda e, m=m: e.memset(thr.t[:, m:m + 1], 16.0 * (m + 1) - 0.5), [], [thr])
            for m in range(16):
                S.op("pool", lambda e, m=m: e.memset(io16.t[:, m:m + 1], float(m)), [], [io16])
            hh2 = [sb(es, [128, D]) for _ in range(2)]
            xn = [sb(es, [128, D]) for _ in range(2)]
            xb2 = [sb(es, [128, D], BF16) for _ in range(2)]
            xT2 = [sb(es, [128, 8, 128], BF16) for _ in range(2)]
            junk2_ = [sb(es, [128, D]) for _ in range(2)]
            st42 = [sb(es, [128, 4]) for _ in range(2)]
            qpT2 = [sb(es, [128, 16, 128], BF16) for _ in range(2)]
            sc2 = [sb(es, [128, 16, 128]) for _ in range(2)]
            t16 = sb(es, [128, 8, 2, 16])
            ix = sb(es, [128, 8, 2, 16], U32)
            ixf = sb(es, [128, 8, 2, 16])
            scr = sb(es, [128, 128])
            cand = sb(es, [128, 8, 256])
            scr2 = sb(es, [128, 256])
            best = sb(es, [128, 8, 16])
            fx = sb(es, [128, 8, 16], U32)
            fxf = sb(es, [128, 128])
            cmp = sb(es, [128, 128, 16])
            fi = sb(es, [128, 128])
            fj = sb(es, [128, 128])
            e1 = sb(es, [128, 128])
            e2 = sb(es, [128, 128])
            ge = sb(es, [128, 8, 16])
            gs = sb(es, [128, 16])
            gg = sb(es, [128, 128])
            TPx = ps(es, [128, 8, 128], BF16)
            QS = ps(es, [128, 16, 128])
            t16s = [[Tile(t16.t[:, h, p, :]) for p in range(2)] for h in range(8)]
            ixs = [[Tile(ix.t[:, h, p, :]) for p in range(2)] for h in range(8)]
            scrs = [[sb(es, [128, 128]) for p in range(2)] for h in range(8)]
            bests = [Tile(best.t[:, h, :]) for h in range(8)]
            fxs = [Tile(fx.t[:, h, :]) for h in range(8)]
            scr2s = [sb(es, [128, 256]) for h in range(8)]

            def stage1(i):
                k = i % 2
                xb, xT, junk, st4, qpT, sc = xb2[k], xT2[k], junk2_[k], st42[k], qpT2[k], sc2[k]
                Hh, X = hh2[k], xn[k]
                DMA("sp", Hh.t[:, :], H2[i], [dH2[i]], [Hh])
                A(junk.t[:, :], Hh.t[:, :], AF.Square, [Hh], [junk, st4], accum_out=st4.t[:, 0:1])
                A(st4.t[:, 1:2], st4.t[:, 0:1], AF.Ln, [st4, epsc], [st4], scale=1.0 / D, bias=epsc.t[:, 0:1])
                A(st4.t[:, 2:3], st4.t[:, 1:2], AF.Exp, [st4], [st4], scale=-0.5)
                STT(X.t[:, :], Hh.t[:, :], st4.t[:, 2:3], V("ffn_norm"), ALU.mult, ALU.mult, [Hh, st4, vec], [X])
                CP("pool", xb.t[:, :], X.t[:, :], [X], [xb])
                for c in range(8):
                    TR(TPx.t[:, c, :], xb.t[:, c * 128:(c + 1) * 128], identb.t[:, :], [xb, identb], [TPx])
                CP("act", xT.t[:, :, :], TPx.t[:, :, :], [TPx], [xT])
                for c in range(16):
                    for kc in range(8):
                        MM(QS.t[:, c, :], w_q_b.t[:, kc, c * 128:(c + 1) * 128], xT.t[:, kc, :], kc == 0, kc == 7,
                           [w_q_b, xT], [QS])
                CP("act", qpT.t[:, 0:8, :], QS.t[:, 0:8, :], [QS], [qpT])
                CP("dve", qpT.t[:, 8:16, :], QS.t[:, 8:16, :], [QS], [qpT])
                for c in range(16):
                    MM(QS.t[:, c, :], qpT.t[:, c, :], skb.t[:, c, :], True, True, [qpT, skb], [QS])
                CP("act", sc.t[:, 0:8, :], QS.t[:, 0:8, :], [QS], [sc])
                CP("dve", sc.t[:, 8:16, :], QS.t[:, 8:16, :], [QS], [sc])
                HP = [(h, p) for h in range(8) for p in range(2)]
                for h, p in HP:
                    dve(lambda e, h=h, p=p: e.max(out=t16.t[:, h, p, 0:8], in_=sc.t[:, 2 * h + p, :]), [sc], [t16s[h][p]])
                for h, p in HP:
                    dve(lambda e, h=h, p=p: e.match_replace(out=scrs[h][p].t[:, :], in_to_replace=t16.t[:, h, p, 0:8],
                                                            in_values=sc.t[:, 2 * h + p, :], imm_value=-1e30),
                        [sc, t16s[h][p]], [scrs[h][p]])
                for h, p in HP:
                    dve(lambda e, h=h, p=p: e.max(out=t16.t[:, h, p, 8:16], in_=scrs[h][p].t[:, :]), [scrs[h][p]], [t16s[h][p]])
                for h, p in HP:
                    dve(lambda e, h=h, p=p: e.max_index(out=ix.t[:, h, p, 0:8], in_max=t16.t[:, h, p, 0:8],
                                                        in_values=sc.t[:, 2 * h + p, :]), [sc, t16s[h][p]], [ixs[h][p]])
                for h, p in HP:
                    dve(lambda e, h=h, p=p: e.max_index(out=ix.t[:, h, p, 8:16], in_max=t16.t[:, h, p, 8:16],
                                                        in_values=sc.t[:, 2 * h + p, :]), [sc, t16s[h][p]], [ixs[h][p]])
                all_t16 = [t16s[h][p] for h, p in HP]
                all_ix = [ixs[h][p] for h, p in HP]
                CP("dve", ixf.t[:, :, :, :], ix.t[:, :, :, :], all_ix, [ixf])
                TT(cand.t[:, :, :].rearrange("p h (a b) -> p h a b", a=16), bc(t16.t[:, :, 0, :].unsqueeze(3), [128, 8, 16, 16]),
                   bc(t16.t[:, :, 1, :].unsqueeze(2), [128, 8, 16, 16]), ALU.add, all_t16, [cand])
                for h in range(8):
                    dve(lambda e, h=h: e.max(out=best.t[:, h, 0:8], in_=cand.t[:, h, :]), [cand], [bests[h]])
                for h in range(8):
                    dve(lambda e, h=h: e.match_replace(out=scr2s[h].t[:, :], in_to_replace=best.t[:, h, 0:8],
                                                       in_values=cand.t[:, h, :], imm_value=-1e30), [cand, bests[h]], [scr2s[h]])
                for h in range(8):
                    dve(lambda e, h=h: e.max(out=best.t[:, h, 8:16], in_=scr2s[h].t[:, :]), [scr2s[h]], [bests[h]])
                for h in range(8):
                    dve(lambda e, h=h: e.max_index(out=fx.t[:, h, 0:8], in_max=best.t[:, h, 0:8],
                                                   in_values=cand.t[:, h, :]), [cand, bests[h]], [fxs[h]])
                for h in range(8):
                    dve(lambda e, h=h: e.max_index(out=fx.t[:, h, 8:16], in_max=best.t[:, h, 8:16],
                                                   in_values=cand.t[:, h, :]), [cand, bests[h]], [fxs[h]])
                best_all = bests
                CP("dve", fxf.t[:, :], fx.t[:, :, :].rearrange("p h k -> p (h k)"), fxs, [fxf])
                TT(cmp.t[:, :, 0:15], bc(fxf.t[:, :].unsqueeze(2), [128, 128, 15]), bc(thr.t[:, :].unsqueeze(1), [128, 128, 15]),
                   ALU.is_ge, [fxf, thr], [cmp])
                RED(fi.t[:, :], cmp.t[:, :, 0:15], [cmp], [fi])
                STT(fj.t[:, :], fi.t[:, :], -16.0, fxf.t[:, :], ALU.mult, ALU.add, [fi, fxf], [fj])
                c4 = cmp.t[:, :, :].rearrange("p (h k) i -> p h k i", h=8)
                for (fsel, pidx, eo) in ((fi, 0, e1), (fj, 1, e2)):
                    TT(cmp.t[:, :, :], bc(io16.t[:, :].unsqueeze(1), [128, 128, 16]), bc(fsel.t[:, :].unsqueeze(2), [128, 128, 16]),
                       ALU.is_equal, [io16, fsel], [cmp])
                    TT(c4, c4, bc(ixf.t[:, :, pidx, :].unsqueeze(2), [128, 8, 16, 16]), ALU.mult, [cmp, ixf], [cmp])
                    RED(eo.t[:, :], cmp.t[:, :, :], [cmp], [eo])
                TT(ge.t[:, :, :], best.t[:, :, :], bc(best.t[:, :, 0:1], [128, 8, 16]), ALU.subtract, bests, [ge])
                A(ge.t[:, :, :], ge.t[:, :, :], AF.Exp, [ge], [ge])
                RED(gs.t[:, 0:8], ge.t[:, :, :], [ge], [gs])
                dve(lambda e: e.reciprocal(out=gs.t[:, 8:16], in_=gs.t[:, 0:8]), [gs], [gs])
                TT(gg.t[:, :].rearrange("p (h k) -> p h k", h=8), ge.t[:, :, :], bc(gs.t[:, 8:16].unsqueeze(2), [128, 8, 16]),
                   ALU.mult, [ge, gs], [gg])


            for i in range(NOWN):
                stage1(i)
                DMA("sp", XT[i], xT2[i % 2].t[:, :, :], [xT2[i % 2]], [dXT[i]])
                for j, (srcT, dstT) in enumerate(((e1, E1T), (e2, E2T), (gg, GGT))):
                    MM(QS.t[:, j, :], srcT.t[:, :], ident.t[:, :], True, True, [srcT, ident], [QS])
                    CP("act", dstT.t[:, i * 128:(i + 1) * 128], QS.t[:, j, :], [QS], [dstT])
            S.barrier()
            S.flush()
            es = es_outer
          S.defer = True
          GB = 3
          NGRP = 0 if skip_c2 else -(-NOWN // GB)
          Wg = sb(es, [128, 128, GB * 128], BF16)
          xg = sb(es, [128, 8, GB * 128], BF16)
          hg = [sb(es, [128, D]) for _ in range(GB)]
          ust = [sb(es, [128, 8, 512], BF16) for _ in range(2)]
          vst = [sb(es, [128, 4, 1024], BF16) for _ in range(2)]
          gl = [sb(es, [128, GB * 128], BF16) for _ in range(3)]
          wa = [sb(es, [128, GB * 128], BF16) for _ in range(3)]
          At = [sb(es, [128, 128], BF16) for _ in range(4)]
          Bt = [sb(es, [128, 128], BF16) for _ in range(4)]
          yo = sb(es, [128, D])
          ACC = [ps(es, [128, D]) for _ in range(GB)]
          BK = [ps(es, [128, 512]) for _ in range(2)]
          ldn = [0]
          for grp in range(NGRP):
              blks = list(range(grp * GB, min(NOWN, (grp + 1) * GB)))
              nb_ = len(blks)
              G = nb_ * 128
              for j, bi in enumerate(blks):
                  DMA("sp", xg.t[:, :, j * 128:(j + 1) * 128], XT[bi], [dXT[bi]], [xg])
                  DMA("sp", hg[j].t[:, :], H2[bi], [dH2[bi]], [hg[j]])
              for t0 in range(0, G, 4):
                  bk = BK[(t0 // 4) % 2]
                  bkv = bk.t[:, :].rearrange("p (a b) -> p a b", a=4)
                  for tt in range(4):
                      tg = grp * GB * 128 + t0 + tt
                      a_, b_ = At[(t0 + tt) % 4], Bt[(t0 + tt) % 4]
                      TS(a_.t[:, :], iotaf.t[:, :], E1T.t[:, tg:tg + 1], GGT.t[:, tg:tg + 1], ALU.is_equal, ALU.mult,
                         [iotaf, E1T, GGT], [a_])
                      TS(b_.t[:, :], iotaf.t[:, :], E2T.t[:, tg:tg + 1], None, ALU.is_equal, None, [iotaf, E2T], [b_])
                      MM(bkv[:, tt, :], b_.t[:, :], a_.t[:, :], True, True, [a_, b_], [bk])
                  CP("act", Wg.t[:, :, t0:t0 + 4].rearrange("p n t -> p t n"), bkv, [bk], [Wg])
              def load_w(gq):
                  DMA("sp", ust[gq % 2].t[:, :, :], UB[gq], [dUB[gq]], [ust[gq % 2]])
                  DMA("pool", vst[gq % 2].t[:, :, :], VB[gq], [dVB[gq]], [vst[gq % 2]])

              def h_mm(c):
                  gq, cc = divmod(c, 4)
                  k_ = gq % 2
                  bk = BK[c % 2]
                  for kc in range(8):
                      MM(bk.t[:, 0:G], ust[k_].t[:, kc, cc * 128:(cc + 1) * 128], xg.t[:, kc, 0:G], kc == 0, kc == 7,
                         [ust[k_], xg], [bk])
                  A(gl[c % 3].t[:, 0:G], bk.t[:, 0:G], AF.Gelu, [bk], [gl[c % 3]])
                  TT(wa[c % 3].t[:, 0:G], gl[c % 3].t[:, 0:G], Wg.t[:, c, 0:G], ALU.mult, [gl[c % 3], Wg], [wa[c % 3]])
                  return k_

              def v_mm(c, k_):
                  cc = c % 4
                  for j in range(nb_):
                      for half in range(2):
                          MM(ACC[j].t[:, half * 512:(half + 1) * 512], wa[c % 3].t[:, j * 128:(j + 1) * 128],
                             vst[k_].t[:, cc, half * 512:(half + 1) * 512], c == 0, c == 127, [wa[c % 3], vst[k_]], [ACC[j]])

              load_w(0)
              load_w(1)
              pendv = []
              for c in range(128):
                  k_ = h_mm(c)
                  pendv.append((c, k_))
                  if len(pendv) > 2:
                      v_mm(*pendv.pop(0))
                  if c % 4 == 1 and c > 4 and c // 4 + 1 < 32:
                      load_w(c // 4 + 1)
              while pendv:
                  v_mm(*pendv.pop(0))
              for j, bi in enumerate(blks):
                  TT(yo.t[:, :], ACC[j].t[:, :], hg[j].t[:, :], ALU.add, [ACC[j], hg[j]], [yo])
                  DMA("sp", y[bi], yo.t[:, :], [yo], [])
          S.barrier()
          S.flush()
    return nc


def _t5_bucket_np(n):
    n = np.maximum(n, 0)
    nf = np.maximum(n, 1).astype(np.float32)
    large = 16 + (np.log(nf / 16) / math.log(128 / 16) * 16).astype(np.int32)
    large = np.minimum(large, 31)
    return np.where(n < 16, n, large)


def _bias_index_tables():
    k = np.arange(128)[:, None]
    q = np.arange(128)[None, :]
    diag = np.where(k <= q, _t5_bucket_np(q - k), 32)
    sub = _t5_bucket_np(128 + q - k)
    far = np.full((128, 128), 31)
    allm = np.full((128, 128), 32)
    return {"diag": diag, "sub": sub, "far": far, "allm": allm}


def prepare_inputs(inputs, NOWN, seq_pad_blocks=None):
    x = np.asarray(inputs["x"], np.float32)
    Bn, Sn, _ = x.shape
    NKB = 2 * NOWN
    L = NKB * 128
    meta = np.asarray(inputs["meta_tokens"], np.float32)
    rel_bias = np.asarray(inputs["rel_bias"], np.float32)
    half = 16
    inv_freq = (10000.0 ** (-np.arange(half, dtype=np.float32) / half)).astype(np.float32)
    pos = np.arange(L, dtype=np.float32)
    ang = pos[:, None] * inv_freq[None, :]
    cs_all = np.concatenate([np.cos(ang), np.sin(ang)], axis=1).astype(np.float32)

    def g(name):
        return np.asarray(inputs[name], np.float32)[0]

    vec_parts = {
        "attn_norm": g("attn_norm"), "ffn_norm": g("ffn_norm"), "q_norm": g("mla_q_norm"), "kv_norm": g("mla_kv_norm"),
        "gq": g("mla_qk_norm_q"), "gk": g("mla_qk_norm_k"), "gdq": g("diff_q_norm"), "gdk": g("diff_k_norm"),
        "lq1": g("diff_lambda_q1"), "lk1": g("diff_lambda_k1"), "lq2": g("diff_lambda_q2"), "lk2": g("diff_lambda_k2"),
        "subln": g("diff_subln"), "b31": rel_bias[31, :],
    }
    vecs = np.concatenate([vec_parts[n] for n, _ in VEC_LAYOUT])[None, :].astype(np.float32)
    tabs = _bias_index_tables()
    ext = np.concatenate([rel_bias, np.full((1, 4), NEG, np.float32)], axis=0)
    zero_neg = np.array([0.0, NEG], np.float32)
    tri = zero_neg[(tabs["diag"] == 32).astype(np.int64)]
    allneg = zero_neg[np.ones((128, 128), np.int64)]
    zeros = zero_neg[np.zeros((128, 128), np.int64)]
    common = {
        "w_in": g("w_in").reshape(8, 128, IN_W), "w_uq": g("mla_w_uq").reshape(2, 128, 768),
        "w_ukv": g("mla_w_ukv").reshape(2, 128, 1024), "w_out": g("w_out").reshape(8, 128, 1024),
        "w_query": g("peer_w_query").reshape(8, 128, 2048),
        "skT": np.ascontiguousarray(g("peer_sub_keys").reshape(16, 128, 128).transpose(0, 2, 1)),
        "peer_uT": np.ascontiguousarray(g("peer_u").T).reshape(8, 128, 16384), "peer_v": g("peer_v"), "vecs": vecs,
    }
    in_maps = []
    for b in range(Bn):
        hfull = np.zeros((L, D), np.float32)
        hfull[:NMETA] = meta
        hfull[NMETA:NMETA + Sn] = x[b]
        hseq = hfull.reshape(NKB, 128, D)
        for par in range(2):
            own = np.arange(NOWN) * 2 + par
            if par == 0:
                types = ["sub", "diag", "allm"]
                mm = np.stack([tri, allneg])
            else:
                types = ["far", "sub", "diag"]
                mm = np.stack([zeros, tri])
            bd = np.stack([np.stack([ext[tabs[t], h] for h in range(4)]) for t in types]).astype(np.float32)
            m = dict(common)
            m.update({
                "hseq": hseq, "hown": np.ascontiguousarray(hseq[own]),
                "csk": cs_all.reshape(NKB, 128, 32), "csq": np.ascontiguousarray(cs_all.reshape(NKB, 128, 32)[own]),
                "maskm": mm.astype(np.float32), "biasd": bd,
            })
            in_maps.append(m)
    return in_maps


def assemble(results, Bn, Sn, NOWN):
    NKB = 2 * NOWN
    out = np.zeros((Bn, NKB * 128, D), np.float32)
    for b in range(Bn):
        for par in range(2):
            yv = np.asarray(results[b * 2 + par]["y"]).reshape(NOWN, 128, D)
            full = out[b].reshape(NKB, 128, D)
            full[par::2] = yv
    return np.ascontiguousarray(out[:, NMETA:NMETA + Sn])


_NC_CACHE = {}


def kernel(**inputs):
    x = np.asarray(inputs["x"])
    Bn, Sn, _ = x.shape
    nblocks = -(-(NMETA + Sn) // 128)
    NOWN = (nblocks + 1) // 2
    if NOWN not in _NC_CACHE:
        _NC_CACHE[NOWN] = build(NOWN)
    nc = _NC_CACHE[NOWN]
    in_maps = prepare_inputs(inputs, NOWN)
    res = run_bass_kernel_spmd(nc, in_maps, core_ids=list(range(len(in_maps))))
    return assemble(res.results, Bn, Sn, NOWN).astype(np.float32)
```

```python
import math
from contextlib import ExitStack

import numpy as np
import concourse.bass as bass
import concourse.mybir as mybir
from concourse.bass_utils import run_bass_kernel_spmd

F32 = mybir.dt.float32
BF16 = mybir.dt.bfloat16
U32 = mybir.dt.uint32
AF = mybir.ActivationFunctionType
ALU = mybir.AluOpType
AX = mybir.AxisListType

D = 1024
NMETA = 16
EPS = 1e-6
LAMBDA_INIT = 0.2
NEG = -30000.0
IN_W = 2080

VEC_LAYOUT = [("attn_norm", 1024), ("ffn_norm", 1024), ("q_norm", 256), ("kv_norm", 256),
              ("gq", 96), ("gk", 96), ("gdq", 64), ("gdk", 64), ("lq1", 64), ("lk1", 64),
              ("lq2", 64), ("lk2", 64), ("subln", 128), ("b31", 4)]
VOFF = {}
_o = 0
for _n, _l in VEC_LAYOUT:
    VOFF[_n] = (_o, _o + _l)
    _o += _l
NVEC = _o


class Buf:
    __slots__ = ("w", "r")

    def __init__(self):
        self.w = None
        self.r = {}


class Tile:
    __slots__ = ("t", "b")

    def __init__(self, t):
        self.t = t
        self.b = Buf()


class Sched:
    ENG = ("pe", "act", "dve", "pool", "sp")

    def __init__(self, nc, es):
        self.nc = nc
        self.es = es
        self.sems = []
        self.owner = []
        self.prog = {e: [] for e in self.ENG}
        self.cnt = {e: 0 for e in self.ENG}
        self.esem = {}
        self.seen = {e: {} for e in self.ENG}
        for e in self.ENG:
            self._new_epoch(e)
        self.defer = False
        self.pending = []
        self.dq = {}
        for e, k in (("sp", 16), ("pool", 16), ("act", 4)):
            self.dq[e] = {"sems": [self._sem(None) for _ in range(k)], "cnt": [0] * k, "n": 0}

    def _sem(self, owner):
        s = self.es.enter_context(self.nc.semaphore(f"sm{len(self.sems)}"))
        self.sems.append(s)
        self.owner.append(owner)
        return len(self.sems) - 1

    def _new_epoch(self, e):
        self.esem[e] = self._sem(e)
        self.cnt[e] = 0

    def _waits(self, e, reads, writes):
        need = {}

        def add(s, v, war=False):
            if self.owner[s] == e and e == "pe":
                return
            if need.get(s, 0) < v:
                need[s] = v

        for b in reads:
            if b.w is not None:
                add(*b.w)
        for b in writes:
            if b.w is not None:
                add(*b.w)
            for s, v in b.r.items():
                add(s, v, True)
        out = []
        seen = self.seen[e]
        for s, v in need.items():
            if seen.get(s, 0) >= v:
                continue
            seen[s] = v
            out.append((s, v))
        return out

    def _book(self, ev, reads, writes):
        s, v = ev
        for b in reads:
            if b.r.get(s, 0) < v:
                b.r[s] = v
        for b in writes:
            b.w = ev
            b.r = {}

    def op(self, e, fn, reads=(), writes=(), dur=0.3):
        reads = [x.b if isinstance(x, Tile) else x for x in reads]
        writes = [x.b if isinstance(x, Tile) else x for x in writes]
        if self.defer:
            self.pending.append(("op", e, fn, reads, writes, dur, dur))
            return
        w = self._waits(e, reads, writes)
        if self.cnt[e] >= 60000:
            self._new_epoch(e)
        self.cnt[e] += 1
        ev = (self.esem[e], self.cnt[e])
        self.prog[e].append((w, fn, ev[0], 1))
        self._book(ev, reads, writes)

    def dma(self, e, fn, reads=(), writes=(), dur=3.0):
        reads = [x.b if isinstance(x, Tile) else x for x in reads]
        writes = [x.b if isinstance(x, Tile) else x for x in writes]
        if self.defer:
            self.pending.append(("dma", e, fn, reads, writes, 1.0 if e == "pool" else 0.35, dur))
            return
        q = self.dq[e]
        k = q["n"] % len(q["sems"])
        q["n"] += 1
        s = q["sems"][k]
        w = self._waits(e, reads, writes)
        c = q["cnt"][k]
        if c > 0 and self.seen[e].get(s, 0) < c:
            self.seen[e][s] = c
            w.append((s, c))
        q["cnt"][k] = c + 16
        ev = (s, c + 16)
        self.prog[e].append((w, fn, s, 16))
        self._book(ev, reads, writes)

    def reorder(self):
        import heapq
        ops = self.pending
        self.pending = []
        self.defer = False
        n = len(ops)
        lastw, readers = {}, {}
        deps = [None] * n
        succ = [[] for _ in range(n)]
        for i, (_, e, fn, reads, writes, busy, lat) in enumerate(ops):
            d = set()
            for b in reads:
                if id(b) in lastw:
                    d.add(lastw[id(b)])
            for b in writes:
                if id(b) in lastw:
                    d.add(lastw[id(b)])
                d.update(readers.get(id(b), ()))
            d.discard(i)
            for b in reads:
                readers.setdefault(id(b), []).append(i)
            for b in writes:
                lastw[id(b)] = i
                readers[id(b)] = []
            deps[i] = len(d)
            for j in d:
                succ[j].append(i)
        ready_t = [0.0] * n
        heaps = {e: [] for e in self.ENG}
        for i in range(n):
            if deps[i] == 0:
                heapq.heappush(heaps[ops[i][1]], (0.0, i))
        free = {e: 0.0 for e in self.ENG}
        order = []
        done = 0
        while done < n:
            best = None
            for e in self.ENG:
                h = heaps[e]
                if not h:
                    continue
                rt, i = h[0]
                st = max(rt, free[e])
                if best is None or (st, i) < (best[0], best[2]):
                    best = (st, e, i)
            st, e, i = best
            h = heaps[e]
            cand = []
            while h and h[0][0] <= st:
                cand.append(heapq.heappop(h))
            cand.sort(key=lambda x: x[1])
            rt, i = cand[0]
            for c in cand[1:]:
                heapq.heappush(h, c)
            busy, lat = ops[i][5], ops[i][6]
            free[e] = st + busy
            fin = st + lat
            order.append((st, i))
            done += 1
            for j in succ[i]:
                deps[j] -= 1
                t_ = fin + (0.0 if ops[j][1] == e else 0.12)
                if t_ > ready_t[j]:
                    ready_t[j] = t_
                if deps[j] == 0:
                    heapq.heappush(heaps[ops[j][1]], (ready_t[j], j))
        order.sort()
        for _, i in order:
            kind, e, fn, reads, writes, busy, lat = ops[i]
            if kind == "op":
                self.op(e, fn, reads, writes)
            else:
                self.dma(e, fn, reads, writes)

    def barrier(self):
        if self.pending:
            self.reorder()
        evs = []
        for e in self.ENG:
            if self.cnt[e] > 0:
                evs.append((self.esem[e], self.cnt[e]))
        for q in self.dq.values():
            for s, c in zip(q["sems"], q["cnt"]):
                if c > 0:
                    evs.append((s, c))
        for e in self.ENG:
            w = []
            for s, v in evs:
                if self.owner[s] == e:
                    continue
                if self.seen[e].get(s, 0) >= v:
                    continue
                self.seen[e][s] = v
                w.append((s, v))
            if w:
                self.prog[e].append((w, None, None, 0))

    def _run(self, lst, eng):
        for w, fn, s, inc in lst:
            for sw, v in w:
                eng.wait_ge(self.sems[sw], v)
            if fn is not None:
                fn(eng).then_inc(self.sems[s], inc)

    def flush(self):
        progs = self.prog
        self.prog = {e: [] for e in self.ENG}
        with self.nc.Block() as block:
            @block.tensor
            def _(eng):
                self._run(progs["pe"], eng)

            @block.scalar
            def _(eng):
                self._run(progs["act"], eng)

            @block.vector
            def _(eng):
                self._run(progs["dve"], eng)

            @block.gpsimd
            def _(eng):
                self._run(progs["pool"], eng)

            @block.sync
            def _(eng):
                self._run(progs["sp"], eng)


def bc(ap, shape):
    return ap.to_broadcast(list(shape))


def build(NOWN, debug=False, phases="ABC", skip_c2=False):
    NKB = 2 * NOWN
    nc = bass.Bass("TRN2", target_bir_lowering=False)

    def din(name, shape, dt=F32):
        return nc.dram_tensor(name, list(shape), dt, kind="ExternalInput")

    skind = "ExternalOutput" if debug else "Internal"

    def dsc(name, shape, dt):
        return nc.dram_tensor(name, list(shape), dt, kind=skind)

    hseq = din("hseq", [NKB, 128, D])
    hown = din("hown", [NOWN, 128, D])
    csk = din("csk", [NKB, 128, 32])
    csq = din("csq", [NOWN, 128, 32])
    w_in = din("w_in", [8, 128, IN_W])
    w_uq = din("w_uq", [2, 128, 768])
    w_ukv = din("w_ukv", [2, 128, 1024])
    w_out = din("w_out", [8, 128, 1024])
    w_query = din("w_query", [8, 128, 2048])
    skT = din("skT", [16, 128, 128])
    uT_d = din("peer_uT", [8, 128, 16384])
    v_d = din("peer_v", [16384, D])
    UB = dsc("UB", [32, 128, 8, 512], BF16)
    VB = dsc("VB", [32, 128, 4, 1024], BF16)
    XT = dsc("XT", [NOWN, 128, 8, 128], BF16)
    dUB, dVB = ([Buf() for _ in range(32)] for _ in range(2))
    dXT = [Buf() for _ in range(NOWN)]
    vecs = din("vecs", [1, NVEC])
    maskm = din("maskm", [2, 128, 128])
    biasd = din("biasd", [3, 4, 128, 128])
    y = nc.dram_tensor("y", [NOWN, 128, D], F32, kind="ExternalOutput")

    KVW = 1024 + 520 + 512 + 516
    KVD = dsc("KV", [NKB, 128, KVW], BF16)

    class _Sub:
        def __init__(self, c0, shape, p=128):
            self.c0, self.shape, self.p = c0, shape, p

        def __getitem__(self, blk):
            a, b = self.shape
            return KVD[blk, 0:self.p, self.c0:self.c0 + a * b].rearrange("p (a b) -> p a b", a=a)

    KTm, Vm, KTd, Vd = _Sub(0, (8, 128), 96), _Sub(1024, (8, 65)), _Sub(1544, (4, 128)), _Sub(2056, (4, 129))
    QTm = dsc("QTm", [NOWN, 96, 8, 128], BF16)
    QTd = dsc("QTd", [NOWN, 128, 4, 128], BF16)
    H2 = dsc("H2", [NOWN, 128, D], F32)
    dKTm, dVm, dKTd, dVd = ([Buf() for _ in range(NKB)] for _ in range(4))
    dQTm, dQTd, dH2 = ([Buf() for _ in range(NOWN)] for _ in range(3))

    with ExitStack() as ges:
        S = Sched(nc, ges)
        ncnt = [0]

        def sb(es, shape, dt=F32):
            ncnt[0] += 1
            return Tile(es.enter_context(nc.sbuf_tensor(f"t{ncnt[0]}", list(shape), dt)))

        def ps(es, shape, dt=F32):
            ncnt[0] += 1
            per_bank = 512 if dt == F32 else 1024
            n = int(np.prod(shape[1:]))
            nb_ = -(-n // per_bank)
            t = es.enter_context(nc.psum_tensor(f"p{ncnt[0]}", [128, nb_ * per_bank], dt))
            v = t[:, 0:n]
            if len(shape) == 3:
                v = v.rearrange("p (a b) -> p a b", a=shape[1])
            return Tile(v)

        def phase(name):
            if name in phases:
                with ExitStack() as pes:
                    yield pes

        def nel(ap):
            n = 1
            for s_ in ap.shape[1:]:
                n *= int(s_)
            return n

        def act(fn, r, w, dur=0.3):
            S.op("act", fn, r, w, dur)

        def dve(fn, r, w, dur=0.3):
            S.op("dve", fn, r, w, dur)

        def pe(fn, r, w, dur=0.15):
            S.op("pe", fn, r, w, dur)

        def A(out, in_, func, r, w, **kw):
            act(lambda e: e.activation(out=out, in_=in_, func=func, **kw), r, w, 0.25 + nel(out) * 0.00075)

        def TT(out, in0, in1, op, r, w, eng="dve"):
            S.op(eng, lambda e: e.tensor_tensor(out=out, in0=in0, in1=in1, op=op), r, w, 0.12 + nel(out) * 0.0016)

        def TS(out, in0, s1, s2, op0, op1, r, w, eng="dve"):
            if op1 is None:
                S.op(eng, lambda e: e.tensor_scalar(out=out, in0=in0, scalar1=s1, scalar2=None, op0=op0), r, w,
                     0.12 + nel(out) * 0.001)
            else:
                S.op(eng, lambda e: e.tensor_scalar(out=out, in0=in0, scalar1=s1, scalar2=s2, op0=op0, op1=op1), r, w,
                     0.12 + nel(out) * 0.001)

        def STT(out, in0, sc, in1, op0, op1, r, w, accum=None):
            if accum is None:
                dve(lambda e: e.scalar_tensor_tensor(out=out, in0=in0, scalar=sc, in1=in1, op0=op0, op1=op1), r, w,
                    0.12 + nel(out) * 0.0016)
            else:
                dve(lambda e: e.scalar_tensor_tensor(out=out, in0=in0, scalar=sc, in1=in1, op0=op0, op1=op1,
                                                     accum_out=accum), r, w, 0.25 + nel(out) * 0.0016)

        def RED(out, in_, r, w):
            dve(lambda e: e.tensor_reduce(out=out, in_=in_, axis=AX.X, op=ALU.add), r, w, 0.12 + nel(in_) * 0.00105)

        def CP(eng, out, in_, r, w):
            if eng == "act":
                act(lambda e: e.copy(out=out, in_=in_), r, w, 0.25 + nel(out) * 0.00075)
            else:
                S.op(eng, lambda e: e.tensor_copy(out=out, in_=in_), r, w,
                     (0.12 + nel(out) * 0.00105) if eng == "dve" else (0.3 + nel(out) * 0.0006))

        def MM(out, lhsT, rhs, start, stop, r, w):
            pe(lambda e: e.matmul(out, lhsT, rhs, start=start, stop=stop), r, w, 0.1 + nel(rhs) * 0.00052)

        def TR(out, in_, ident, r, w):
            pe(lambda e: e.transpose(out, in_, ident), r, w, 0.2)

        def DMA(eng, out, in_, r, w):
            S.dma(eng, lambda e: e.dma_start(out=out, in_=in_), r, w, 2.5 + nel(out) * 128 * 4 / 1.0e5)

        vec = sb(ges, [128, NVEC])
        ident = sb(ges, [128, 128])
        identb = sb(ges, [128, 128], BF16)
        epsc = sb(ges, [128, 1])
        gqs = sb(ges, [128, 96])
        gdqs = sb(ges, [128, 64])
        subl8 = sb(ges, [128, 128])
        neglam = sb(ges, [128, 1])
        ltmp = sb(ges, [128, 64])
        lsc = sb(ges, [128, 4])

        def V(name):
            a, b = VOFF[name]
            return vec.t[:, a:b]

        DMA("sp", vec.t[:, :], vecs[0:1, :].to_broadcast([128, NVEC]), [], [vec])
        S.op("pool", lambda e: e.memset(ident.t[:, :], 0.0), [], [ident])
        S.op("pool", lambda e: e.affine_select(out=ident.t[:, :], in_=ident.t[:, :], pattern=[[-1, 128]],
                                               compare_op=ALU.not_equal, fill=1.0, base=0, channel_multiplier=1),
             [ident], [ident])
        CP("dve", identb.t[:, :], ident.t[:, :], [ident], [identb])
        S.op("pool", lambda e: e.memset(epsc.t[:, :], EPS), [], [epsc])
        TS(gqs.t[:, :], V("gq"), 96.0 ** -0.5, None, ALU.mult, None, [vec], [gqs])
        TS(gdqs.t[:, :], V("gdq"), 0.125, None, ALU.mult, None, [vec], [gdqs])
        TS(subl8.t[:, :], V("subln"), 1.0 - LAMBDA_INIT, None, ALU.mult, None, [vec], [subl8])
        STT(ltmp.t[:, :], V("lq1"), 1.0, V("lk1"), ALU.mult, ALU.mult, [vec], [ltmp, lsc], accum=lsc.t[:, 0:1])
        STT(ltmp.t[:, :], V("lq2"), 1.0, V("lk2"), ALU.mult, ALU.mult, [vec], [ltmp, lsc], accum=lsc.t[:, 1:2])
        A(lsc.t[:, 2:4], lsc.t[:, 0:2], AF.Exp, [lsc], [lsc])
        TT(neglam.t[:, :], lsc.t[:, 3:4], lsc.t[:, 2:3], ALU.subtract, [lsc], [neglam])
        TS(neglam.t[:, :], neglam.t[:, :], -LAMBDA_INIT, None, ALU.add, None, [neglam], [neglam])

        def load_w_bf16(es, dram, nchunk, ncol, stage_pool):
            wt = sb(es, [128, nchunk, ncol], BF16)
            for c in range(nchunk):
                st = stage_pool[c % len(stage_pool)]
                DMA("sp", st.t[:, 0:ncol], dram[c], [], [st])
                if c % 2 == 0:
                    CP("dve", wt.t[:, c, :], st.t[:, 0:ncol], [st], [wt])
                else:
                    CP("pool", wt.t[:, c, :], st.t[:, 0:ncol], [st], [wt])
            return wt

        for es in phase("A"):
            S.defer = True
            stage = [sb(es, [128, IN_W]) for _ in range(2)]
            w_in_b = load_w_bf16(es, w_in, 8, IN_W, stage)
            w_uq_b = load_w_bf16(es, w_uq, 2, 768, stage)
            w_ukv_b = load_w_bf16(es, w_ukv, 2, 1024, stage)

            NB_ = 2
            hb = [sb(es, [128, D]) for _ in range(NB_)]
            cs = [sb(es, [128, 32]) for _ in range(NB_)]
            junkA = [sb(es, [128, D]) for _ in range(NB_)]
            st4 = [sb(es, [128, 8]) for _ in range(NB_)]
            nb = [sb(es, [128, D], BF16) for _ in range(NB_)]
            nT = [sb(es, [128, 8, 128], BF16) for _ in range(NB_)]
            cn = [sb(es, [128, 256], BF16) for _ in range(NB_)]
            cT = [sb(es, [128, 2, 128], BF16) for _ in range(NB_)]
            kvs = [sb(es, [128, 1024]) for _ in range(NB_)]
            dks = [sb(es, [128, 512]) for _ in range(NB_)]
            pas = [sb(es, [128, 288]) for _ in range(NB_)]
            sqA = [sb(es, [128, 1024]) for _ in range(NB_)]
            s8 = [sb(es, [128, 32]) for _ in range(NB_)]
            krg = [sb(es, [128, 8, 32]) for _ in range(NB_)]
            rr = [sb(es, [128, 8, 32]) for _ in range(NB_)]
            kk = [sb(es, [128, 8, 96], BF16) for _ in range(NB_)]
            kd = [sb(es, [128, 512], BF16) for _ in range(NB_)]
            tmp8A = [sb(es, [128, 8, 64]) for _ in range(NB_)]
            ktm = [sb(es, [128, 8, 128], BF16) for _ in range(NB_)]
            ktd = [sb(es, [128, 4, 128], BF16) for _ in range(NB_)]
            vv = [sb(es, [128, 8, 65], BF16) for _ in range(NB_)]
            vd = [sb(es, [128, 4, 129], BF16) for _ in range(NB_)]
            for t in ktm:
                S.op("pool", lambda e, t=t: e.memset(t.t[:, :, :], 0.0), [], [t])
            for t in vv:
                S.op("pool", lambda e, t=t: e.memset(t.t[:, :, 64:65], 1.0), [], [t])
            for t in vd:
                S.op("pool", lambda e, t=t: e.memset(t.t[:, :, 128:129], 1.0), [], [t])

            TP = ps(es, [128, 8, 128], BF16)
            TK = ps(es, [128, 16, 128], BF16)
            PA = ps(es, [128, 512])
            PB = ps(es, [128, 512])
            PC = ps(es, [128, 512])
            PD = ps(es, [128, 1024])

            def rstd_of(out, ss, dim, rbufs, wbuf, tmp):
                A(tmp, ss, AF.Ln, rbufs + [epsc], [wbuf], scale=1.0 / dim, bias=epsc.t[:, 0:1])
                A(out, tmp, AF.Exp, [wbuf], [wbuf], scale=-0.5)

            def p1(kside, blk, it):
                k = it % NB_
                junk, sq, tmp8 = junkA[k], sqA[k], tmp8A[k]
                H, C = hb[k], cs[k]
                src = hseq[blk] if kside else hown[blk]
                DMA("sp", H.t[:, :], src, [], [H])
                DMA("sp", C.t[:, :], (csk if kside else csq)[blk], [], [C])
                st = st4[k]
                A(junk.t[:, :], H.t[:, :], AF.Square, [H], [junk, st], accum_out=st.t[:, 0:1])
                rstd_of(st.t[:, 2:3], st.t[:, 0:1], D, [st], st, st.t[:, 1:2])
                STT(nb[k].t[:, :], H.t[:, :], st.t[:, 2:3], V("attn_norm"), ALU.mult, ALU.mult, [H, st, vec], [nb[k]])
                for c in range(8):
                    TR(TP.t[:, c, :], nb[k].t[:, c * 128:(c + 1) * 128], identb.t[:, :], [nb[k], identb], [TP])
                CP("act", nT[k].t[:, :, :], TP.t[:, :, :], [TP], [nT[k]])
                if kside:
                    groups = [(PA, 288, 256), (PB, 512, 1056), (PC, 512, 1568)]
                else:
                    groups = [(PA, 256, 0), (PB, 512, 544)]
                for (pt, n, c0) in groups:
                    for c in range(8):
                        MM(pt.t[:, 0:n], nT[k].t[:, c, :], w_in_b.t[:, c, c0:c0 + n], c == 0, c == 7,
                           [nT[k], w_in_b], [pt])
                npa = 288 if kside else 256
                CP("act", pas[k].t[:, 0:npa], PA.t[:, 0:npa], [PA], [pas[k]])
                CP("act", dks[k].t[:, :], PB.t[:, :], [PB], [dks[k]])
                if kside:
                    CP("dve", vd[k].t[:, :, 0:128], PC.t[:, :].rearrange("p (h e) -> p h e", h=4), [PC], [vd[k]])
                    DMA("sp", Vd[blk], vd[k].t[:, :, :], [vd[k]], [dVd[blk]])

            def p2(kside, blk, it):
                k = it % NB_
                junk, sq, tmp8 = junkA[k], sqA[k], tmp8A[k]
                C = cs[k]
                st = st4[k]
                gname = "kv_norm" if kside else "q_norm"
                A(junk.t[:, 0:256], pas[k].t[:, 0:256], AF.Square, [pas[k]], [junk, st], accum_out=st.t[:, 3:4])
                rstd_of(st.t[:, 5:6], st.t[:, 3:4], 256, [st], st, st.t[:, 4:5])
                STT(cn[k].t[:, :], pas[k].t[:, 0:256], st.t[:, 5:6], V(gname), ALU.mult, ALU.mult, [pas[k], st, vec], [cn[k]])
                for c in range(2):
                    TR(TK.t[:, 12 + c, :], cn[k].t[:, c * 128:(c + 1) * 128], identb.t[:, :], [cn[k], identb], [TK])
                CP("act", cT[k].t[:, :, :], TK.t[:, 12:14, :], [TK], [cT[k]])
                wup = w_ukv_b if kside else w_uq_b
                nup = 1024 if kside else 768
                for h0 in range(0, nup, 512):
                    n = min(512, nup - h0)
                    for c in range(2):
                        MM(PD.t[:, h0:h0 + n], cT[k].t[:, c, :], wup.t[:, c, h0:h0 + n], c == 0, c == 1,
                           [cT[k], wup], [PD])
                KV = kvs[k]
                CP("act", KV.t[:, 0:nup], PD.t[:, 0:nup], [PD], [KV])
                s = s8[k]
                if kside:
                    kv3 = KV.t[:, :].rearrange("p (h e) -> p h e", h=8)
                    TT(sq.t[:, 0:512].rearrange("p (h e) -> p h e", h=8), kv3[:, :, 0:64], kv3[:, :, 0:64], ALU.mult,
                       [KV], [sq])
                    RED(s.t[:, 0:8], sq.t[:, 0:512].rearrange("p (h e) -> p h e", h=8), [sq], [s])
                    CP("act", krg[k].t[:, 0, :], pas[k].t[:, 256:288], [pas[k]], [krg[k]])
                    A(junk.t[:, 0:32], krg[k].t[:, 0, :], AF.Square, [krg[k]], [junk, st], accum_out=st.t[:, 6:7])
                    TS(s.t[:, 0:8], s.t[:, 0:8], st.t[:, 6:7], None, ALU.add, None, [s, st], [s])
                    rstd_of(s.t[:, 16:24], s.t[:, 0:8], 96, [s], s, s.t[:, 8:16])
                    K3 = kk[k].t
                    TT(tmp8.t[:, :, :], kv3[:, :, 0:64], bc(s.t[:, 16:24].unsqueeze(2), [128, 8, 64]), ALU.mult,
                       [KV, s], [tmp8])
                    TT(K3[:, :, 0:64], tmp8.t[:, :, :], bc(V("gk")[:, 0:64].unsqueeze(1), [128, 8, 64]), ALU.mult,
                       [tmp8, vec], [kk[k]])
                    t0 = krg[k].t[:, 1, :]
                    TT(t0, krg[k].t[:, 0, :], V("gk")[:, 64:96], ALU.mult, [krg[k], vec], [krg[k]])
                    co, si = C.t[:, 0:16], C.t[:, 16:32]
                    r1, r2 = rr[k].t[:, 0, 0:16], rr[k].t[:, 0, 16:32]
                    a1, a2 = rr[k].t[:, 1, 0:16], rr[k].t[:, 1, 16:32]
                    TT(a1, t0[:, 0:16], co, ALU.mult, [krg[k], C], [rr[k]])
                    TT(a2, t0[:, 16:32], si, ALU.mult, [krg[k], C], [rr[k]])
                    TT(r1, a1, a2, ALU.subtract, [rr[k]], [rr[k]])
                    TT(a1, t0[:, 0:16], si, ALU.mult, [krg[k], C, rr[k]], [rr[k]])
                    TT(a2, t0[:, 16:32], co, ALU.mult, [krg[k], C, rr[k]], [rr[k]])
                    TT(r2, a1, a2, ALU.add, [rr[k]], [rr[k]])
                    TT(K3[:, :, 64:96], bc(rr[k].t[:, 0:1, :], [128, 8, 32]), bc(s.t[:, 16:24].unsqueeze(2), [128, 8, 32]),
                       ALU.mult, [rr[k], s], [kk[k]])
                    for h in range(8):
                        TR(TK.t[0:96, h, :], K3[:, h, :], identb.t[:, :], [kk[k], identb], [TK])
                    CP("act", ktm[k].t[0:96, :, :], TK.t[0:96, 0:8, :], [TK], [ktm[k]])
                    DMA("sp", KVD[blk, :, 0:1024], ktm[k].t[:, :, :].rearrange("p a b -> p (a b)"), [ktm[k]], [dKTm[blk]])
                    CP("pool", vv[k].t[:, :, 0:64], kv3[:, :, 64:128], [KV], [vv[k]])
                    DMA("sp", Vm[blk], vv[k].t[:, :, :], [vv[k]], [dVm[blk]])
                    d3 = dks[k].t[:, :].rearrange("p (g e) -> p g e", g=8)
                    TT(sq.t[:, 512:1024].rearrange("p (g e) -> p g e", g=8), d3, d3, ALU.mult, [dks[k]], [sq])
                    RED(s.t[:, 24:32], sq.t[:, 512:1024].rearrange("p (g e) -> p g e", g=8), [sq], [s])
                    rstd_of(s.t[:, 24:32], s.t[:, 24:32], 64, [s], s, s.t[:, 8:16])
                    TT(tmp8.t[:, :, :], d3, bc(s.t[:, 24:32].unsqueeze(2), [128, 8, 64]), ALU.mult, [dks[k], s], [tmp8])
                    TT(kd[k].t[:, :].rearrange("p (g e) -> p g e", g=8), tmp8.t[:, :, :],
                       bc(V("gdk").unsqueeze(1), [128, 8, 64]), ALU.mult, [tmp8, vec], [kd[k]])
                    for h in range(4):
                        TR(TK.t[:, 8 + h, :], kd[k].t[:, h * 128:(h + 1) * 128], identb.t[:, :], [kd[k], identb], [TK])
                    CP("act", ktd[k].t[:, :, :], TK.t[:, 8:12, :], [TK], [ktd[k]])
                    DMA("sp", KTd[blk], ktd[k].t[:, :, :], [ktd[k]], [dKTd[blk]])
                else:
                    q3 = KV.t[:, 0:768].rearrange("p (h e) -> p h e", h=8)
                    TT(sq.t[:, 0:768].rearrange("p (h e) -> p h e", h=8), q3, q3, ALU.mult, [KV], [sq])
                    RED(s.t[:, 0:8], sq.t[:, 0:768].rearrange("p (h e) -> p h e", h=8), [sq], [s])
                    rstd_of(s.t[:, 16:24], s.t[:, 0:8], 96, [s], s, s.t[:, 8:16])
                    Q3 = kk[k].t
                    TT(tmp8.t[:, :, :], q3[:, :, 0:64], bc(s.t[:, 16:24].unsqueeze(2), [128, 8, 64]), ALU.mult,
                       [KV, s], [tmp8])
                    TT(Q3[:, :, 0:64], tmp8.t[:, :, :], bc(gqs.t[:, 0:64].unsqueeze(1), [128, 8, 64]), ALU.mult,
                       [tmp8, gqs], [kk[k]])
                    tq = krg[k].t
                    TT(tq[:, :, :], q3[:, :, 64:96], bc(s.t[:, 16:24].unsqueeze(2), [128, 8, 32]), ALU.mult,
                       [KV, s], [krg[k]])
                    TT(tq[:, :, :], tq[:, :, :], bc(gqs.t[:, 64:96].unsqueeze(1), [128, 8, 32]), ALU.mult,
                       [krg[k], gqs], [krg[k]])
                    co = bc(C.t[:, 0:16].unsqueeze(1), [128, 8, 16])
                    si = bc(C.t[:, 16:32].unsqueeze(1), [128, 8, 16])
                    a1, a2 = rr[k].t[:, :, 0:16], rr[k].t[:, :, 16:32]
                    TT(a1, tq[:, :, 0:16], co, ALU.mult, [krg[k], C], [rr[k]])
                    TT(a2, tq[:, :, 16:32], si, ALU.mult, [krg[k], C], [rr[k]])
                    TT(Q3[:, :, 64:80], a1, a2, ALU.subtract, [rr[k]], [kk[k]])
                    TT(a1, tq[:, :, 0:16], si, ALU.mult, [krg[k], C, kk[k]], [rr[k]])
                    TT(a2, tq[:, :, 16:32], co, ALU.mult, [krg[k], C, kk[k]], [rr[k]])
                    TT(Q3[:, :, 80:96], a1, a2, ALU.add, [rr[k]], [kk[k]])
                    for h in range(8):
                        TR(TK.t[0:96, h, :], Q3[:, h, :], identb.t[:, :], [kk[k], identb], [TK])
                    CP("act", ktm[k].t[0:96, :, :], TK.t[0:96, 0:8, :], [TK], [ktm[k]])
                    DMA("sp", QTm[blk], ktm[k].t[0:96, :, :], [ktm[k]], [dQTm[blk]])
                    d3 = dks[k].t[:, :].rearrange("p (g e) -> p g e", g=8)
                    TT(sq.t[:, 512:1024].rearrange("p (g e) -> p g e", g=8), d3, d3, ALU.mult, [dks[k]], [sq])
                    RED(s.t[:, 24:32], sq.t[:, 512:1024].rearrange("p (g e) -> p g e", g=8), [sq], [s])
                    rstd_of(s.t[:, 24:32], s.t[:, 24:32], 64, [s], s, s.t[:, 8:16])
                    TT(tmp8.t[:, :, :], d3, bc(s.t[:, 24:32].unsqueeze(2), [128, 8, 64]), ALU.mult, [dks[k], s], [tmp8])
                    TT(kd[k].t[:, :].rearrange("p (g e) -> p g e", g=8), tmp8.t[:, :, :],
                       bc(gdqs.t[:, :].unsqueeze(1), [128, 8, 64]), ALU.mult, [tmp8, gdqs], [kd[k]])
                    for h in range(4):
                        TR(TK.t[:, 8 + h, :], kd[k].t[:, h * 128:(h + 1) * 128], identb.t[:, :], [kd[k], identb], [TK])
                    CP("act", ktd[k].t[:, :, :], TK.t[:, 8:12, :], [TK], [ktd[k]])
                    DMA("sp", QTd[blk], ktd[k].t[:, :, :], [ktd[k]], [dQTd[blk]])

            seq = [(True, blk) for blk in range(NKB)] + [(False, blk) for blk in range(NOWN)]
            p1(seq[0][0], seq[0][1], 0)
            for n in range(len(seq)):
                if n + 1 < len(seq):
                    p1(seq[n + 1][0], seq[n + 1][1], n + 1)
                p2(seq[n][0], seq[n][1], n)
            S.barrier()
            S.flush()

        for es in phase("B"):
            S.defer = True
            stage = [sb(es, [128, 1024]) for _ in range(2)]
            w_out_b = load_w_bf16(es, w_out, 8, 1024, stage)
            mk = sb(es, [128, 2, 128])
            bd = sb(es, [128, 12, 128])
            b31c = V("b31")
            for t in range(2):
                DMA("sp", mk.t[:, t, :], maskm[t], [], [mk])
            for t in range(3):
                for h in range(4):
                    DMA("sp", bd.t[:, t * 4 + h, :], biasd[t, h], [], [bd])
            for t in range(3):
                for h in range(4):
                    TS(bd.t[:, t * 4 + h, :], bd.t[:, t * 4 + h, :], b31c[:, h:h + 1], None, ALU.subtract, None,
                       [bd, vec], [bd])
            mkb = sb(es, [128, 2, 128], BF16)
            bdh = sb(es, [128, 12, 128], BF16)
            bdl = sb(es, [128, 12, 128], BF16)
            bdr = sb(es, [128, 12, 128])
            CP("dve", mkb.t[:, :, :], mk.t[:, :, :], [mk], [mkb])
            CP("dve", bdh.t[:, :, :], bd.t[:, :, :], [bd], [bdh])
            TT(bdr.t[:, :, :], bd.t[:, :, :], bdh.t[:, :, :], ALU.subtract, [bd, bdh], [bdr])
            CP("dve", bdl.t[:, :, :], bdr.t[:, :, :], [bdr], [bdl])
            NKV = 6
            kvt = [sb(es, [128, KVW], BF16) for _ in range(NKV)]
            qtm = [sb(es, [128, 8, 128], BF16) for _ in range(2)]
            qtd = [sb(es, [128, 4, 128], BF16) for _ in range(2)]
            ptm = [sb(es, [128, 4, 128], BF16) for _ in range(4)]
            ptd = [sb(es, [128, 4, 128], BF16) for _ in range(4)]
            hb = [sb(es, [128, D]) for _ in range(2)]
            mixb = sb(es, [128, D], BF16)
            mixT = sb(es, [128, 8, 128], BF16)
            h2 = [sb(es, [128, D]) for _ in range(2)]
            rec = sb(es, [128, 16])
            od = sb(es, [128, 8, 128])
            odd = sb(es, [128, 4, 128])
            sq = sb(es, [128, 4, 128])
            s4 = sb(es, [128, 12])

            AM = [ps(es, [128, 4, 65]) for _ in range(2)]
            AD = [ps(es, [128, 3, 129]), ps(es, [128, 3, 129]), ps(es, [128, 2, 129])]
            SS = [ps(es, [128, 4, 128]) for _ in range(3)]
            sidx = [0]

            def next_s():
                t = SS[sidx[0] % 3]
                sidx[0] += 1
                return t

            if "C" in phases:
                cst = [sb(es, [128, 4096]) for _ in range(2)]
                cbf = [sb(es, [128, 4096], BF16) for _ in range(2)]
                for gq in range(32):
                    for which in range(2):
                        j = (gq * 2 + which) % 2
                        if which == 0:
                            src_ap = uT_d[:, :, gq * 512:(gq + 1) * 512].rearrange("k d e -> d k e")
                            dst_ap, dbuf = UB[gq].rearrange("d k e -> d (k e)"), dUB[gq]
                            stv = cst[j].t[:, :].rearrange("p (k e) -> p k e", k=8)
                        else:
                            src_ap = v_d[gq * 512:(gq + 1) * 512, :].rearrange("(c e) d -> e c d", c=4)
                            dst_ap, dbuf = VB[gq].rearrange("e c d -> e (c d)"), dVB[gq]
                            stv = cst[j].t[:, :].rearrange("p (c d) -> p c d", c=4)
                        DMA("pool", stv, src_ap, [], [cst[j]])
                        CP("pool", cbf[j].t[:, :], cst[j].t[:, :], [cst[j]], [cbf[j]])
                        DMA("pool", dst_ap, cbf[j].t[:, :], [cbf[j]], [dbuf])
            pcount = [0]
            kvit = 0
            for i in range(NOWN):
                Qm, Qd = qtm[i % 2], qtd[i % 2]
                DMA("sp", Qm.t[0:96, :, :], QTm[i], [dQTm[i]], [Qm])
                DMA("sp", Qd.t[:, :, :], QTd[i], [dQTd[i]], [Qd])
                H = hb[i % 2]
                DMA("sp", H.t[:, :], hown[i], [], [H])
                last = 2 * i + 1
                pend = []
                for kb in range(0, last + 1):
                    kq = kvit % NKV
                    kvit += 1
                    kvb = kvt[kq]
                    DMA("sp", kvb.t[:, :], KVD[kb], [dKTm[kb], dVm[kb], dKTd[kb], dVd[kb]], [kvb])

                    def _v(c0, a, b, kvb=kvb):
                        t_ = Tile(kvb.t[:, c0:c0 + a * b].rearrange("p (a b) -> p a b", a=a))
                        t_.b = kvb.b
                        return t_

                    Km, Vmt, Kd, Vdt = _v(0, 8, 128), _v(1024, 8, 65), _v(1544, 4, 128), _v(2056, 4, 129)
                    t = kb - 2 * i
                    first, fin = (kb == 0), (kb == last)
                    def mk_mla(half, t=t, first=first, fin=fin, Km=Km, Vmt=Vmt):
                        box = {}

                        def fS():
                            St = next_s()
                            for hh in range(4):
                                h = half * 4 + hh
                                sp_ = t in (0, 1)
                                MM(St.t[:, hh, :], Km.t[0:96, h, :], Qm.t[0:96, h, :], True, not sp_, [Km, Qm], [St])
                                if sp_:
                                    MM(St.t[:, hh, :], identb.t[:, :], mkb.t[:, t, :], False, True, [identb, mkb], [St])
                            P = ptm[pcount[0] % 4]
                            pcount[0] += 1
                            A(P.t[:, :, :], St.t[:, :, :], AF.Exp, [St], [P])
                            box["P"] = P

                        def fPV():
                            P = box["P"]
                            for hh in range(4):
                                h = half * 4 + hh
                                MM(AM[half].t[:, hh, :], P.t[:, hh, :], Vmt.t[:, h, :], first and hh == 0, fin and hh == 3,
                                   [P, Vmt], [AM[half]])
                        return fS, fPV

                    def mk_diff(t=t, first=first, fin=fin, Kd=Kd, Vdt=Vdt):
                        box = {}

                        def fS():
                            sp_ = t in (-1, 0, 1)
                            Sm = [next_s(), next_s()]
                            for h in range(4):
                                for m in range(2):
                                    MM(Sm[m].t[:, h, :], Kd.t[m * 64:(m + 1) * 64, h, :], Qd.t[m * 64:(m + 1) * 64, h, :],
                                       True, not sp_, [Kd, Qd], [Sm[m]])
                                    if sp_:
                                        MM(Sm[m].t[:, h, :], identb.t[:, :], bdh.t[:, (t + 1) * 4 + h, :], False, False,
                                           [identb, bdh], [Sm[m]])
                                        MM(Sm[m].t[:, h, :], identb.t[:, :], bdl.t[:, (t + 1) * 4 + h, :], False, True,
                                           [identb, bdl], [Sm[m]])
                            Pm = []
                            for m in range(2):
                                P = ptd[pcount[0] % 4]
                                pcount[0] += 1
                                A(P.t[:, :, :], Sm[m].t[:, :, :], AF.Exp, [Sm[m]], [P])
                                Pm.append(P)
                            box["Pm"] = Pm

                        def fPV():
                            Pm = box["Pm"]
                            for h in range(4):
                                for m in range(2):
                                    g = h * 2 + m
                                    MM(AD[g // 3].t[:, g % 3, :], Pm[m].t[:, h, :], Vdt.t[:, h, :], first and g in (0, 3, 6),
                                       fin and g in (2, 5, 7), [Pm[m], Vdt], [AD[g // 3]])
                        return fS, fPV

                    for stg in (mk_mla(0), mk_mla(1), mk_diff()):
                        stg[0]()
                        if pend:
                            pend.pop(0)()
                        pend.append(stg[1])
                while pend:
                    pend.pop(0)()
                for half in range(2):
                    dve(lambda e, half=half: e.reciprocal(out=rec.t[:, half * 4:half * 4 + 4],
                                                          in_=AM[half].t[:, :, 64]), [AM[half]], [rec])
                    TT(mixb.t[:, half * 256:(half + 1) * 256].rearrange("p (h e) -> p h e", h=4), AM[half].t[:, :, 0:64],
                       bc(rec.t[:, half * 4:half * 4 + 4].unsqueeze(2), [128, 4, 64]), ALU.mult, [AM[half], rec], [mixb])
                for a, n0, n in ((0, 0, 3), (1, 3, 3), (2, 6, 2)):
                    dve(lambda e, a=a, n0=n0, n=n: e.reciprocal(out=rec.t[:, 8 + n0:8 + n0 + n], in_=AD[a].t[:, :, 128]),
                        [AD[a]], [rec])
                    TT(od.t[:, n0:n0 + n, :], AD[a].t[:, :, 0:128], bc(rec.t[:, 8 + n0:8 + n0 + n].unsqueeze(2), [128, n, 128]),
                       ALU.mult, [AD[a], rec], [od])
                o4 = od.t[:, :, :].rearrange("p (h m) e -> p h m e", m=2)
                STT(odd.t[:, :, :], o4[:, :, 1, :], neglam.t[:, 0:1], o4[:, :, 0, :], ALU.mult, ALU.add, [od, neglam], [odd])
                TT(sq.t[:, :, :], odd.t[:, :, :], odd.t[:, :, :], ALU.mult, [odd], [sq])
                RED(s4.t[:, 0:4], sq.t[:, :, :], [sq], [s4])
                A(s4.t[:, 4:8], s4.t[:, 0:4], AF.Ln, [s4, epsc], [s4], scale=1.0 / 128, bias=epsc.t[:, 0:1])
                A(s4.t[:, 8:12], s4.t[:, 4:8], AF.Exp, [s4], [s4], scale=-0.5)
                TT(sq.t[:, :, :], odd.t[:, :, :], bc(s4.t[:, 8:12].unsqueeze(2), [128, 4, 128]), ALU.mult, [odd, s4], [sq])
                TT(mixb.t[:, 512:1024].rearrange("p (h e) -> p h e", h=4), sq.t[:, :, :],
                   bc(subl8.t[:, :].unsqueeze(1), [128, 4, 128]), ALU.mult, [sq, subl8], [mixb])
                Ta = next_s()
                Tv = Ta.t[:, :, :].rearrange("p a b -> p (a b)").bitcast(BF16).rearrange("p (c q) -> p c q", c=8)
                for c in range(8):
                    TR(Tv[:, c, :], mixb.t[:, c * 128:(c + 1) * 128], identb.t[:, :], [mixb, identb], [Ta])
                CP("act", mixT.t[:, :, :], Tv, [Ta], [mixT])
                Hh = h2[i % 2]
                for half in range(2):
                    Po = next_s()
                    Pf = Po.t[:, :, :].rearrange("p a b -> p (a b)")
                    for c in range(8):
                        MM(Pf, mixT.t[:, c, :], w_out_b.t[:, c, half * 512:(half + 1) * 512], c == 0, c == 7,
                           [mixT, w_out_b], [Po])
                    TT(Hh.t[:, half * 512:(half + 1) * 512], Pf, H.t[:, half * 512:(half + 1) * 512], ALU.add,
                       [Po, H], [Hh])
                DMA("sp", H2[i], Hh.t[:, :], [Hh], [dH2[i]])
            S.barrier()
            S.flush()

        for es in phase("C"):
          T = NOWN * 128
          E1T = sb(es, [128, T], BF16)
          E2T = sb(es, [128, T], BF16)
          GGT = sb(es, [128, T], BF16)
          iotaf = sb(es, [128, 128])
          S.op("pool", lambda e: e.iota(iotaf.t[:, :], pattern=[[1, 128]], base=0, channel_multiplier=0,
                                        allow_small_or_imprecise_dtypes=True), [], [iotaf])
          with ExitStack() as es1:
            es_outer = es
            es = es1
            S.defer = True
            stage = [sb(es, [128, 2048]) for _ in range(2)]
            w_q_b = load_w_bf16(es, w_query, 8, 2048, stage)
            skb = sb(es, [128, 16, 128], BF16)
            for c in range(16):
                st = stage[c % 2]
                DMA("sp", st.t[:, 0:128], skT[c], [], [st])
                CP("dve", skb.t[:, c, :], st.t[:, 0:128], [st], [skb])
            thr = sb(es, [128, 15])
            io16 = sb(es, [128, 16])
            for m in range(15):
                S.op("pool", lambda e, m=m: e.memset(thr.t[:, m:m + 1], 16.0 * (m + 1) - 0.5), [], [thr])
            for m in range(16):
                S.op("pool", lambda e, m=m: e.memset(io16.t[:, m:m + 1], float(m)), [], [io16])
            hh2 = [sb(es, [128, D]) for _ in range(2)]
            xn = [sb(es, [128, D]) for _ in range(2)]
            xb2 = [sb(es, [128, D], BF16) for _ in range(2)]
            xT2 = [sb(es, [128, 8, 128], BF16) for _ in range(2)]
            junk2_ = [sb(es, [128, D]) for _ in range(2)]
            st42 = [sb(es, [128, 4]) for _ in range(2)]
            qpT2 = [sb(es, [128, 16, 128], BF16) for _ in range(2)]
            sc2 = [sb(es, [128, 16, 128]) for _ in range(2)]
            t16 = sb(es, [128, 8, 2, 16])
            ix = sb(es, [128, 8, 2, 16], U32)
            ixf = sb(es, [128, 8, 2, 16])
            scr = sb(es, [128, 128])
            cand = sb(es, [128, 8, 256])
            scr2 = sb(es, [128, 256])
            best = sb(es, [128, 8, 16])
            fx = sb(es, [128, 8, 16], U32)
            fxf = sb(es, [128, 128])
            cmp = sb(es, [128, 128, 16])
            fi = sb(es, [128, 128])
            fj = sb(es, [128, 128])
            e1 = sb(es, [128, 128])
            e2 = sb(es, [128, 128])
            ge = sb(es, [128, 8, 16])
            gs = sb(es, [128, 16])
            gg = sb(es, [128, 128])
            TPx = ps(es, [128, 8, 128], BF16)
            QS = ps(es, [128, 16, 128])
            t16s = [[Tile(t16.t[:, h, p, :]) for p in range(2)] for h in range(8)]
            ixs = [[Tile(ix.t[:, h, p, :]) for p in range(2)] for h in range(8)]
            scrs = [[sb(es, [128, 128]) for p in range(2)] for h in range(8)]
            bests = [Tile(best.t[:, h, :]) for h in range(8)]
            fxs = [Tile(fx.t[:, h, :]) for h in range(8)]
            scr2s = [sb(es, [128, 256]) for h in range(8)]

            def stage1(i):
                k = i % 2
                xb, xT, junk, st4, qpT, sc = xb2[k], xT2[k], junk2_[k], st42[k], qpT2[k], sc2[k]
                Hh, X = hh2[k], xn[k]
                DMA("sp", Hh.t[:, :], H2[i], [dH2[i]], [Hh])
                A(junk.t[:, :], Hh.t[:, :], AF.Square, [Hh], [junk, st4], accum_out=st4.t[:, 0:1])
                A(st4.t[:, 1:2], st4.t[:, 0:1], AF.Ln, [st4, epsc], [st4], scale=1.0 / D, bias=epsc.t[:, 0:1])
                A(st4.t[:, 2:3], st4.t[:, 1:2], AF.Exp, [st4], [st4], scale=-0.5)
                STT(X.t[:, :], Hh.t[:, :], st4.t[:, 2:3], V("ffn_norm"), ALU.mult, ALU.mult, [Hh, st4, vec], [X])
                CP("pool", xb.t[:, :], X.t[:, :], [X], [xb])
                for c in range(8):
                    TR(TPx.t[:, c, :], xb.t[:, c * 128:(c + 1) * 128], identb.t[:, :], [xb, identb], [TPx])
                CP("act", xT.t[:, :, :], TPx.t[:, :, :], [TPx], [xT])
                for c in range(16):
                    for kc in range(8):
                        MM(QS.t[:, c, :], w_q_b.t[:, kc, c * 128:(c + 1) * 128], xT.t[:, kc, :], kc == 0, kc == 7,
                           [w_q_b, xT], [QS])
                CP("act", qpT.t[:, 0:8, :], QS.t[:, 0:8, :], [QS], [qpT])
                CP("dve", qpT.t[:, 8:16, :], QS.t[:, 8:16, :], [QS], [qpT])
                for c in range(16):
                    MM(QS.t[:, c, :], qpT.t[:, c, :], skb.t[:, c, :], True, True, [qpT, skb], [QS])
                CP("act", sc.t[:, 0:8, :], QS.t[:, 0:8, :], [QS], [sc])
                CP("dve", sc.t[:, 8:16, :], QS.t[:, 8:16, :], [QS], [sc])
                HP = [(h, p) for h in range(8) for p in range(2)]
                for h, p in HP:
                    dve(lambda e, h=h, p=p: e.max(out=t16.t[:, h, p, 0:8], in_=sc.t[:, 2 * h + p, :]), [sc], [t16s[h][p]])
                for h, p in HP:
                    dve(lambda e, h=h, p=p: e.match_replace(out=scrs[h][p].t[:, :], in_to_replace=t16.t[:, h, p, 0:8],
                                                            in_values=sc.t[:, 2 * h + p, :], imm_value=-1e30),
                        [sc, t16s[h][p]], [scrs[h][p]])
                for h, p in HP:
                    dve(lambda e, h=h, p=p: e.max(out=t16.t[:, h, p, 8:16], in_=scrs[h][p].t[:, :]), [scrs[h][p]], [t16s[h][p]])
                for h, p in HP:
                    dve(lambda e, h=h, p=p: e.max_index(out=ix.t[:, h, p, 0:8], in_max=t16.t[:, h, p, 0:8],
                                                        in_values=sc.t[:, 2 * h + p, :]), [sc, t16s[h][p]], [ixs[h][p]])
                for h, p in HP:
                    dve(lambda e, h=h, p=p: e.max_index(out=ix.t[:, h, p, 8:16], in_max=t16.t[:, h, p, 8:16],
                                                        in_values=sc.t[:, 2 * h + p, :]), [sc, t16s[h][p]], [ixs[h][p]])
                all_t16 = [t16s[h][p] for h, p in HP]
                all_ix = [ixs[h][p] for h, p in HP]
                CP("dve", ixf.t[:, :, :, :], ix.t[:, :, :, :], all_ix, [ixf])
                TT(cand.t[:, :, :].rearrange("p h (a b) -> p h a b", a=16), bc(t16.t[:, :, 0, :].unsqueeze(3), [128, 8, 16, 16]),
                   bc(t16.t[:, :, 1, :].unsqueeze(2), [128, 8, 16, 16]), ALU.add, all_t16, [cand])
                for h in range(8):
                    dve(lambda e, h=h: e.max(out=best.t[:, h, 0:8], in_=cand.t[:, h, :]), [cand], [bests[h]])
                for h in range(8):
                    dve(lambda e, h=h: e.match_replace(out=scr2s[h].t[:, :], in_to_replace=best.t[:, h, 0:8],
                                                       in_values=cand.t[:, h, :], imm_value=-1e30), [cand, bests[h]], [scr2s[h]])
                for h in range(8):
                    dve(lambda e, h=h: e.max(out=best.t[:, h, 8:16], in_=scr2s[h].t[:, :]), [scr2s[h]], [bests[h]])
                for h in range(8):
                    dve(lambda e, h=h: e.max_index(out=fx.t[:, h, 0:8], in_max=best.t[:, h, 0:8],
                                                   in_values=cand.t[:, h, :]), [cand, bests[h]], [fxs[h]])
                for h in range(8):
                    dve(lambda e, h=h: e.max_index(out=fx.t[:, h, 8:16], in_max=best.t[:, h, 8:16],
                                                   in_values=cand.t[:, h, :]), [cand, bests[h]], [fxs[h]])
                best_all = bests
                CP("dve", fxf.t[:, :], fx.t[:, :, :].rearrange("p h k -> p (h k)"), fxs, [fxf])
                TT(cmp.t[:, :, 0:15], bc(fxf.t[:, :].unsqueeze(2), [128, 128, 15]), bc(thr.t[:, :].unsqueeze(1), [128, 128, 15]),
                   ALU.is_ge, [fxf, thr], [cmp])
                RED(fi.t[:, :], cmp.t[:, :, 0:15], [cmp], [fi])
                STT(fj.t[:, :], fi.t[:, :], -16.0, fxf.t[:, :], ALU.mult, ALU.add, [fi, fxf], [fj])
                c4 = cmp.t[:, :, :].rearrange("p (h k) i -> p h k i", h=8)
                for (fsel, pidx, eo) in ((fi, 0, e1), (fj, 1, e2)):
                    TT(cmp.t[:, :, :], bc(io16.t[:, :].unsqueeze(1), [128, 128, 16]), bc(fsel.t[:, :].unsqueeze(2), [128, 128, 16]),
                       ALU.is_equal, [io16, fsel], [cmp])
                    TT(c4, c4, bc(ixf.t[:, :, pidx, :].unsqueeze(2), [128, 8, 16, 16]), ALU.mult, [cmp, ixf], [cmp])
                    RED(eo.t[:, :], cmp.t[:, :, :], [cmp], [eo])
                TT(ge.t[:, :, :], best.t[:, :, :], bc(best.t[:, :, 0:1], [128, 8, 16]), ALU.subtract, bests, [ge])
                A(ge.t[:, :, :], ge.t[:, :, :], AF.Exp, [ge], [ge])
                RED(gs.t[:, 0:8], ge.t[:, :, :], [ge], [gs])
                dve(lambda e: e.reciprocal(out=gs.t[:, 8:16], in_=gs.t[:, 0:8]), [gs], [gs])
                TT(gg.t[:, :].rearrange("p (h k) -> p h k", h=8), ge.t[:, :, :], bc(gs.t[:, 8:16].unsqueeze(2), [128, 8, 16]),
                   ALU.mult, [ge, gs], [gg])


            for i in range(NOWN):
                stage1(i)
                DMA("sp", XT[i], xT2[i % 2].t[:, :, :], [xT2[i % 2]], [dXT[i]])
                for j, (srcT, dstT) in enumerate(((e1, E1T), (e2, E2T), (gg, GGT))):
                    MM(QS.t[:, j, :], srcT.t[:, :], ident.t[:, :], True, True, [srcT, ident], [QS])
                    CP("act", dstT.t[:, i * 128:(i + 1) * 128], QS.t[:, j, :], [QS], [dstT])
            S.barrier()
            S.flush()
            es = es_outer
          S.defer = True
          GB = 3
          NGRP = 0 if skip_c2 else -(-NOWN // GB)
          Wg = sb(es, [128, 128, GB * 128], BF16)
          xg = sb(es, [128, 8, GB * 128], BF16)
          hg = [sb(es, [128, D]) for _ in range(GB)]
          ust = [sb(es, [128, 8, 512], BF16) for _ in range(2)]
          vst = [sb(es, [128, 4, 1024], BF16) for _ in range(2)]
          gl = [sb(es, [128, GB * 128], BF16) for _ in range(3)]
          wa = [sb(es, [128, GB * 128], BF16) for _ in range(3)]
          At = [sb(es, [128, 128], BF16) for _ in range(4)]
          Bt = [sb(es, [128, 128], BF16) for _ in range(4)]
          yo = sb(es, [128, D])
          ACC = [ps(es, [128, D]) for _ in range(GB)]
          BK = [ps(es, [128, 512]) for _ in range(2)]
          ldn = [0]
          for grp in range(NGRP):
              blks = list(range(grp * GB, min(NOWN, (grp + 1) * GB)))
              nb_ = len(blks)
              G = nb_ * 128
              for j, bi in enumerate(blks):
                  DMA("sp", xg.t[:, :, j * 128:(j + 1) * 128], XT[bi], [dXT[bi]], [xg])
                  DMA("sp", hg[j].t[:, :], H2[bi], [dH2[bi]], [hg[j]])
              for t0 in range(0, G, 4):
                  bk = BK[(t0 // 4) % 2]
                  bkv = bk.t[:, :].rearrange("p (a b) -> p a b", a=4)
                  for tt in range(4):
                      tg = grp * GB * 128 + t0 + tt
                      a_, b_ = At[(t0 + tt) % 4], Bt[(t0 + tt) % 4]
                      TS(a_.t[:, :], iotaf.t[:, :], E1T.t[:, tg:tg + 1], GGT.t[:, tg:tg + 1], ALU.is_equal, ALU.mult,
                         [iotaf, E1T, GGT], [a_])
                      TS(b_.t[:, :], iotaf.t[:, :], E2T.t[:, tg:tg + 1], None, ALU.is_equal, None, [iotaf, E2T], [b_])
                      MM(bkv[:, tt, :], b_.t[:, :], a_.t[:, :], True, True, [a_, b_], [bk])
                  CP("act", Wg.t[:, :, t0:t0 + 4].rearrange("p n t -> p t n"), bkv, [bk], [Wg])
              def load_w(gq):
                  DMA("sp", ust[gq % 2].t[:, :, :], UB[gq], [dUB[gq]], [ust[gq % 2]])
                  DMA("pool", vst[gq % 2].t[:, :, :], VB[gq], [dVB[gq]], [vst[gq % 2]])

              def h_mm(c):
                  gq, cc = divmod(c, 4)
                  k_ = gq % 2
                  bk = BK[c % 2]
                  for kc in range(8):
                      MM(bk.t[:, 0:G], ust[k_].t[:, kc, cc * 128:(cc + 1) * 128], xg.t[:, kc, 0:G], kc == 0, kc == 7,
                         [ust[k_], xg], [bk])
                  A(gl[c % 3].t[:, 0:G], bk.t[:, 0:G], AF.Gelu, [bk], [gl[c % 3]])
                  TT(wa[c % 3].t[:, 0:G], gl[c % 3].t[:, 0:G], Wg.t[:, c, 0:G], ALU.mult, [gl[c % 3], Wg], [wa[c % 3]])
                  return k_

              def v_mm(c, k_):
                  cc = c % 4
                  for j in range(nb_):
                      for half in range(2):
                          MM(ACC[j].t[:, half * 512:(half + 1) * 512], wa[c % 3].t[:, j * 128:(j + 1) * 128],
                             vst[k_].t[:, cc, half * 512:(half + 1) * 512], c == 0, c == 127, [wa[c % 3], vst[k_]], [ACC[j]])

              load_w(0)
              load_w(1)
              pendv = []
              for c in range(128):
                  k_ = h_mm(c)
                  pendv.append((c, k_))
                  if len(pendv) > 2:
                      v_mm(*pendv.pop(0))
                  if c % 4 == 1 and c > 4 and c // 4 + 1 < 32:
                      load_w(c // 4 + 1)
              while pendv:
                  v_mm(*pendv.pop(0))
              for j, bi in enumerate(blks):
                  TT(yo.t[:, :], ACC[j].t[:, :], hg[j].t[:, :], ALU.add, [ACC[j], hg[j]], [yo])
                  DMA("sp", y[bi], yo.t[:, :], [yo], [])
          S.barrier()
          S.flush()
    return nc


def _t5_bucket_np(n):
    n = np.maximum(n, 0)
    nf = np.maximum(n, 1).astype(np.float32)
    large = 16 + (np.log(nf / 16) / math.log(128 / 16) * 16).astype(np.int32)
    large = np.minimum(large, 31)
    return np.where(n < 16, n, large)


def _bias_index_tables():
    k = np.arange(128)[:, None]
    q = np.arange(128)[None, :]
    diag = np.where(k <= q, _t5_bucket_np(q - k), 32)
    sub = _t5_bucket_np(128 + q - k)
    far = np.full((128, 128), 31)
    allm = np.full((128, 128), 32)
    return {"diag": diag, "sub": sub, "far": far, "allm": allm}


def prepare_inputs(inputs, NOWN, seq_pad_blocks=None):
    x = np.asarray(inputs["x"], np.float32)
    Bn, Sn, _ = x.shape
    NKB = 2 * NOWN
    L = NKB * 128
    meta = np.asarray(inputs["meta_tokens"], np.float32)
    rel_bias = np.asarray(inputs["rel_bias"], np.float32)
    half = 16
    inv_freq = (10000.0 ** (-np.arange(half, dtype=np.float32) / half)).astype(np.float32)
    pos = np.arange(L, dtype=np.float32)
    ang = pos[:, None] * inv_freq[None, :]
    cs_all = np.concatenate([np.cos(ang), np.sin(ang)], axis=1).astype(np.float32)

    def g(name):
        return np.asarray(inputs[name], np.float32)[0]

    vec_parts = {
        "attn_norm": g("attn_norm"), "ffn_norm": g("ffn_norm"), "q_norm": g("mla_q_norm"), "kv_norm": g("mla_kv_norm"),
        "gq": g("mla_qk_norm_q"), "gk": g("mla_qk_norm_k"), "gdq": g("diff_q_norm"), "gdk": g("diff_k_norm"),
        "lq1": g("diff_lambda_q1"), "lk1": g("diff_lambda_k1"), "lq2": g("diff_lambda_q2"), "lk2": g("diff_lambda_k2"),
        "subln": g("diff_subln"), "b31": rel_bias[31, :],
    }
    vecs = np.concatenate([vec_parts[n] for n, _ in VEC_LAYOUT])[None, :].astype(np.float32)
    tabs = _bias_index_tables()
    ext = np.concatenate([rel_bias, np.full((1, 4), NEG, np.float32)], axis=0)
    zero_neg = np.array([0.0, NEG], np.float32)
    tri = zero_neg[(tabs["diag"] == 32).astype(np.int64)]
    allneg = zero_neg[np.ones((128, 128), np.int64)]
    zeros = zero_neg[np.zeros((128, 128), np.int64)]
    common = {
        "w_in": g("w_in").reshape(8, 128, IN_W), "w_uq": g("mla_w_uq").reshape(2, 128, 768),
        "w_ukv": g("mla_w_ukv").reshape(2, 128, 1024), "w_out": g("w_out").reshape(8, 128, 1024),
        "w_query": g("peer_w_query").reshape(8, 128, 2048),
        "skT": np.ascontiguousarray(g("peer_sub_keys").reshape(16, 128, 128).transpose(0, 2, 1)),
        "peer_uT": np.ascontiguousarray(g("peer_u").T).reshape(8, 128, 16384), "peer_v": g("peer_v"), "vecs": vecs,
    }
    in_maps = []
    for b in range(Bn):
        hfull = np.zeros((L, D), np.float32)
        hfull[:NMETA] = meta
        hfull[NMETA:NMETA + Sn] = x[b]
        hseq = hfull.reshape(NKB, 128, D)
        for par in range(2):
            own = np.arange(NOWN) * 2 + par
            if par == 0:
                types = ["sub", "diag", "allm"]
                mm = np.stack([tri, allneg])
            else:
                types = ["far", "sub", "diag"]
                mm = np.stack([zeros, tri])
            bd = np.stack([np.stack([ext[tabs[t], h] for h in range(4)]) for t in types]).astype(np.float32)
            m = dict(common)
            m.update({
                "hseq": hseq, "hown": np.ascontiguousarray(hseq[own]),
                "csk": cs_all.reshape(NKB, 128, 32), "csq": np.ascontiguousarray(cs_all.reshape(NKB, 128, 32)[own]),
                "maskm": mm.astype(np.float32), "biasd": bd,
            })
            in_maps.append(m)
    return in_maps


def assemble(results, Bn, Sn, NOWN):
    NKB = 2 * NOWN
    out = np.zeros((Bn, NKB * 128, D), np.float32)
    for b in range(Bn):
        for par in range(2):
            yv = np.asarray(results[b * 2 + par]["y"]).reshape(NOWN, 128, D)
            full = out[b].reshape(NKB, 128, D)
            full[par::2] = yv
    return np.ascontiguousarray(out[:, NMETA:NMETA + Sn])


_NC_CACHE = {}


def kernel(**inputs):
    x = np.asarray(inputs["x"])
    Bn, Sn, _ = x.shape
    nblocks = -(-(NMETA + Sn) // 128)
    NOWN = (nblocks + 1) // 2
    if NOWN not in _NC_CACHE:
        _NC_CACHE[NOWN] = build(NOWN)
    nc = _NC_CACHE[NOWN]
    in_maps = prepare_inputs(inputs, NOWN)
    res = run_bass_kernel_spmd(nc, in_maps, core_ids=list(range(len(in_maps))))
    return assemble(res.results, Bn, Sn, NOWN).astype(np.float32)
```

```python
import math
from contextlib import ExitStack

import numpy as np
import concourse.bass as bass
import concourse.mybir as mybir
from concourse.bass_utils import run_bass_kernel_spmd

F32 = mybir.dt.float32
BF16 = mybir.dt.bfloat16
U32 = mybir.dt.uint32
AF = mybir.ActivationFunctionType
ALU = mybir.AluOpType
AX = mybir.AxisListType

D = 1024
NMETA = 16
EPS = 1e-6
LAMBDA_INIT = 0.2
NEG = -30000.0
IN_W = 2080

VEC_LAYOUT = [("attn_norm", 1024), ("ffn_norm", 1024), ("q_norm", 256), ("kv_norm", 256),
              ("gq", 96), ("gk", 96), ("gdq", 64), ("gdk", 64), ("lq1", 64), ("lk1", 64),
              ("lq2", 64), ("lk2", 64), ("subln", 128), ("b31", 4)]
VOFF = {}
_o = 0
for _n, _l in VEC_LAYOUT:
    VOFF[_n] = (_o, _o + _l)
    _o += _l
NVEC = _o


class Buf:
    __slots__ = ("w", "r")

    def __init__(self):
        self.w = None
        self.r = {}


class Tile:
    __slots__ = ("t", "b")

    def __init__(self, t):
        self.t = t
        self.b = Buf()


class Sched:
    ENG = ("pe", "act", "dve", "pool", "sp")

    def __init__(self, nc, es):
        self.nc = nc
        self.es = es
        self.sems = []
        self.owner = []
        self.prog = {e: [] for e in self.ENG}
        self.cnt = {e: 0 for e in self.ENG}
        self.esem = {}
        self.seen = {e: {} for e in self.ENG}
        for e in self.ENG:
            self._new_epoch(e)
        self.defer = False
        self.pending = []
        self.dq = {}
        for e, k in (("sp", 16), ("pool", 16), ("act", 4)):
            self.dq[e] = {"sems": [self._sem(None) for _ in range(k)], "cnt": [0] * k, "n": 0}

    def _sem(self, owner):
        s = self.es.enter_context(self.nc.semaphore(f"sm{len(self.sems)}"))
        self.sems.append(s)
        self.owner.append(owner)
        return len(self.sems) - 1

    def _new_epoch(self, e):
        self.esem[e] = self._sem(e)
        self.cnt[e] = 0

    def _waits(self, e, reads, writes):
        need = {}

        def add(s, v, war=False):
            if self.owner[s] == e and e == "pe":
                return
            if need.get(s, 0) < v:
                need[s] = v

        for b in reads:
            if b.w is not None:
                add(*b.w)
        for b in writes:
            if b.w is not None:
                add(*b.w)
            for s, v in b.r.items():
                add(s, v, True)
        out = []
        seen = self.seen[e]
        for s, v in need.items():
            if seen.get(s, 0) >= v:
                continue
            seen[s] = v
            out.append((s, v))
        return out

    def _book(self, ev, reads, writes):
        s, v = ev
        for b in reads:
            if b.r.get(s, 0) < v:
                b.r[s] = v
        for b in writes:
            b.w = ev
            b.r = {}

    def op(self, e, fn, reads=(), writes=(), dur=0.3):
        reads = [x.b if isinstance(x, Tile) else x for x in reads]
        writes = [x.b if isinstance(x, Tile) else x for x in writes]
        if self.defer:
            self.pending.append(("op", e, fn, reads, writes, dur, dur))
            return
        w = self._waits(e, reads, writes)
        if self.cnt[e] >= 60000:
            self._new_epoch(e)
        self.cnt[e] += 1
        ev = (self.esem[e], self.cnt[e])
        self.prog[e].append((w, fn, ev[0], 1))
        self._book(ev, reads, writes)

    def dma(self, e, fn, reads=(), writes=(), dur=3.0):
        reads = [x.b if isinstance(x, Tile) else x for x in reads]
        writes = [x.b if isinstance(x, Tile) else x for x in writes]
        if self.defer:
            self.pending.append(("dma", e, fn, reads, writes, 1.0 if e == "pool" else 0.35, dur))
            return
        q = self.dq[e]
        k = q["n"] % len(q["sems"])
        q["n"] += 1
        s = q["sems"][k]
        w = self._waits(e, reads, writes)
        c = q["cnt"][k]
        if c > 0 and self.seen[e].get(s, 0) < c:
            self.seen[e][s] = c
            w.append((s, c))
        q["cnt"][k] = c + 16
        ev = (s, c + 16)
        self.prog[e].append((w, fn, s, 16))
        self._book(ev, reads, writes)

    def reorder(self):
        import heapq
        ops = self.pending
        self.pending = []
        self.defer = False
        n = len(ops)
        lastw, readers = {}, {}
        deps = [None] * n
        succ = [[] for _ in range(n)]
        for i, (_, e, fn, reads, writes, busy, lat) in enumerate(ops):
            d = set()
            for b in reads:
                if id(b) in lastw:
                    d.add(lastw[id(b)])
            for b in writes:
                if id(b) in lastw:
                    d.add(lastw[id(b)])
                d.update(readers.get(id(b), ()))
            d.discard(i)
            for b in reads:
                readers.setdefault(id(b), []).append(i)
            for b in writes:
                lastw[id(b)] = i
                readers[id(b)] = []
            deps[i] = len(d)
            for j in d:
                succ[j].append(i)
        ready_t = [0.0] * n
        heaps = {e: [] for e in self.ENG}
        for i in range(n):
            if deps[i] == 0:
                heapq.heappush(heaps[ops[i][1]], (0.0, i))
        free = {e: 0.0 for e in self.ENG}
        order = []
        done = 0
        while done < n:
            best = None
            for e in self.ENG:
                h = heaps[e]
                if not h:
                    continue
                rt, i = h[0]
                st = max(rt, free[e])
                if best is None or (st, i) < (best[0], best[2]):
                    best = (st, e, i)
            st, e, i = best
            h = heaps[e]
            cand = []
            while h and h[0][0] <= st:
                cand.append(heapq.heappop(h))
            cand.sort(key=lambda x: x[1])
            rt, i = cand[0]
            for c in cand[1:]:
                heapq.heappush(h, c)
            busy, lat = ops[i][5], ops[i][6]
            free[e] = st + busy
            fin = st + lat
            order.append((st, i))
            done += 1
            for j in succ[i]:
                deps[j] -= 1
                t_ = fin + (0.0 if ops[j][1] == e else 0.3)
                if t_ > ready_t[j]:
                    ready_t[j] = t_
                if deps[j] == 0:
                    heapq.heappush(heaps[ops[j][1]], (ready_t[j], j))
        order.sort()
        for _, i in order:
            kind, e, fn, reads, writes, busy, lat = ops[i]
            if kind == "op":
                self.op(e, fn, reads, writes)
            else:
                self.dma(e, fn, reads, writes)

    def barrier(self):
        if self.pending:
            self.reorder()
        evs = []
        for e in self.ENG:
            if self.cnt[e] > 0:
                evs.append((self.esem[e], self.cnt[e]))
        for q in self.dq.values():
            for s, c in zip(q["sems"], q["cnt"]):
                if c > 0:
                    evs.append((s, c))
        for e in self.ENG:
            w = []
            for s, v in evs:
                if self.owner[s] == e:
                    continue
                if self.seen[e].get(s, 0) >= v:
                    continue
                self.seen[e][s] = v
                w.append((s, v))
            if w:
                self.prog[e].append((w, None, None, 0))

    def _run(self, lst, eng):
        for w, fn, s, inc in lst:
            for sw, v in w:
                eng.wait_ge(self.sems[sw], v)
            if fn is not None:
                fn(eng).then_inc(self.sems[s], inc)

    def flush(self):
        progs = self.prog
        self.prog = {e: [] for e in self.ENG}
        with self.nc.Block() as block:
            @block.tensor
            def _(eng):
                self._run(progs["pe"], eng)

            @block.scalar
            def _(eng):
                self._run(progs["act"], eng)

            @block.vector
            def _(eng):
                self._run(progs["dve"], eng)

            @block.gpsimd
            def _(eng):
                self._run(progs["pool"], eng)

            @block.sync
            def _(eng):
                self._run(progs["sp"], eng)


def bc(ap, shape):
    return ap.to_broadcast(list(shape))


def build(NOWN, debug=False, phases="ABC", skip_c2=False):
    NKB = 2 * NOWN
    nc = bass.Bass("TRN2", target_bir_lowering=False)

    def din(name, shape, dt=F32):
        return nc.dram_tensor(name, list(shape), dt, kind="ExternalInput")

    skind = "ExternalOutput" if debug else "Internal"

    def dsc(name, shape, dt):
        return nc.dram_tensor(name, list(shape), dt, kind=skind)

    hseq = din("hseq", [NKB, 128, D])
    hown = din("hown", [NOWN, 128, D])
    csk = din("csk", [NKB, 128, 32])
    csq = din("csq", [NOWN, 128, 32])
    w_in = din("w_in", [8, 128, IN_W])
    w_uq = din("w_uq", [2, 128, 768])
    w_ukv = din("w_ukv", [2, 128, 1024])
    w_out = din("w_out", [8, 128, 1024])
    w_query = din("w_query", [8, 128, 2048])
    skT = din("skT", [16, 128, 128])
    uT_d = din("peer_uT", [8, 128, 16384])
    v_d = din("peer_v", [16384, D])
    UB = dsc("UB", [32, 128, 8, 512], BF16)
    VB = dsc("VB", [32, 128, 4, 1024], BF16)
    XT = dsc("XT", [NOWN, 128, 8, 128], BF16)
    dUB, dVB = ([Buf() for _ in range(32)] for _ in range(2))
    dXT = [Buf() for _ in range(NOWN)]
    vecs = din("vecs", [1, NVEC])
    maskm = din("maskm", [2, 128, 128])
    biasd = din("biasd", [3, 4, 128, 128])
    y = nc.dram_tensor("y", [NOWN, 128, D], F32, kind="ExternalOutput")

    KVW = 1024 + 520 + 512 + 516
    KVD = dsc("KV", [NKB, 128, KVW], BF16)

    class _Sub:
        def __init__(self, c0, shape, p=128):
            self.c0, self.shape, self.p = c0, shape, p

        def __getitem__(self, blk):
            a, b = self.shape
            return KVD[blk, 0:self.p, self.c0:self.c0 + a * b].rearrange("p (a b) -> p a b", a=a)

    KTm, Vm, KTd, Vd = _Sub(0, (8, 128), 96), _Sub(1024, (8, 65)), _Sub(1544, (4, 128)), _Sub(2056, (4, 129))
    QTm = dsc("QTm", [NOWN, 96, 8, 128], BF16)
    QTd = dsc("QTd", [NOWN, 128, 4, 128], BF16)
    H2 = dsc("H2", [NOWN, 128, D], F32)
    dKTm, dVm, dKTd, dVd = ([Buf() for _ in range(NKB)] for _ in range(4))
    dQTm, dQTd, dH2 = ([Buf() for _ in range(NOWN)] for _ in range(3))

    with ExitStack() as ges:
        S = Sched(nc, ges)
        ncnt = [0]

        def sb(es, shape, dt=F32):
            ncnt[0] += 1
            return Tile(es.enter_context(nc.sbuf_tensor(f"t{ncnt[0]}", list(shape), dt)))

        def ps(es, shape, dt=F32):
            ncnt[0] += 1
            per_bank = 512 if dt == F32 else 1024
            n = int(np.prod(shape[1:]))
            nb_ = -(-n // per_bank)
            t = es.enter_context(nc.psum_tensor(f"p{ncnt[0]}", [128, nb_ * per_bank], dt))
            v = t[:, 0:n]
            if len(shape) == 3:
                v = v.rearrange("p (a b) -> p a b", a=shape[1])
            return Tile(v)

        def phase(name):
            if name in phases:
                with ExitStack() as pes:
                    yield pes

        def nel(ap):
            n = 1
            for s_ in ap.shape[1:]:
                n *= int(s_)
            return n

        def act(fn, r, w, dur=0.3):
            S.op("act", fn, r, w, dur)

        def dve(fn, r, w, dur=0.3):
            S.op("dve", fn, r, w, dur)

        def pe(fn, r, w, dur=0.15):
            S.op("pe", fn, r, w, dur)

        def A(out, in_, func, r, w, **kw):
            act(lambda e: e.activation(out=out, in_=in_, func=func, **kw), r, w, 0.25 + nel(out) * 0.00075)

        def TT(out, in0, in1, op, r, w, eng="dve"):
            S.op(eng, lambda e: e.tensor_tensor(out=out, in0=in0, in1=in1, op=op), r, w, 0.12 + nel(out) * 0.0016)

        def TS(out, in0, s1, s2, op0, op1, r, w, eng="dve"):
            if op1 is None:
                S.op(eng, lambda e: e.tensor_scalar(out=out, in0=in0, scalar1=s1, scalar2=None, op0=op0), r, w,
                     0.12 + nel(out) * 0.001)
            else:
                S.op(eng, lambda e: e.tensor_scalar(out=out, in0=in0, scalar1=s1, scalar2=s2, op0=op0, op1=op1), r, w,
                     0.12 + nel(out) * 0.001)

        def STT(out, in0, sc, in1, op0, op1, r, w, accum=None):
            if accum is None:
                dve(lambda e: e.scalar_tensor_tensor(out=out, in0=in0, scalar=sc, in1=in1, op0=op0, op1=op1), r, w,
                    0.12 + nel(out) * 0.0016)
            else:
                dve(lambda e: e.scalar_tensor_tensor(out=out, in0=in0, scalar=sc, in1=in1, op0=op0, op1=op1,
                                                     accum_out=accum), r, w, 0.25 + nel(out) * 0.0016)

        def RED(out, in_, r, w):
            dve(lambda e: e.tensor_reduce(out=out, in_=in_, axis=AX.X, op=ALU.add), r, w, 0.12 + nel(in_) * 0.00105)

        def CP(eng, out, in_, r, w):
            if eng == "act":
                act(lambda e: e.copy(out=out, in_=in_), r, w, 0.25 + nel(out) * 0.00075)
            else:
                S.op(eng, lambda e: e.tensor_copy(out=out, in_=in_), r, w,
                     (0.12 + nel(out) * 0.00105) if eng == "dve" else (0.3 + nel(out) * 0.0006))

        def MM(out, lhsT, rhs, start, stop, r, w):
            pe(lambda e: e.matmul(out, lhsT, rhs, start=start, stop=stop), r, w, 0.1 + nel(rhs) * 0.00052)

        def TR(out, in_, ident, r, w):
            pe(lambda e: e.transpose(out, in_, ident), r, w, 0.2)

        def DMA(eng, out, in_, r, w):
            S.dma(eng, lambda e: e.dma_start(out=out, in_=in_), r, w, 2.5 + nel(out) * 128 * 4 / 1.0e5)

        vec = sb(ges, [128, NVEC])
        ident = sb(ges, [128, 128])
        identb = sb(ges, [128, 128], BF16)
        epsc = sb(ges, [128, 1])
        gqs = sb(ges, [128, 96])
        gdqs = sb(ges, [128, 64])
        subl8 = sb(ges, [128, 128])
        neglam = sb(ges, [128, 1])
        ltmp = sb(ges, [128, 64])
        lsc = sb(ges, [128, 4])

        def V(name):
            a, b = VOFF[name]
            return vec.t[:, a:b]

        DMA("sp", vec.t[:, :], vecs[0:1, :].to_broadcast([128, NVEC]), [], [vec])
        S.op("pool", lambda e: e.memset(ident.t[:, :], 0.0), [], [ident])
        S.op("pool", lambda e: e.affine_select(out=ident.t[:, :], in_=ident.t[:, :], pattern=[[-1, 128]],
                                               compare_op=ALU.not_equal, fill=1.0, base=0, channel_multiplier=1),
             [ident], [ident])
        CP("dve", identb.t[:, :], ident.t[:, :], [ident], [identb])
        S.op("pool", lambda e: e.memset(epsc.t[:, :], EPS), [], [epsc])
        TS(gqs.t[:, :], V("gq"), 96.0 ** -0.5, None, ALU.mult, None, [vec], [gqs])
        TS(gdqs.t[:, :], V("gdq"), 0.125, None, ALU.mult, None, [vec], [gdqs])
        TS(subl8.t[:, :], V("subln"), 1.0 - LAMBDA_INIT, None, ALU.mult, None, [vec], [subl8])
        STT(ltmp.t[:, :], V("lq1"), 1.0, V("lk1"), ALU.mult, ALU.mult, [vec], [ltmp, lsc], accum=lsc.t[:, 0:1])
        STT(ltmp.t[:, :], V("lq2"), 1.0, V("lk2"), ALU.mult, ALU.mult, [vec], [ltmp, lsc], accum=lsc.t[:, 1:2])
        A(lsc.t[:, 2:4], lsc.t[:, 0:2], AF.Exp, [lsc], [lsc])
        TT(neglam.t[:, :], lsc.t[:, 3:4], lsc.t[:, 2:3], ALU.subtract, [lsc], [neglam])
        TS(neglam.t[:, :], neglam.t[:, :], -LAMBDA_INIT, None, ALU.add, None, [neglam], [neglam])

        def load_w_bf16(es, dram, nchunk, ncol, stage_pool):
            wt = sb(es, [128, nchunk, ncol], BF16)
            for c in range(nchunk):
                st = stage_pool[c % len(stage_pool)]
                DMA("sp", st.t[:, 0:ncol], dram[c], [], [st])
                if c % 2 == 0:
                    CP("dve", wt.t[:, c, :], st.t[:, 0:ncol], [st], [wt])
                else:
                    CP("pool", wt.t[:, c, :], st.t[:, 0:ncol], [st], [wt])
            return wt

        for es in phase("A"):
            S.defer = True
            stage = [sb(es, [128, IN_W]) for _ in range(2)]
            w_in_b = load_w_bf16(es, w_in, 8, IN_W, stage)
            w_uq_b = load_w_bf16(es, w_uq, 2, 768, stage)
            w_ukv_b = load_w_bf16(es, w_ukv, 2, 1024, stage)

            NB_ = 3
            hb = [sb(es, [128, D]) for _ in range(NB_)]
            cs = [sb(es, [128, 32]) for _ in range(NB_)]
            junkA = [sb(es, [128, D]) for _ in range(NB_)]
            st4 = [sb(es, [128, 8]) for _ in range(NB_)]
            nb = [sb(es, [128, D], BF16) for _ in range(NB_)]
            nT = [sb(es, [128, 8, 128], BF16) for _ in range(NB_)]
            cn = [sb(es, [128, 256], BF16) for _ in range(NB_)]
            cT = [sb(es, [128, 2, 128], BF16) for _ in range(NB_)]
            kvs = [sb(es, [128, 1024]) for _ in range(NB_)]
            dks = [sb(es, [128, 512]) for _ in range(NB_)]
            pas = [sb(es, [128, 288]) for _ in range(NB_)]
            sqA = [sb(es, [128, 1024]) for _ in range(NB_)]
            s8 = [sb(es, [128, 32]) for _ in range(NB_)]
            krg = [sb(es, [128, 8, 32]) for _ in range(NB_)]
            rr = [sb(es, [128, 8, 32]) for _ in range(NB_)]
            kk = [sb(es, [128, 8, 96], BF16) for _ in range(NB_)]
            kd = [sb(es, [128, 512], BF16) for _ in range(NB_)]
            tmp8A = [sb(es, [128, 8, 64]) for _ in range(NB_)]
            ktm = [sb(es, [128, 8, 128], BF16) for _ in range(NB_)]
            ktd = [sb(es, [128, 4, 128], BF16) for _ in range(NB_)]
            vv = [sb(es, [128, 8, 65], BF16) for _ in range(NB_)]
            vd = [sb(es, [128, 4, 129], BF16) for _ in range(NB_)]
            for t in ktm:
                S.op("pool", lambda e, t=t: e.memset(t.t[:, :, :], 0.0), [], [t])
            for t in vv:
                S.op("pool", lambda e, t=t: e.memset(t.t[:, :, 64:65], 1.0), [], [t])
            for t in vd:
                S.op("pool", lambda e, t=t: e.memset(t.t[:, :, 128:129], 1.0), [], [t])

            TP = ps(es, [128, 8, 128], BF16)
            TK = ps(es, [128, 16, 128], BF16)
            PA = ps(es, [128, 512])
            PB = ps(es, [128, 512])
            PC = ps(es, [128, 512])
            PD = ps(es, [128, 1024])

            def rstd_of(out, ss, dim, rbufs, wbuf, tmp):
                A(tmp, ss, AF.Ln, rbufs + [epsc], [wbuf], scale=1.0 / dim, bias=epsc.t[:, 0:1])
                A(out, tmp, AF.Exp, [wbuf], [wbuf], scale=-0.5)

            def p1(kside, blk, it):
                k = it % NB_
                junk, sq, tmp8 = junkA[k], sqA[k], tmp8A[k]
                H, C = hb[k], cs[k]
                src = hseq[blk] if kside else hown[blk]
                DMA("sp", H.t[:, :], src, [], [H])
                DMA("sp", C.t[:, :], (csk if kside else csq)[blk], [], [C])
                st = st4[k]
                A(junk.t[:, :], H.t[:, :], AF.Square, [H], [junk, st], accum_out=st.t[:, 0:1])
                rstd_of(st.t[:, 2:3], st.t[:, 0:1], D, [st], st, st.t[:, 1:2])
                STT(nb[k].t[:, :], H.t[:, :], st.t[:, 2:3], V("attn_norm"), ALU.mult, ALU.mult, [H, st, vec], [nb[k]])
                for c in range(8):
                    TR(TP.t[:, c, :], nb[k].t[:, c * 128:(c + 1) * 128], identb.t[:, :], [nb[k], identb], [TP])
                CP("act", nT[k].t[:, :, :], TP.t[:, :, :], [TP], [nT[k]])
                if kside:
                    groups = [(PA, 288, 256), (PB, 512, 1056), (PC, 512, 1568)]
                else:
                    groups = [(PA, 256, 0), (PB, 512, 544)]
                for (pt, n, c0) in groups:
                    for c in range(8):
                        MM(pt.t[:, 0:n], nT[k].t[:, c, :], w_in_b.t[:, c, c0:c0 + n], c == 0, c == 7,
                           [nT[k], w_in_b], [pt])
                npa = 288 if kside else 256
                CP("act", pas[k].t[:, 0:npa], PA.t[:, 0:npa], [PA], [pas[k]])
                CP("act", dks[k].t[:, :], PB.t[:, :], [PB], [dks[k]])
                if kside:
                    CP("dve", vd[k].t[:, :, 0:128], PC.t[:, :].rearrange("p (h e) -> p h e", h=4), [PC], [vd[k]])
                    DMA("sp", Vd[blk], vd[k].t[:, :, :], [vd[k]], [dVd[blk]])

            def p2(kside, blk, it):
                k = it % NB_
                junk, sq, tmp8 = junkA[k], sqA[k], tmp8A[k]
                C = cs[k]
                st = st4[k]
                gname = "kv_norm" if kside else "q_norm"
                A(junk.t[:, 0:256], pas[k].t[:, 0:256], AF.Square, [pas[k]], [junk, st], accum_out=st.t[:, 3:4])
                rstd_of(st.t[:, 5:6], st.t[:, 3:4], 256, [st], st, st.t[:, 4:5])
                STT(cn[k].t[:, :], pas[k].t[:, 0:256], st.t[:, 5:6], V(gname), ALU.mult, ALU.mult, [pas[k], st, vec], [cn[k]])
                for c in range(2):
                    TR(TK.t[:, 12 + c, :], cn[k].t[:, c * 128:(c + 1) * 128], identb.t[:, :], [cn[k], identb], [TK])
                CP("act", cT[k].t[:, :, :], TK.t[:, 12:14, :], [TK], [cT[k]])
                wup = w_ukv_b if kside else w_uq_b
                nup = 1024 if kside else 768
                for h0 in range(0, nup, 512):
                    n = min(512, nup - h0)
                    for c in range(2):
                        MM(PD.t[:, h0:h0 + n], cT[k].t[:, c, :], wup.t[:, c, h0:h0 + n], c == 0, c == 1,
                           [cT[k], wup], [PD])
                KV = kvs[k]
                CP("act", KV.t[:, 0:nup], PD.t[:, 0:nup], [PD], [KV])
                s = s8[k]
                if kside:
                    kv3 = KV.t[:, :].rearrange("p (h e) -> p h e", h=8)
                    TT(sq.t[:, 0:512].rearrange("p (h e) -> p h e", h=8), kv3[:, :, 0:64], kv3[:, :, 0:64], ALU.mult,
                       [KV], [sq])
                    RED(s.t[:, 0:8], sq.t[:, 0:512].rearrange("p (h e) -> p h e", h=8), [sq], [s])
                    CP("act", krg[k].t[:, 0, :], pas[k].t[:, 256:288], [pas[k]], [krg[k]])
                    A(junk.t[:, 0:32], krg[k].t[:, 0, :], AF.Square, [krg[k]], [junk, st], accum_out=st.t[:, 6:7])
                    TS(s.t[:, 0:8], s.t[:, 0:8], st.t[:, 6:7], None, ALU.add, None, [s, st], [s])
                    rstd_of(s.t[:, 16:24], s.t[:, 0:8], 96, [s], s, s.t[:, 8:16])
                    K3 = kk[k].t
                    TT(tmp8.t[:, :, :], kv3[:, :, 0:64], bc(s.t[:, 16:24].unsqueeze(2), [128, 8, 64]), ALU.mult,
                       [KV, s], [tmp8])
                    TT(K3[:, :, 0:64], tmp8.t[:, :, :], bc(V("gk")[:, 0:64].unsqueeze(1), [128, 8, 64]), ALU.mult,
                       [tmp8, vec], [kk[k]])
                    t0 = krg[k].t[:, 1, :]
                    TT(t0, krg[k].t[:, 0, :], V("gk")[:, 64:96], ALU.mult, [krg[k], vec], [krg[k]])
                    co, si = C.t[:, 0:16], C.t[:, 16:32]
                    r1, r2 = rr[k].t[:, 0, 0:16], rr[k].t[:, 0, 16:32]
                    a1, a2 = rr[k].t[:, 1, 0:16], rr[k].t[:, 1, 16:32]
                    TT(a1, t0[:, 0:16], co, ALU.mult, [krg[k], C], [rr[k]])
                    TT(a2, t0[:, 16:32], si, ALU.mult, [krg[k], C], [rr[k]])
                    TT(r1, a1, a2, ALU.subtract, [rr[k]], [rr[k]])
                    TT(a1, t0[:, 0:16], si, ALU.mult, [krg[k], C, rr[k]], [rr[k]])
                    TT(a2, t0[:, 16:32], co, ALU.mult, [krg[k], C, rr[k]], [rr[k]])
                    TT(r2, a1, a2, ALU.add, [rr[k]], [rr[k]])
                    TT(K3[:, :, 64:96], bc(rr[k].t[:, 0:1, :], [128, 8, 32]), bc(s.t[:, 16:24].unsqueeze(2), [128, 8, 32]),
                       ALU.mult, [rr[k], s], [kk[k]])
                    for h in range(8):
                        TR(TK.t[0:96, h, :], K3[:, h, :], identb.t[:, :], [kk[k], identb], [TK])
                    CP("act", ktm[k].t[0:96, :, :], TK.t[0:96, 0:8, :], [TK], [ktm[k]])
                    DMA("sp", KVD[blk, :, 0:1024], ktm[k].t[:, :, :].rearrange("p a b -> p (a b)"), [ktm[k]], [dKTm[blk]])
                    CP("pool", vv[k].t[:, :, 0:64], kv3[:, :, 64:128], [KV], [vv[k]])
                    DMA("sp", Vm[blk], vv[k].t[:, :, :], [vv[k]], [dVm[blk]])
                    d3 = dks[k].t[:, :].rearrange("p (g e) -> p g e", g=8)
                    TT(sq.t[:, 512:1024].rearrange("p (g e) -> p g e", g=8), d3, d3, ALU.mult, [dks[k]], [sq])
                    RED(s.t[:, 24:32], sq.t[:, 512:1024].rearrange("p (g e) -> p g e", g=8), [sq], [s])
                    rstd_of(s.t[:, 24:32], s.t[:, 24:32], 64, [s], s, s.t[:, 8:16])
                    TT(tmp8.t[:, :, :], d3, bc(s.t[:, 24:32].unsqueeze(2), [128, 8, 64]), ALU.mult, [dks[k], s], [tmp8])
                    TT(kd[k].t[:, :].rearrange("p (g e) -> p g e", g=8), tmp8.t[:, :, :],
                       bc(V("gdk").unsqueeze(1), [128, 8, 64]), ALU.mult, [tmp8, vec], [kd[k]])
                    for h in range(4):
                        TR(TK.t[:, 8 + h, :], kd[k].t[:, h * 128:(h + 1) * 128], identb.t[:, :], [kd[k], identb], [TK])
                    CP("act", ktd[k].t[:, :, :], TK.t[:, 8:12, :], [TK], [ktd[k]])
                    DMA("sp", KTd[blk], ktd[k].t[:, :, :], [ktd[k]], [dKTd[blk]])
                else:
                    q3 = KV.t[:, 0:768].rearrange("p (h e) -> p h e", h=8)
                    TT(sq.t[:, 0:768].rearrange("p (h e) -> p h e", h=8), q3, q3, ALU.mult, [KV], [sq])
                    RED(s.t[:, 0:8], sq.t[:, 0:768].rearrange("p (h e) -> p h e", h=8), [sq], [s])
                    rstd_of(s.t[:, 16:24], s.t[:, 0:8], 96, [s], s, s.t[:, 8:16])
                    Q3 = kk[k].t
                    TT(tmp8.t[:, :, :], q3[:, :, 0:64], bc(s.t[:, 16:24].unsqueeze(2), [128, 8, 64]), ALU.mult,
                       [KV, s], [tmp8])
                    TT(Q3[:, :, 0:64], tmp8.t[:, :, :], bc(gqs.t[:, 0:64].unsqueeze(1), [128, 8, 64]), ALU.mult,
                       [tmp8, gqs], [kk[k]])
                    tq = krg[k].t
                    TT(tq[:, :, :], q3[:, :, 64:96], bc(s.t[:, 16:24].unsqueeze(2), [128, 8, 32]), ALU.mult,
                       [KV, s], [krg[k]])
                    TT(tq[:, :, :], tq[:, :, :], bc(gqs.t[:, 64:96].unsqueeze(1), [128, 8, 32]), ALU.mult,
                       [krg[k], gqs], [krg[k]])
                    co = bc(C.t[:, 0:16].unsqueeze(1), [128, 8, 16])
                    si = bc(C.t[:, 16:32].unsqueeze(1), [128, 8, 16])
                    a1, a2 = rr[k].t[:, :, 0:16], rr[k].t[:, :, 16:32]
                    TT(a1, tq[:, :, 0:16], co, ALU.mult, [krg[k], C], [rr[k]])
                    TT(a2, tq[:, :, 16:32], si, ALU.mult, [krg[k], C], [rr[k]])
                    TT(Q3[:, :, 64:80], a1, a2, ALU.subtract, [rr[k]], [kk[k]])
                    TT(a1, tq[:, :, 0:16], si, ALU.mult, [krg[k], C, kk[k]], [rr[k]])
                    TT(a2, tq[:, :, 16:32], co, ALU.mult, [krg[k], C, kk[k]], [rr[k]])
                    TT(Q3[:, :, 80:96], a1, a2, ALU.add, [rr[k]], [kk[k]])
                    for h in range(8):
                        TR(TK.t[0:96, h, :], Q3[:, h, :], identb.t[:, :], [kk[k], identb], [TK])
                    CP("act", ktm[k].t[0:96, :, :], TK.t[0:96, 0:8, :], [TK], [ktm[k]])
                    DMA("sp", QTm[blk], ktm[k].t[0:96, :, :], [ktm[k]], [dQTm[blk]])
                    d3 = dks[k].t[:, :].rearrange("p (g e) -> p g e", g=8)
                    TT(sq.t[:, 512:1024].rearrange("p (g e) -> p g e", g=8), d3, d3, ALU.mult, [dks[k]], [sq])
                    RED(s.t[:, 24:32], sq.t[:, 512:1024].rearrange("p (g e) -> p g e", g=8), [sq], [s])
                    rstd_of(s.t[:, 24:32], s.t[:, 24:32], 64, [s], s, s.t[:, 8:16])
                    TT(tmp8.t[:, :, :], d3, bc(s.t[:, 24:32].unsqueeze(2), [128, 8, 64]), ALU.mult, [dks[k], s], [tmp8])
                    TT(kd[k].t[:, :].rearrange("p (g e) -> p g e", g=8), tmp8.t[:, :, :],
                       bc(gdqs.t[:, :].unsqueeze(1), [128, 8, 64]), ALU.mult, [tmp8, gdqs], [kd[k]])
                    for h in range(4):
                        TR(TK.t[:, 8 + h, :], kd[k].t[:, h * 128:(h + 1) * 128], identb.t[:, :], [kd[k], identb], [TK])
                    CP("act", ktd[k].t[:, :, :], TK.t[:, 8:12, :], [TK], [ktd[k]])
                    DMA("sp", QTd[blk], ktd[k].t[:, :, :], [ktd[k]], [dQTd[blk]])

            seq = [(True, blk) for blk in range(NKB)] + [(False, blk) for blk in range(NOWN)]
            p1(seq[0][0], seq[0][1], 0)
            for n in range(len(seq)):
                if n + 1 < len(seq):
                    p1(seq[n + 1][0], seq[n + 1][1], n + 1)
                p2(seq[n][0], seq[n][1], n)
            S.barrier()
            S.flush()

        for es in phase("B"):
            S.defer = True
            stage = [sb(es, [128, 1024]) for _ in range(2)]
            w_out_b = load_w_bf16(es, w_out, 8, 1024, stage)
            mk = sb(es, [128, 2, 128])
            bd = sb(es, [128, 12, 128])
            b31c = V("b31")
            for t in range(2):
                DMA("sp", mk.t[:, t, :], maskm[t], [], [mk])
            for t in range(3):
                for h in range(4):
                    DMA("sp", bd.t[:, t * 4 + h, :], biasd[t, h], [], [bd])
            for t in range(3):
                for h in range(4):
                    TS(bd.t[:, t * 4 + h, :], bd.t[:, t * 4 + h, :], b31c[:, h:h + 1], None, ALU.subtract, None,
                       [bd, vec], [bd])
            mkb = sb(es, [128, 2, 128], BF16)
            bdh = sb(es, [128, 12, 128], BF16)
            bdl = sb(es, [128, 12, 128], BF16)
            bdr = sb(es, [128, 12, 128])
            CP("dve", mkb.t[:, :, :], mk.t[:, :, :], [mk], [mkb])
            CP("dve", bdh.t[:, :, :], bd.t[:, :, :], [bd], [bdh])
            TT(bdr.t[:, :, :], bd.t[:, :, :], bdh.t[:, :, :], ALU.subtract, [bd, bdh], [bdr])
            CP("dve", bdl.t[:, :, :], bdr.t[:, :, :], [bdr], [bdl])
            NKV = 6
            kvt = [sb(es, [128, KVW], BF16) for _ in range(NKV)]
            qtm = [sb(es, [128, 8, 128], BF16) for _ in range(2)]
            qtd = [sb(es, [128, 4, 128], BF16) for _ in range(2)]
            ptm = [sb(es, [128, 4, 128], BF16) for _ in range(4)]
            ptd = [sb(es, [128, 4, 128], BF16) for _ in range(4)]
            hb = [sb(es, [128, D]) for _ in range(2)]
            mixb = sb(es, [128, D], BF16)
            mixT = sb(es, [128, 8, 128], BF16)
            h2 = [sb(es, [128, D]) for _ in range(2)]
            rec = sb(es, [128, 16])
            od = sb(es, [128, 8, 128])
            odd = sb(es, [128, 4, 128])
            sq = sb(es, [128, 4, 128])
            s4 = sb(es, [128, 12])

            AM = [ps(es, [128, 4, 65]) for _ in range(2)]
            AD = [ps(es, [128, 3, 129]), ps(es, [128, 3, 129]), ps(es, [128, 2, 129])]
            SS = [ps(es, [128, 4, 128]) for _ in range(3)]
            sidx = [0]

            def next_s():
                t = SS[sidx[0] % 3]
                sidx[0] += 1
                return t

            if "C" in phases:
                cst = [sb(es, [128, 4096]) for _ in range(2)]
                cbf = [sb(es, [128, 4096], BF16) for _ in range(2)]
                for gq in range(32):
                    for which in range(2):
                        j = (gq * 2 + which) % 2
                        if which == 0:
                            src_ap = uT_d[:, :, gq * 512:(gq + 1) * 512].rearrange("k d e -> d k e")
                            dst_ap, dbuf = UB[gq].rearrange("d k e -> d (k e)"), dUB[gq]
                            stv = cst[j].t[:, :].rearrange("p (k e) -> p k e", k=8)
                        else:
                            src_ap = v_d[gq * 512:(gq + 1) * 512, :].rearrange("(c e) d -> e c d", c=4)
                            dst_ap, dbuf = VB[gq].rearrange("e c d -> e (c d)"), dVB[gq]
                            stv = cst[j].t[:, :].rearrange("p (c d) -> p c d", c=4)
                        DMA("pool", stv, src_ap, [], [cst[j]])
                        CP("pool", cbf[j].t[:, :], cst[j].t[:, :], [cst[j]], [cbf[j]])
                        DMA("pool", dst_ap, cbf[j].t[:, :], [cbf[j]], [dbuf])
            pcount = [0]
            kvit = 0
            for i in range(NOWN):
                Qm, Qd = qtm[i % 2], qtd[i % 2]
                DMA("sp", Qm.t[0:96, :, :], QTm[i], [dQTm[i]], [Qm])
                DMA("sp", Qd.t[:, :, :], QTd[i], [dQTd[i]], [Qd])
                H = hb[i % 2]
                DMA("sp", H.t[:, :], hown[i], [], [H])
                last = 2 * i + 1
                pend = []
                for kb in range(0, last + 1):
                    kq = kvit % NKV
                    kvit += 1
                    kvb = kvt[kq]
                    DMA("sp", kvb.t[:, :], KVD[kb], [dKTm[kb], dVm[kb], dKTd[kb], dVd[kb]], [kvb])

                    def _v(c0, a, b, kvb=kvb):
                        t_ = Tile(kvb.t[:, c0:c0 + a * b].rearrange("p (a b) -> p a b", a=a))
                        t_.b = kvb.b
                        return t_

                    Km, Vmt, Kd, Vdt = _v(0, 8, 128), _v(1024, 8, 65), _v(1544, 4, 128), _v(2056, 4, 129)
                    t = kb - 2 * i
                    first, fin = (kb == 0), (kb == last)
                    def mk_mla(half, t=t, first=first, fin=fin, Km=Km, Vmt=Vmt):
                        box = {}

                        def fS():
                            St = next_s()
                            for hh in range(4):
                                h = half * 4 + hh
                                sp_ = t in (0, 1)
                                MM(St.t[:, hh, :], Km.t[0:96, h, :], Qm.t[0:96, h, :], True, not sp_, [Km, Qm], [St])
                                if sp_:
                                    MM(St.t[:, hh, :], identb.t[:, :], mkb.t[:, t, :], False, True, [identb, mkb], [St])
                            P = ptm[pcount[0] % 4]
                            pcount[0] += 1
                            A(P.t[:, :, :], St.t[:, :, :], AF.Exp, [St], [P])
                            box["P"] = P

                        def fPV():
                            P = box["P"]
                            for hh in range(4):
                                h = half * 4 + hh
                                MM(AM[half].t[:, hh, :], P.t[:, hh, :], Vmt.t[:, h, :], first and hh == 0, fin and hh == 3,
                                   [P, Vmt], [AM[half]])
                        return fS, fPV

                    def mk_diff(t=t, first=first, fin=fin, Kd=Kd, Vdt=Vdt):
                        box = {}

                        def fS():
                            sp_ = t in (-1, 0, 1)
                            Sm = [next_s(), next_s()]
                            for h in range(4):
                                for m in range(2):
                                    MM(Sm[m].t[:, h, :], Kd.t[m * 64:(m + 1) * 64, h, :], Qd.t[m * 64:(m + 1) * 64, h, :],
                                       True, not sp_, [Kd, Qd], [Sm[m]])
                                    if sp_:
                                        MM(Sm[m].t[:, h, :], identb.t[:, :], bdh.t[:, (t + 1) * 4 + h, :], False, False,
                                           [identb, bdh], [Sm[m]])
                                        MM(Sm[m].t[:, h, :], identb.t[:, :], bdl.t[:, (t + 1) * 4 + h, :], False, True,
                                           [identb, bdl], [Sm[m]])
                            Pm = []
                            for m in range(2):
                                P = ptd[pcount[0] % 4]
                                pcount[0] += 1
                                A(P.t[:, :, :], Sm[m].t[:, :, :], AF.Exp, [Sm[m]], [P])
                                Pm.append(P)
                            box["Pm"] = Pm

                        def fPV():
                            Pm = box["Pm"]
                            for h in range(4):
                                for m in range(2):
                                    g = h * 2 + m
                                    MM(AD[g // 3].t[:, g % 3, :], Pm[m].t[:, h, :], Vdt.t[:, h, :], first and g in (0, 3, 6),
                                       fin and g in (2, 5, 7), [Pm[m], Vdt], [AD[g // 3]])
                        return fS, fPV

                    for stg in (mk_mla(0), mk_mla(1), mk_diff()):
                        stg[0]()
                        if pend:
                            pend.pop(0)()
                        pend.append(stg[1])
                while pend:
                    pend.pop(0)()
                for half in range(2):
                    dve(lambda e, half=half: e.reciprocal(out=rec.t[:, half * 4:half * 4 + 4],
                                                          in_=AM[half].t[:, :, 64]), [AM[half]], [rec])
                    TT(mixb.t[:, half * 256:(half + 1) * 256].rearrange("p (h e) -> p h e", h=4), AM[half].t[:, :, 0:64],
                       bc(rec.t[:, half * 4:half * 4 + 4].unsqueeze(2), [128, 4, 64]), ALU.mult, [AM[half], rec], [mixb])
                for a, n0, n in ((0, 0, 3), (1, 3, 3), (2, 6, 2)):
                    dve(lambda e, a=a, n0=n0, n=n: e.reciprocal(out=rec.t[:, 8 + n0:8 + n0 + n], in_=AD[a].t[:, :, 128]),
                        [AD[a]], [rec])
                    TT(od.t[:, n0:n0 + n, :], AD[a].t[:, :, 0:128], bc(rec.t[:, 8 + n0:8 + n0 + n].unsqueeze(2), [128, n, 128]),
                       ALU.mult, [AD[a], rec], [od])
                o4 = od.t[:, :, :].rearrange("p (h m) e -> p h m e", m=2)
                STT(odd.t[:, :, :], o4[:, :, 1, :], neglam.t[:, 0:1], o4[:, :, 0, :], ALU.mult, ALU.add, [od, neglam], [odd])
                TT(sq.t[:, :, :], odd.t[:, :, :], odd.t[:, :, :], ALU.mult, [odd], [sq])
                RED(s4.t[:, 0:4], sq.t[:, :, :], [sq], [s4])
                A(s4.t[:, 4:8], s4.t[:, 0:4], AF.Ln, [s4, epsc], [s4], scale=1.0 / 128, bias=epsc.t[:, 0:1])
                A(s4.t[:, 8:12], s4.t[:, 4:8], AF.Exp, [s4], [s4], scale=-0.5)
                TT(sq.t[:, :, :], odd.t[:, :, :], bc(s4.t[:, 8:12].unsqueeze(2), [128, 4, 128]), ALU.mult, [odd, s4], [sq])
                TT(mixb.t[:, 512:1024].rearrange("p (h e) -> p h e", h=4), sq.t[:, :, :],
                   bc(subl8.t[:, :].unsqueeze(1), [128, 4, 128]), ALU.mult, [sq, subl8], [mixb])
                Ta = next_s()
                Tv = Ta.t[:, :, :].rearrange("p a b -> p (a b)").bitcast(BF16).rearrange("p (c q) -> p c q", c=8)
                for c in range(8):
                    TR(Tv[:, c, :], mixb.t[:, c * 128:(c + 1) * 128], identb.t[:, :], [mixb, identb], [Ta])
                CP("act", mixT.t[:, :, :], Tv, [Ta], [mixT])
                Hh = h2[i % 2]
                for half in range(2):
                    Po = next_s()
                    Pf = Po.t[:, :, :].rearrange("p a b -> p (a b)")
                    for c in range(8):
                        MM(Pf, mixT.t[:, c, :], w_out_b.t[:, c, half * 512:(half + 1) * 512], c == 0, c == 7,
                           [mixT, w_out_b], [Po])
                    TT(Hh.t[:, half * 512:(half + 1) * 512], Pf, H.t[:, half * 512:(half + 1) * 512], ALU.add,
                       [Po, H], [Hh])
                DMA("sp", H2[i], Hh.t[:, :], [Hh], [dH2[i]])
            S.barrier()
            S.flush()

        for es in phase("C"):
          T = NOWN * 128
          E1T = sb(es, [128, T], BF16)
          E2T = sb(es, [128, T], BF16)
          GGT = sb(es, [128, T], BF16)
          iotaf = sb(es, [128, 128])
          S.op("pool", lambda e: e.iota(iotaf.t[:, :], pattern=[[1, 128]], base=0, channel_multiplier=0,
                                        allow_small_or_imprecise_dtypes=True), [], [iotaf])
          with ExitStack() as es1:
            es_outer = es
            es = es1
            S.defer = True
            stage = [sb(es, [128, 2048]) for _ in range(2)]
            w_q_b = load_w_bf16(es, w_query, 8, 2048, stage)
            skb = sb(es, [128, 16, 128], BF16)
            for c in range(16):
                st = stage[c % 2]
                DMA("sp", st.t[:, 0:128], skT[c], [], [st])
                CP("dve", skb.t[:, c, :], st.t[:, 0:128], [st], [skb])
            thr = sb(es, [128, 15])
            io16 = sb(es, [128, 16])
            for m in range(15):
                S.op("pool", lambda e, m=m: e.memset(thr.t[:, m:m + 1], 16.0 * (m + 1) - 0.5), [], [thr])
            for m in range(16):
                S.op("pool", lambda e, m=m: e.memset(io16.t[:, m:m + 1], float(m)), [], [io16])
            hh2 = [sb(es, [128, D]) for _ in range(2)]
            xn = [sb(es, [128, D]) for _ in range(2)]
            xb2 = [sb(es, [128, D], BF16) for _ in range(2)]
            xT2 = [sb(es, [128, 8, 128], BF16) for _ in range(2)]
            junk2_ = [sb(es, [128, D]) for _ in range(2)]
            st42 = [sb(es, [128, 4]) for _ in range(2)]
            qpT2 = [sb(es, [128, 16, 128], BF16) for _ in range(2)]
            sc2 = [sb(es, [128, 16, 128]) for _ in range(2)]
            t16 = sb(es, [128, 8, 2, 16])
            ix = sb(es, [128, 8, 2, 16], U32)
            ixf = sb(es, [128, 8, 2, 16])
            scr = sb(es, [128, 128])
            cand = sb(es, [128, 8, 256])
            scr2 = sb(es, [128, 256])
            best = sb(es, [128, 8, 16])
            fx = sb(es, [128, 8, 16], U32)
            fxf = sb(es, [128, 128])
            cmp = sb(es, [128, 128, 16])
            fi = sb(es, [128, 128])
            fj = sb(es, [128, 128])
            e1 = sb(es, [128, 128])
            e2 = sb(es, [128, 128])
            ge = sb(es, [128, 8, 16])
            gs = sb(es, [128, 16])
            gg = sb(es, [128, 128])
            TPx = ps(es, [128, 8, 128], BF16)
            QS = ps(es, [128, 16, 128])
            t16s = [[Tile(t16.t[:, h, p, :]) for p in range(2)] for h in range(8)]
            ixs = [[Tile(ix.t[:, h, p, :]) for p in range(2)] for h in range(8)]
            scrs = [[sb(es, [128, 128]) for p in range(2)] for h in range(8)]
            bests = [Tile(best.t[:, h, :]) for h in range(8)]
            fxs = [Tile(fx.t[:, h, :]) for h in range(8)]
            scr2s = [sb(es, [128, 256]) for h in range(8)]

            def stage1(i):
                k = i % 2
                xb, xT, junk, st4, qpT, sc = xb2[k], xT2[k], junk2_[k], st42[k], qpT2[k], sc2[k]
                Hh, X = hh2[k], xn[k]
                DMA("sp", Hh.t[:, :], H2[i], [dH2[i]], [Hh])
                A(junk.t[:, :], Hh.t[:, :], AF.Square, [Hh], [junk, st4], accum_out=st4.t[:, 0:1])
                A(st4.t[:, 1:2], st4.t[:, 0:1], AF.Ln, [st4, epsc], [st4], scale=1.0 / D, bias=epsc.t[:, 0:1])
                A(st4.t[:, 2:3], st4.t[:, 1:2], AF.Exp, [st4], [st4], scale=-0.5)
                STT(X.t[:, :], Hh.t[:, :], st4.t[:, 2:3], V("ffn_norm"), ALU.mult, ALU.mult, [Hh, st4, vec], [X])
                CP("pool", xb.t[:, :], X.t[:, :], [X], [xb])
                for c in range(8):
                    TR(TPx.t[:, c, :], xb.t[:, c * 128:(c + 1) * 128], identb.t[:, :], [xb, identb], [TPx])
                CP("act", xT.t[:, :, :], TPx.t[:, :, :], [TPx], [xT])
                for c in range(16):
                    for kc in range(8):
                        MM(QS.t[:, c, :], w_q_b.t[:, kc, c * 128:(c + 1) * 128], xT.t[:, kc, :], kc == 0, kc == 7,
                           [w_q_b, xT], [QS])
                CP("act", qpT.t[:, 0:8, :], QS.t[:, 0:8, :], [QS], [qpT])
                CP("dve", qpT.t[:, 8:16, :], QS.t[:, 8:16, :], [QS], [qpT])
                for c in range(16):
                    MM(QS.t[:, c, :], qpT.t[:, c, :], skb.t[:, c, :], True, True, [qpT, skb], [QS])
                CP("act", sc.t[:, 0:8, :], QS.t[:, 0:8, :], [QS], [sc])
                CP("dve", sc.t[:, 8:16, :], QS.t[:, 8:16, :], [QS], [sc])
                HP = [(h, p) for h in range(8) for p in range(2)]
                for h, p in HP:
                    dve(lambda e, h=h, p=p: e.max(out=t16.t[:, h, p, 0:8], in_=sc.t[:, 2 * h + p, :]), [sc], [t16s[h][p]])
                for h, p in HP:
                    dve(lambda e, h=h, p=p: e.match_replace(out=scrs[h][p].t[:, :], in_to_replace=t16.t[:, h, p, 0:8],
                                                            in_values=sc.t[:, 2 * h + p, :], imm_value=-1e30),
                        [sc, t16s[h][p]], [scrs[h][p]])
                for h, p in HP:
                    dve(lambda e, h=h, p=p: e.max(out=t16.t[:, h, p, 8:16], in_=scrs[h][p].t[:, :]), [scrs[h][p]], [t16s[h][p]])
                for h, p in HP:
                    dve(lambda e, h=h, p=p: e.max_index(out=ix.t[:, h, p, 0:8], in_max=t16.t[:, h, p, 0:8],
                                                        in_values=sc.t[:, 2 * h + p, :]), [sc, t16s[h][p]], [ixs[h][p]])
                for h, p in HP:
                    dve(lambda e, h=h, p=p: e.max_index(out=ix.t[:, h, p, 8:16], in_max=t16.t[:, h, p, 8:16],
                                                        in_values=sc.t[:, 2 * h + p, :]), [sc, t16s[h][p]], [ixs[h][p]])
                all_t16 = [t16s[h][p] for h, p in HP]
                all_ix = [ixs[h][p] for h, p in HP]
                CP("dve", ixf.t[:, :, :, :], ix.t[:, :, :, :], all_ix, [ixf])
                TT(cand.t[:, :, :].rearrange("p h (a b) -> p h a b", a=16), bc(t16.t[:, :, 0, :].unsqueeze(3), [128, 8, 16, 16]),
                   bc(t16.t[:, :, 1, :].unsqueeze(2), [128, 8, 16, 16]), ALU.add, all_t16, [cand])
                for h in range(8):
                    dve(lambda e, h=h: e.max(out=best.t[:, h, 0:8], in_=cand.t[:, h, :]), [cand], [bests[h]])
                for h in range(8):
                    dve(lambda e, h=h: e.match_replace(out=scr2s[h].t[:, :], in_to_replace=best.t[:, h, 0:8],
                                                       in_values=cand.t[:, h, :], imm_value=-1e30), [cand, bests[h]], [scr2s[h]])
                for h in range(8):
                    dve(lambda e, h=h: e.max(out=best.t[:, h, 8:16], in_=scr2s[h].t[:, :]), [scr2s[h]], [bests[h]])
                for h in range(8):
                    dve(lambda e, h=h: e.max_index(out=fx.t[:, h, 0:8], in_max=best.t[:, h, 0:8],
                                                   in_values=cand.t[:, h, :]), [cand, bests[h]], [fxs[h]])
                for h in range(8):
                    dve(lambda e, h=h: e.max_index(out=fx.t[:, h, 8:16], in_max=best.t[:, h, 8:16],
                                                   in_values=cand.t[:, h, :]), [cand, bests[h]], [fxs[h]])
                best_all = bests
                CP("dve", fxf.t[:, :], fx.t[:, :, :].rearrange("p h k -> p (h k)"), fxs, [fxf])
                TT(cmp.t[:, :, 0:15], bc(fxf.t[:, :].unsqueeze(2), [128, 128, 15]), bc(thr.t[:, :].unsqueeze(1), [128, 128, 15]),
                   ALU.is_ge, [fxf, thr], [cmp])
                RED(fi.t[:, :], cmp.t[:, :, 0:15], [cmp], [fi])
                STT(fj.t[:, :], fi.t[:, :], -16.0, fxf.t[:, :], ALU.mult, ALU.add, [fi, fxf], [fj])
                c4 = cmp.t[:, :, :].rearrange("p (h k) i -> p h k i", h=8)
                for (fsel, pidx, eo) in ((fi, 0, e1), (fj, 1, e2)):
                    TT(cmp.t[:, :, :], bc(io16.t[:, :].unsqueeze(1), [128, 128, 16]), bc(fsel.t[:, :].unsqueeze(2), [128, 128, 16]),
                       ALU.is_equal, [io16, fsel], [cmp])
                    TT(c4, c4, bc(ixf.t[:, :, pidx, :].unsqueeze(2), [128, 8, 16, 16]), ALU.mult, [cmp, ixf], [cmp])
                    RED(eo.t[:, :], cmp.t[:, :, :], [cmp], [eo])
                TT(ge.t[:, :, :], best.t[:, :, :], bc(best.t[:, :, 0:1], [128, 8, 16]), ALU.subtract, bests, [ge])
                A(ge.t[:, :, :], ge.t[:, :, :], AF.Exp, [ge], [ge])
                RED(gs.t[:, 0:8], ge.t[:, :, :], [ge], [gs])
                dve(lambda e: e.reciprocal(out=gs.t[:, 8:16], in_=gs.t[:, 0:8]), [gs], [gs])
                TT(gg.t[:, :].rearrange("p (h k) -> p h k", h=8), ge.t[:, :, :], bc(gs.t[:, 8:16].unsqueeze(2), [128, 8, 16]),
                   ALU.mult, [ge, gs], [gg])


            for i in range(NOWN):
                stage1(i)
                DMA("sp", XT[i], xT2[i % 2].t[:, :, :], [xT2[i % 2]], [dXT[i]])
                for j, (srcT, dstT) in enumerate(((e1, E1T), (e2, E2T), (gg, GGT))):
                    MM(QS.t[:, j, :], srcT.t[:, :], ident.t[:, :], True, True, [srcT, ident], [QS])
                    CP("act", dstT.t[:, i * 128:(i + 1) * 128], QS.t[:, j, :], [QS], [dstT])
            S.barrier()
            S.flush()
            es = es_outer
          S.defer = True
          GB = 3
          NGRP = 0 if skip_c2 else -(-NOWN // GB)
          Wg = sb(es, [128, 128, GB * 128], BF16)
          xg = sb(es, [128, 8, GB * 128], BF16)
          hg = [sb(es, [128, D]) for _ in range(GB)]
          ust = [sb(es, [128, 8, 512], BF16) for _ in range(2)]
          vst = [sb(es, [128, 4, 1024], BF16) for _ in range(2)]
          gl = [sb(es, [128, GB * 128], BF16) for _ in range(3)]
          wa = [sb(es, [128, GB * 128], BF16) for _ in range(3)]
          At = [sb(es, [128, 128], BF16) for _ in range(4)]
          Bt = [sb(es, [128, 128], BF16) for _ in range(4)]
          yo = sb(es, [128, D])
          ACC = [ps(es, [128, D]) for _ in range(GB)]
          BK = [ps(es, [128, 512]) for _ in range(2)]
          ldn = [0]
          for grp in range(NGRP):
              blks = list(range(grp * GB, min(NOWN, (grp + 1) * GB)))
              nb_ = len(blks)
              G = nb_ * 128
              for j, bi in enumerate(blks):
                  DMA("sp", xg.t[:, :, j * 128:(j + 1) * 128], XT[bi], [dXT[bi]], [xg])
                  DMA("sp", hg[j].t[:, :], H2[bi], [dH2[bi]], [hg[j]])
              for t0 in range(0, G, 4):
                  bk = BK[(t0 // 4) % 2]
                  bkv = bk.t[:, :].rearrange("p (a b) -> p a b", a=4)
                  for tt in range(4):
                      tg = grp * GB * 128 + t0 + tt
                      a_, b_ = At[(t0 + tt) % 4], Bt[(t0 + tt) % 4]
                      TS(a_.t[:, :], iotaf.t[:, :], E1T.t[:, tg:tg + 1], GGT.t[:, tg:tg + 1], ALU.is_equal, ALU.mult,
                         [iotaf, E1T, GGT], [a_])
                      TS(b_.t[:, :], iotaf.t[:, :], E2T.t[:, tg:tg + 1], None, ALU.is_equal, None, [iotaf, E2T], [b_])
                      MM(bkv[:, tt, :], b_.t[:, :], a_.t[:, :], True, True, [a_, b_], [bk])
                  CP("act", Wg.t[:, :, t0:t0 + 4].rearrange("p n t -> p t n"), bkv, [bk], [Wg])
              def load_w(gq):
                  DMA("sp", ust[gq % 2].t[:, :, :], UB[gq], [dUB[gq]], [ust[gq % 2]])
                  DMA("pool", vst[gq % 2].t[:, :, :], VB[gq], [dVB[gq]], [vst[gq % 2]])

              def h_mm(c):
                  gq, cc = divmod(c, 4)
                  k_ = gq % 2
                  bk = BK[c % 2]
                  for kc in range(8):
                      MM(bk.t[:, 0:G], ust[k_].t[:, kc, cc * 128:(cc + 1) * 128], xg.t[:, kc, 0:G], kc == 0, kc == 7,
                         [ust[k_], xg], [bk])
                  A(gl[c % 3].t[:, 0:G], bk.t[:, 0:G], AF.Gelu, [bk], [gl[c % 3]])
                  TT(wa[c % 3].t[:, 0:G], gl[c % 3].t[:, 0:G], Wg.t[:, c, 0:G], ALU.mult, [gl[c % 3], Wg], [wa[c % 3]])
                  return k_

              def v_mm(c, k_):
                  cc = c % 4
                  for j in range(nb_):
                      for half in range(2):
                          MM(ACC[j].t[:, half * 512:(half + 1) * 512], wa[c % 3].t[:, j * 128:(j + 1) * 128],
                             vst[k_].t[:, cc, half * 512:(half + 1) * 512], c == 0, c == 127, [wa[c % 3], vst[k_]], [ACC[j]])

              load_w(0)
              load_w(1)
              pendv = []
              for c in range(128):
                  k_ = h_mm(c)
                  pendv.append((c, k_))
                  if len(pendv) > 2:
                      v_mm(*pendv.pop(0))
                  if c % 4 == 1 and c > 4 and c // 4 + 1 < 32:
                      load_w(c // 4 + 1)
              while pendv:
                  v_mm(*pendv.pop(0))
              for j, bi in enumerate(blks):
                  TT(yo.t[:, :], ACC[j].t[:, :], hg[j].t[:, :], ALU.add, [ACC[j], hg[j]], [yo])
                  DMA("sp", y[bi], yo.t[:, :], [yo], [])
          S.barrier()
          S.flush()
    return nc


def _t5_bucket_np(n):
    n = np.maximum(n, 0)
    nf = np.maximum(n, 1).astype(np.float32)
    large = 16 + (np.log(nf / 16) / math.log(128 / 16) * 16).astype(np.int32)
    large = np.minimum(large, 31)
    return np.where(n < 16, n, large)


def _bias_index_tables():
    k = np.arange(128)[:, None]
    q = np.arange(128)[None, :]
    diag = np.where(k <= q, _t5_bucket_np(q - k), 32)
    sub = _t5_bucket_np(128 + q - k)
    far = np.full((128, 128), 31)
    allm = np.full((128, 128), 32)
    return {"diag": diag, "sub": sub, "far": far, "allm": allm}


def prepare_inputs(inputs, NOWN, seq_pad_blocks=None):
    x = np.asarray(inputs["x"], np.float32)
    Bn, Sn, _ = x.shape
    NKB = 2 * NOWN
    L = NKB * 128
    meta = np.asarray(inputs["meta_tokens"], np.float32)
    rel_bias = np.asarray(inputs["rel_bias"], np.float32)
    half = 16
    inv_freq = (10000.0 ** (-np.arange(half, dtype=np.float32) / half)).astype(np.float32)
    pos = np.arange(L, dtype=np.float32)
    ang = pos[:, None] * inv_freq[None, :]
    cs_all = np.concatenate([np.cos(ang), np.sin(ang)], axis=1).astype(np.float32)

    def g(name):
        return np.asarray(inputs[name], np.float32)[0]

    vec_parts = {
        "attn_norm": g("attn_norm"), "ffn_norm": g("ffn_norm"), "q_norm": g("mla_q_norm"), "kv_norm": g("mla_kv_norm"),
        "gq": g("mla_qk_norm_q"), "gk": g("mla_qk_norm_k"), "gdq": g("diff_q_norm"), "gdk": g("diff_k_norm"),
        "lq1": g("diff_lambda_q1"), "lk1": g("diff_lambda_k1"), "lq2": g("diff_lambda_q2"), "lk2": g("diff_lambda_k2"),
        "subln": g("diff_subln"), "b31": rel_bias[31, :],
    }
    vecs = np.concatenate([vec_parts[n] for n, _ in VEC_LAYOUT])[None, :].astype(np.float32)
    tabs = _bias_index_tables()
    ext = np.concatenate([rel_bias, np.full((1, 4), NEG, np.float32)], axis=0)
    zero_neg = np.array([0.0, NEG], np.float32)
    tri = zero_neg[(tabs["diag"] == 32).astype(np.int64)]
    allneg = zero_neg[np.ones((128, 128), np.int64)]
    zeros = zero_neg[np.zeros((128, 128), np.int64)]
    common = {
        "w_in": g("w_in").reshape(8, 128, IN_W), "w_uq": g("mla_w_uq").reshape(2, 128, 768),
        "w_ukv": g("mla_w_ukv").reshape(2, 128, 1024), "w_out": g("w_out").reshape(8, 128, 1024),
        "w_query": g("peer_w_query").reshape(8, 128, 2048),
        "skT": np.ascontiguousarray(g("peer_sub_keys").reshape(16, 128, 128).transpose(0, 2, 1)),
        "peer_uT": np.ascontiguousarray(g("peer_u").T).reshape(8, 128, 16384), "peer_v": g("peer_v"), "vecs": vecs,
    }
    in_maps = []
    for b in range(Bn):
        hfull = np.zeros((L, D), np.float32)
        hfull[:NMETA] = meta
        hfull[NMETA:NMETA + Sn] = x[b]
        hseq = hfull.reshape(NKB, 128, D)
        for par in range(2):
            own = np.arange(NOWN) * 2 + par
            if par == 0:
                types = ["sub", "diag", "allm"]
                mm = np.stack([tri, allneg])
            else:
                types = ["far", "sub", "diag"]
                mm = np.stack([zeros, tri])
            bd = np.stack([np.stack([ext[tabs[t], h] for h in range(4)]) for t in types]).astype(np.float32)
            m = dict(common)
            m.update({
                "hseq": hseq, "hown": np.ascontiguousarray(hseq[own]),
                "csk": cs_all.reshape(NKB, 128, 32), "csq": np.ascontiguousarray(cs_all.reshape(NKB, 128, 32)[own]),
                "maskm": mm.astype(np.float32), "biasd": bd,
            })
            in_maps.append(m)
    return in_maps


def assemble(results, Bn, Sn, NOWN):
    NKB = 2 * NOWN
    out = np.zeros((Bn, NKB * 128, D), np.float32)
    for b in range(Bn):
        for par in range(2):
            yv = np.asarray(results[b * 2 + par]["y"]).reshape(NOWN, 128, D)
            full = out[b].reshape(NKB, 128, D)
            full[par::2] = yv
    return np.ascontiguousarray(out[:, NMETA:NMETA + Sn])


_NC_CACHE = {}


def kernel(**inputs):
    x = np.asarray(inputs["x"])
    Bn, Sn, _ = x.shape
    nblocks = -(-(NMETA + Sn) // 128)
    NOWN = (nblocks + 1) // 2
    if NOWN not in _NC_CACHE:
        _NC_CACHE[NOWN] = build(NOWN)
    nc = _NC_CACHE[NOWN]
    in_maps = prepare_inputs(inputs, NOWN)
    res = run_bass_kernel_spmd(nc, in_maps, core_ids=list(range(len(in_maps))))
    return assemble(res.results, Bn, Sn, NOWN).astype(np.float32)
```

```python
import math
from contextlib import ExitStack

import numpy as np
import concourse.bass as bass
import concourse.mybir as mybir
from concourse.bass_utils import run_bass_kernel_spmd

F32 = mybir.dt.float32
BF16 = mybir.dt.bfloat16
U32 = mybir.dt.uint32
AF = mybir.ActivationFunctionType
ALU = mybir.AluOpType
AX = mybir.AxisListType

D = 1024
NMETA = 16
EPS = 1e-6
LAMBDA_INIT = 0.2
NEG = -30000.0
IN_W = 2080

VEC_LAYOUT = [("attn_norm", 1024), ("ffn_norm", 1024), ("q_norm", 256), ("kv_norm", 256),
              ("gq", 96), ("gk", 96), ("gdq", 64), ("gdk", 64), ("lq1", 64), ("lk1", 64),
              ("lq2", 64), ("lk2", 64), ("subln", 128), ("b31", 4)]
VOFF = {}
_o = 0
for _n, _l in VEC_LAYOUT:
    VOFF[_n] = (_o, _o + _l)
    _o += _l
NVEC = _o


class Buf:
    __slots__ = ("w", "r")

    def __init__(self):
        self.w = None
        self.r = {}


class Tile:
    __slots__ = ("t", "b")

    def __init__(self, t):
        self.t = t
        self.b = Buf()


class Sched:
    ENG = ("pe", "act", "dve", "pool", "sp")

    def __init__(self, nc, es):
        self.nc = nc
        self.es = es
        self.sems = []
        self.owner = []
        self.prog = {e: [] for e in self.ENG}
        self.cnt = {e: 0 for e in self.ENG}
        self.esem = {}
        self.seen = {e: {} for e in self.ENG}
        for e in self.ENG:
            self._new_epoch(e)
        self.defer = False
        self.pending = []
        self.dq = {}
        for e, k in (("sp", 16), ("pool", 16), ("act", 4)):
            self.dq[e] = {"sems": [self._sem(None) for _ in range(k)], "cnt": [0] * k, "n": 0}

    def _sem(self, owner):
        s = self.es.enter_context(self.nc.semaphore(f"sm{len(self.sems)}"))
        self.sems.append(s)
        self.owner.append(owner)
        return len(self.sems) - 1

    def _new_epoch(self, e):
        self.esem[e] = self._sem(e)
        self.cnt[e] = 0

    def _waits(self, e, reads, writes):
        need = {}

        def add(s, v, war=False):
            if self.owner[s] == e and e == "pe":
                return
            if need.get(s, 0) < v:
                need[s] = v

        for b in reads:
            if b.w is not None:
                add(*b.w)
        for b in writes:
            if b.w is not None:
                add(*b.w)
            for s, v in b.r.items():
                add(s, v, True)
        out = []
        seen = self.seen[e]
        for s, v in need.items():
            if seen.get(s, 0) >= v:
                continue
            seen[s] = v
            out.append((s, v))
        return out

    def _book(self, ev, reads, writes):
        s, v = ev
        for b in reads:
            if b.r.get(s, 0) < v:
                b.r[s] = v
        for b in writes:
            b.w = ev
            b.r = {}

    def op(self, e, fn, reads=(), writes=(), dur=0.3):
        reads = [x.b if isinstance(x, Tile) else x for x in reads]
        writes = [x.b if isinstance(x, Tile) else x for x in writes]
        if self.defer:
            self.pending.append(("op", e, fn, reads, writes, dur, dur))
            return
        w = self._waits(e, reads, writes)
        if self.cnt[e] >= 60000:
            self._new_epoch(e)
        self.cnt[e] += 1
        ev = (self.esem[e], self.cnt[e])
        self.prog[e].append((w, fn, ev[0], 1))
        self._book(ev, reads, writes)

    def dma(self, e, fn, reads=(), writes=(), dur=3.0):
        reads = [x.b if isinstance(x, Tile) else x for x in reads]
        writes = [x.b if isinstance(x, Tile) else x for x in writes]
        if self.defer:
            self.pending.append(("dma", e, fn, reads, writes, 1.0 if e == "pool" else 0.35, dur))
            return
        q = self.dq[e]
        k = q["n"] % len(q["sems"])
        q["n"] += 1
        s = q["sems"][k]
        w = self._waits(e, reads, writes)
        c = q["cnt"][k]
        if c > 0 and self.seen[e].get(s, 0) < c:
            self.seen[e][s] = c
            w.append((s, c))
        q["cnt"][k] = c + 16
        ev = (s, c + 16)
        self.prog[e].append((w, fn, s, 16))
        self._book(ev, reads, writes)

    def reorder(self):
        import heapq
        ops = self.pending
        self.pending = []
        self.defer = False
        n = len(ops)
        lastw, readers = {}, {}
        deps = [None] * n
        succ = [[] for _ in range(n)]
        for i, (_, e, fn, reads, writes, busy, lat) in enumerate(ops):
            d = set()
            for b in reads:
                if id(b) in lastw:
                    d.add(lastw[id(b)])
            for b in writes:
                if id(b) in lastw:
                    d.add(lastw[id(b)])
                d.update(readers.get(id(b), ()))
            d.discard(i)
            for b in reads:
                readers.setdefault(id(b), []).append(i)
            for b in writes:
                lastw[id(b)] = i
                readers[id(b)] = []
            deps[i] = len(d)
            for j in d:
                succ[j].append(i)
        ready_t = [0.0] * n
        heaps = {e: [] for e in self.ENG}
        for i in range(n):
            if deps[i] == 0:
                heapq.heappush(heaps[ops[i][1]], (0.0, i))
        free = {e: 0.0 for e in self.ENG}
        order = []
        done = 0
        while done < n:
            best = None
            for e in self.ENG:
                h = heaps[e]
                if not h:
                    continue
                rt, i = h[0]
                st = max(rt, free[e])
                if best is None or (st, i) < (best[0], best[2]):
                    best = (st, e, i)
            st, e, i = best
            h = heaps[e]
            cand = []
            while h and h[0][0] <= st:
                cand.append(heapq.heappop(h))
            cand.sort(key=lambda x: x[1])
            rt, i = cand[0]
            for c in cand[1:]:
                heapq.heappush(h, c)
            busy, lat = ops[i][5], ops[i][6]
            free[e] = st + busy
            fin = st + lat
            order.append((st, i))
            done += 1
            for j in succ[i]:
                deps[j] -= 1
                t_ = fin + (0.0 if ops[j][1] == e else 0.3)
                if t_ > ready_t[j]:
                    ready_t[j] = t_
                if deps[j] == 0:
                    heapq.heappush(heaps[ops[j][1]], (ready_t[j], j))
        order.sort()
        for _, i in order:
            kind, e, fn, reads, writes, busy, lat = ops[i]
            if kind == "op":
                self.op(e, fn, reads, writes)
            else:
                self.dma(e, fn, reads, writes)

    def barrier(self):
        if self.pending:
            self.reorder()
        evs = []
        for e in self.ENG:
            if self.cnt[e] > 0:
                evs.append((self.esem[e], self.cnt[e]))
        for q in self.dq.values():
            for s, c in zip(q["sems"], q["cnt"]):
                if c > 0:
                    evs.append((s, c))
        for e in self.ENG:
            w = []
            for s, v in evs:
                if self.owner[s] == e:
                    continue
                if self.seen[e].get(s, 0) >= v:
                    continue
                self.seen[e][s] = v
                w.append((s, v))
            if w:
                self.prog[e].append((w, None, None, 0))

    def _run(self, lst, eng):
        for w, fn, s, inc in lst:
            for sw, v in w:
                eng.wait_ge(self.sems[sw], v)
            if fn is not None:
                fn(eng).then_inc(self.sems[s], inc)

    def flush(self):
        progs = self.prog
        self.prog = {e: [] for e in self.ENG}
        with self.nc.Block() as block:
            @block.tensor
            def _(eng):
                self._run(progs["pe"], eng)

            @block.scalar
            def _(eng):
                self._run(progs["act"], eng)

            @block.vector
            def _(eng):
                self._run(progs["dve"], eng)

            @block.gpsimd
            def _(eng):
                self._run(progs["pool"], eng)

            @block.sync
            def _(eng):
                self._run(progs["sp"], eng)


def bc(ap, shape):
    return ap.to_broadcast(list(shape))


def build(NOWN, debug=False, phases="ABC", skip_c2=False):
    NKB = 2 * NOWN
    nc = bass.Bass("TRN2", target_bir_lowering=False)

    def din(name, shape, dt=F32):
        return nc.dram_tensor(name, list(shape), dt, kind="ExternalInput")

    skind = "ExternalOutput" if debug else "Internal"

    def dsc(name, shape, dt):
        return nc.dram_tensor(name, list(shape), dt, kind=skind)

    hseq = din("hseq", [NKB, 128, D])
    hown = din("hown", [NOWN, 128, D])
    csk = din("csk", [NKB, 128, 32])
    csq = din("csq", [NOWN, 128, 32])
    w_in = din("w_in", [8, 128, IN_W])
    w_uq = din("w_uq", [2, 128, 768])
    w_ukv = din("w_ukv", [2, 128, 1024])
    w_out = din("w_out", [8, 128, 1024])
    w_query = din("w_query", [8, 128, 2048])
    skT = din("skT", [16, 128, 128])
    uT_d = din("peer_uT", [8, 128, 16384])
    v_d = din("peer_v", [16384, D])
    UB = dsc("UB", [32, 128, 8, 512], BF16)
    VB = dsc("VB", [32, 128, 4, 1024], BF16)
    XT = dsc("XT", [NOWN, 128, 8, 128], BF16)
    dUB, dVB = ([Buf() for _ in range(32)] for _ in range(2))
    dXT = [Buf() for _ in range(NOWN)]
    vecs = din("vecs", [1, NVEC])
    maskm = din("maskm", [2, 128, 128])
    biasd = din("biasd", [3, 4, 128, 128])
    y = nc.dram_tensor("y", [NOWN, 128, D], F32, kind="ExternalOutput")

    KVW = 1024 + 520 + 512 + 516
    KVD = dsc("KV", [NKB, 128, KVW], BF16)

    class _Sub:
        def __init__(self, c0, shape, p=128):
            self.c0, self.shape, self.p = c0, shape, p

        def __getitem__(self, blk):
            a, b = self.shape
            return KVD[blk, 0:self.p, self.c0:self.c0 + a * b].rearrange("p (a b) -> p a b", a=a)

    KTm, Vm, KTd, Vd = _Sub(0, (8, 128), 96), _Sub(1024, (8, 65)), _Sub(1544, (4, 128)), _Sub(2056, (4, 129))
    QTm = dsc("QTm", [NOWN, 96, 8, 128], BF16)
    QTd = dsc("QTd", [NOWN, 128, 4, 128], BF16)
    H2 = dsc("H2", [NOWN, 128, D], F32)
    dKTm, dVm, dKTd, dVd = ([Buf() for _ in range(NKB)] for _ in range(4))
    dQTm, dQTd, dH2 = ([Buf() for _ in range(NOWN)] for _ in range(3))

    with ExitStack() as ges:
        S = Sched(nc, ges)
        ncnt = [0]

        def sb(es, shape, dt=F32):
            ncnt[0] += 1
            return Tile(es.enter_context(nc.sbuf_tensor(f"t{ncnt[0]}", list(shape), dt)))

        def ps(es, shape, dt=F32):
            ncnt[0] += 1
            per_bank = 512 if dt == F32 else 1024
            n = int(np.prod(shape[1:]))
            nb_ = -(-n // per_bank)
            t = es.enter_context(nc.psum_tensor(f"p{ncnt[0]}", [128, nb_ * per_bank], dt))
            v = t[:, 0:n]
            if len(shape) == 3:
                v = v.rearrange("p (a b) -> p a b", a=shape[1])
            return Tile(v)

        def phase(name):
            if name in phases:
                with ExitStack() as pes:
                    yield pes

        def nel(ap):
            n = 1
            for s_ in ap.shape[1:]:
                n *= int(s_)
            return n

        def act(fn, r, w, dur=0.3):
            S.op("act", fn, r, w, dur)

        def dve(fn, r, w, dur=0.3):
            S.op("dve", fn, r, w, dur)

        def pe(fn, r, w, dur=0.15):
            S.op("pe", fn, r, w, dur)

        def A(out, in_, func, r, w, **kw):
            act(lambda e: e.activation(out=out, in_=in_, func=func, **kw), r, w, 0.25 + nel(out) * 0.00075)

        def TT(out, in0, in1, op, r, w, eng="dve"):
            S.op(eng, lambda e: e.tensor_tensor(out=out, in0=in0, in1=in1, op=op), r, w, 0.12 + nel(out) * 0.0016)

        def TS(out, in0, s1, s2, op0, op1, r, w, eng="dve"):
            if op1 is None:
                S.op(eng, lambda e: e.tensor_scalar(out=out, in0=in0, scalar1=s1, scalar2=None, op0=op0), r, w,
                     0.12 + nel(out) * 0.001)
            else:
                S.op(eng, lambda e: e.tensor_scalar(out=out, in0=in0, scalar1=s1, scalar2=s2, op0=op0, op1=op1), r, w,
                     0.12 + nel(out) * 0.001)

        def STT(out, in0, sc, in1, op0, op1, r, w, accum=None):
            if accum is None:
                dve(lambda e: e.scalar_tensor_tensor(out=out, in0=in0, scalar=sc, in1=in1, op0=op0, op1=op1), r, w,
                    0.12 + nel(out) * 0.0016)
            else:
                dve(lambda e: e.scalar_tensor_tensor(out=out, in0=in0, scalar=sc, in1=in1, op0=op0, op1=op1,
                                                     accum_out=accum), r, w, 0.25 + nel(out) * 0.0016)

        def RED(out, in_, r, w):
            dve(lambda e: e.tensor_reduce(out=out, in_=in_, axis=AX.X, op=ALU.add), r, w, 0.12 + nel(in_) * 0.00105)

        def CP(eng, out, in_, r, w):
            if eng == "act":
                act(lambda e: e.copy(out=out, in_=in_), r, w, 0.25 + nel(out) * 0.00075)
            else:
                S.op(eng, lambda e: e.tensor_copy(out=out, in_=in_), r, w,
                     (0.12 + nel(out) * 0.00105) if eng == "dve" else (0.3 + nel(out) * 0.0006))

        def MM(out, lhsT, rhs, start, stop, r, w):
            pe(lambda e: e.matmul(out, lhsT, rhs, start=start, stop=stop), r, w, 0.1 + nel(rhs) * 0.00052)

        def TR(out, in_, ident, r, w):
            pe(lambda e: e.transpose(out, in_, ident), r, w, 0.2)

        def DMA(eng, out, in_, r, w):
            S.dma(eng, lambda e: e.dma_start(out=out, in_=in_), r, w, 2.5 + nel(out) * 128 * 4 / 1.0e5)

        vec = sb(ges, [128, NVEC])
        ident = sb(ges, [128, 128])
        identb = sb(ges, [128, 128], BF16)
        epsc = sb(ges, [128, 1])
        gqs = sb(ges, [128, 96])
        gdqs = sb(ges, [128, 64])
        subl8 = sb(ges, [128, 128])
        neglam = sb(ges, [128, 1])
        ltmp = sb(ges, [128, 64])
        lsc = sb(ges, [128, 4])

        def V(name):
            a, b = VOFF[name]
            return vec.t[:, a:b]

        DMA("sp", vec.t[:, :], vecs[0:1, :].to_broadcast([128, NVEC]), [], [vec])
        S.op("pool", lambda e: e.memset(ident.t[:, :], 0.0), [], [ident])
        S.op("pool", lambda e: e.affine_select(out=ident.t[:, :], in_=ident.t[:, :], pattern=[[-1, 128]],
                                               compare_op=ALU.not_equal, fill=1.0, base=0, channel_multiplier=1),
             [ident], [ident])
        CP("dve", identb.t[:, :], ident.t[:, :], [ident], [identb])
        S.op("pool", lambda e: e.memset(epsc.t[:, :], EPS), [], [epsc])
        TS(gqs.t[:, :], V("gq"), 96.0 ** -0.5, None, ALU.mult, None, [vec], [gqs])
        TS(gdqs.t[:, :], V("gdq"), 0.125, None, ALU.mult, None, [vec], [gdqs])
        TS(subl8.t[:, :], V("subln"), 1.0 - LAMBDA_INIT, None, ALU.mult, None, [vec], [subl8])
        STT(ltmp.t[:, :], V("lq1"), 1.0, V("lk1"), ALU.mult, ALU.mult, [vec], [ltmp, lsc], accum=lsc.t[:, 0:1])
        STT(ltmp.t[:, :], V("lq2"), 1.0, V("lk2"), ALU.mult, ALU.mult, [vec], [ltmp, lsc], accum=lsc.t[:, 1:2])
        A(lsc.t[:, 2:4], lsc.t[:, 0:2], AF.Exp, [lsc], [lsc])
        TT(neglam.t[:, :], lsc.t[:, 3:4], lsc.t[:, 2:3], ALU.subtract, [lsc], [neglam])
        TS(neglam.t[:, :], neglam.t[:, :], -LAMBDA_INIT, None, ALU.add, None, [neglam], [neglam])

        def load_w_bf16(es, dram, nchunk, ncol, stage_pool):
            wt = sb(es, [128, nchunk, ncol], BF16)
            for c in range(nchunk):
                st = stage_pool[c % len(stage_pool)]
                DMA("sp", st.t[:, 0:ncol], dram[c], [], [st])
                if c % 2 == 0:
                    CP("dve", wt.t[:, c, :], st.t[:, 0:ncol], [st], [wt])
                else:
                    CP("pool", wt.t[:, c, :], st.t[:, 0:ncol], [st], [wt])
            return wt

        for es in phase("A"):
            S.defer = True
            stage = [sb(es, [128, IN_W]) for _ in range(2)]
            w_in_b = load_w_bf16(es, w_in, 8, IN_W, stage)
            w_uq_b = load_w_bf16(es, w_uq, 2, 768, stage)
            w_ukv_b = load_w_bf16(es, w_ukv, 2, 1024, stage)

            NB_ = 3
            hb = [sb(es, [128, D]) for _ in range(NB_)]
            cs = [sb(es, [128, 32]) for _ in range(NB_)]
            junkA = [sb(es, [128, D]) for _ in range(NB_)]
            st4 = [sb(es, [128, 8]) for _ in range(NB_)]
            nb = [sb(es, [128, D], BF16) for _ in range(NB_)]
            nT = [sb(es, [128, 8, 128], BF16) for _ in range(NB_)]
            cn = [sb(es, [128, 256], BF16) for _ in range(NB_)]
            cT = [sb(es, [128, 2, 128], BF16) for _ in range(NB_)]
            kvs = [sb(es, [128, 1024]) for _ in range(NB_)]
            dks = [sb(es, [128, 512]) for _ in range(NB_)]
            pas = [sb(es, [128, 288]) for _ in range(NB_)]
            sqA = [sb(es, [128, 1024]) for _ in range(NB_)]
            s8 = [sb(es, [128, 32]) for _ in range(NB_)]
            krg = [sb(es, [128, 8, 32]) for _ in range(NB_)]
            rr = [sb(es, [128, 8, 32]) for _ in range(NB_)]
            kk = [sb(es, [128, 8, 96], BF16) for _ in range(NB_)]
            kd = [sb(es, [128, 512], BF16) for _ in range(NB_)]
            tmp8A = [sb(es, [128, 8, 64]) for _ in range(NB_)]
            ktm = [sb(es, [128, 8, 128], BF16) for _ in range(NB_)]
            ktd = [sb(es, [128, 4, 128], BF16) for _ in range(NB_)]
            vv = [sb(es, [128, 8, 65], BF16) for _ in range(NB_)]
            vd = [sb(es, [128, 4, 129], BF16) for _ in range(NB_)]
            for t in ktm:
                S.op("pool", lambda e, t=t: e.memset(t.t[:, :, :], 0.0), [], [t])
            for t in vv:
                S.op("pool", lambda e, t=t: e.memset(t.t[:, :, 64:65], 1.0), [], [t])
            for t in vd:
                S.op("pool", lambda e, t=t: e.memset(t.t[:, :, 128:129], 1.0), [], [t])

            TP = ps(es, [128, 8, 128], BF16)
            TK = ps(es, [128, 16, 128], BF16)
            PA = ps(es, [128, 512])
            PB = ps(es, [128, 512])
            PC = ps(es, [128, 512])
            PD = ps(es, [128, 1024])

            def rstd_of(out, ss, dim, rbufs, wbuf, tmp):
                A(tmp, ss, AF.Ln, rbufs + [epsc], [wbuf], scale=1.0 / dim, bias=epsc.t[:, 0:1])
                A(out, tmp, AF.Exp, [wbuf], [wbuf], scale=-0.5)

            def p1(kside, blk, it):
                k = it % NB_
                junk, sq, tmp8 = junkA[k], sqA[k], tmp8A[k]
                H, C = hb[k], cs[k]
                src = hseq[blk] if kside else hown[blk]
                DMA("sp", H.t[:, :], src, [], [H])
                DMA("sp", C.t[:, :], (csk if kside else csq)[blk], [], [C])
                st = st4[k]
                A(junk.t[:, :], H.t[:, :], AF.Square, [H], [junk, st], accum_out=st.t[:, 0:1])
                rstd_of(st.t[:, 2:3], st.t[:, 0:1], D, [st], st, st.t[:, 1:2])
                STT(nb[k].t[:, :], H.t[:, :], st.t[:, 2:3], V("attn_norm"), ALU.mult, ALU.mult, [H, st, vec], [nb[k]])
                for c in range(8):
                    TR(TP.t[:, c, :], nb[k].t[:, c * 128:(c + 1) * 128], identb.t[:, :], [nb[k], identb], [TP])
                CP("act", nT[k].t[:, :, :], TP.t[:, :, :], [TP], [nT[k]])
                if kside:
                    groups = [(PA, 288, 256), (PB, 512, 1056), (PC, 512, 1568)]
                else:
                    groups = [(PA, 256, 0), (PB, 512, 544)]
                for (pt, n, c0) in groups:
                    for c in range(8):
                        MM(pt.t[:, 0:n], nT[k].t[:, c, :], w_in_b.t[:, c, c0:c0 + n], c == 0, c == 7,
                           [nT[k], w_in_b], [pt])
                npa = 288 if kside else 256
                CP("act", pas[k].t[:, 0:npa], PA.t[:, 0:npa], [PA], [pas[k]])
                CP("act", dks[k].t[:, :], PB.t[:, :], [PB], [dks[k]])
                if kside:
                    CP("dve", vd[k].t[:, :, 0:128], PC.t[:, :].rearrange("p (h e) -> p h e", h=4), [PC], [vd[k]])
                    DMA("sp", Vd[blk], vd[k].t[:, :, :], [vd[k]], [dVd[blk]])

            def p2(kside, blk, it):
                k = it % NB_
                junk, sq, tmp8 = junkA[k], sqA[k], tmp8A[k]
                C = cs[k]
                st = st4[k]
                gname = "kv_norm" if kside else "q_norm"
                A(junk.t[:, 0:256], pas[k].t[:, 0:256], AF.Square, [pas[k]], [junk, st], accum_out=st.t[:, 3:4])
                rstd_of(st.t[:, 5:6], st.t[:, 3:4], 256, [st], st, st.t[:, 4:5])
                STT(cn[k].t[:, :], pas[k].t[:, 0:256], st.t[:, 5:6], V(gname), ALU.mult, ALU.mult, [pas[k], st, vec], [cn[k]])
                for c in range(2):
                    TR(TK.t[:, 12 + c, :], cn[k].t[:, c * 128:(c + 1) * 128], identb.t[:, :], [cn[k], identb], [TK])
                CP("act", cT[k].t[:, :, :], TK.t[:, 12:14, :], [TK], [cT[k]])
                wup = w_ukv_b if kside else w_uq_b
                nup = 1024 if kside else 768
                for h0 in range(0, nup, 512):
                    n = min(512, nup - h0)
                    for c in range(2):
                        MM(PD.t[:, h0:h0 + n], cT[k].t[:, c, :], wup.t[:, c, h0:h0 + n], c == 0, c == 1,
                           [cT[k], wup], [PD])
                KV = kvs[k]
                CP("act", KV.t[:, 0:nup], PD.t[:, 0:nup], [PD], [KV])
                s = s8[k]
                if kside:
                    kv3 = KV.t[:, :].rearrange("p (h e) -> p h e", h=8)
                    TT(sq.t[:, 0:512].rearrange("p (h e) -> p h e", h=8), kv3[:, :, 0:64], kv3[:, :, 0:64], ALU.mult,
                       [KV], [sq])
                    RED(s.t[:, 0:8], sq.t[:, 0:512].rearrange("p (h e) -> p h e", h=8), [sq], [s])
                    CP("act", krg[k].t[:, 0, :], pas[k].t[:, 256:288], [pas[k]], [krg[k]])
                    A(junk.t[:, 0:32], krg[k].t[:, 0, :], AF.Square, [krg[k]], [junk, st], accum_out=st.t[:, 6:7])
                    TS(s.t[:, 0:8], s.t[:, 0:8], st.t[:, 6:7], None, ALU.add, None, [s, st], [s])
                    rstd_of(s.t[:, 16:24], s.t[:, 0:8], 96, [s], s, s.t[:, 8:16])
                    K3 = kk[k].t
                    TT(tmp8.t[:, :, :], kv3[:, :, 0:64], bc(s.t[:, 16:24].unsqueeze(2), [128, 8, 64]), ALU.mult,
                       [KV, s], [tmp8])
                    TT(K3[:, :, 0:64], tmp8.t[:, :, :], bc(V("gk")[:, 0:64].unsqueeze(1), [128, 8, 64]), ALU.mult,
                       [tmp8, vec], [kk[k]])
                    t0 = krg[k].t[:, 1, :]
                    TT(t0, krg[k].t[:, 0, :], V("gk")[:, 64:96], ALU.mult, [krg[k], vec], [krg[k]])
                    co, si = C.t[:, 0:16], C.t[:, 16:32]
                    r1, r2 = rr[k].t[:, 0, 0:16], rr[k].t[:, 0, 16:32]
                    a1, a2 = rr[k].t[:, 1, 0:16], rr[k].t[:, 1, 16:32]
                    TT(a1, t0[:, 0:16], co, ALU.mult, [krg[k], C], [rr[k]])
                    TT(a2, t0[:, 16:32], si, ALU.mult, [krg[k], C], [rr[k]])
                    TT(r1, a1, a2, ALU.subtract, [rr[k]], [rr[k]])
                    TT(a1, t0[:, 0:16], si, ALU.mult, [krg[k], C, rr[k]], [rr[k]])
                    TT(a2, t0[:, 16:32], co, ALU.mult, [krg[k], C, rr[k]], [rr[k]])
                    TT(r2, a1, a2, ALU.add, [rr[k]], [rr[k]])
                    TT(K3[:, :, 64:96], bc(rr[k].t[:, 0:1, :], [128, 8, 32]), bc(s.t[:, 16:24].unsqueeze(2), [128, 8, 32]),
                       ALU.mult, [rr[k], s], [kk[k]])
                    for h in range(8):
                        TR(TK.t[0:96, h, :], K3[:, h, :], identb.t[:, :], [kk[k], identb], [TK])
                    CP("act", ktm[k].t[0:96, :, :], TK.t[0:96, 0:8, :], [TK], [ktm[k]])
                    DMA("sp", KVD[blk, :, 0:1024], ktm[k].t[:, :, :].rearrange("p a b -> p (a b)"), [ktm[k]], [dKTm[blk]])
                    CP("pool", vv[k].t[:, :, 0:64], kv3[:, :, 64:128], [KV], [vv[k]])
                    DMA("sp", Vm[blk], vv[k].t[:, :, :], [vv[k]], [dVm[blk]])
                    d3 = dks[k].t[:, :].rearrange("p (g e) -> p g e", g=8)
                    TT(sq.t[:, 512:1024].rearrange("p (g e) -> p g e", g=8), d3, d3, ALU.mult, [dks[k]], [sq])
                    RED(s.t[:, 24:32], sq.t[:, 512:1024].rearrange("p (g e) -> p g e", g=8), [sq], [s])
                    rstd_of(s.t[:, 24:32], s.t[:, 24:32], 64, [s], s, s.t[:, 8:16])
                    TT(tmp8.t[:, :, :], d3, bc(s.t[:, 24:32].unsqueeze(2), [128, 8, 64]), ALU.mult, [dks[k], s], [tmp8])
                    TT(kd[k].t[:, :].rearrange("p (g e) -> p g e", g=8), tmp8.t[:, :, :],
                       bc(V("gdk").unsqueeze(1), [128, 8, 64]), ALU.mult, [tmp8, vec], [kd[k]])
                    for h in range(4):
                        TR(TK.t[:, 8 + h, :], kd[k].t[:, h * 128:(h + 1) * 128], identb.t[:, :], [kd[k], identb], [TK])
                    CP("act", ktd[k].t[:, :, :], TK.t[:, 8:12, :], [TK], [ktd[k]])
                    DMA("sp", KTd[blk], ktd[k].t[:, :, :], [ktd[k]], [dKTd[blk]])
                else:
                    q3 = KV.t[:, 0:768].rearrange("p (h e) -> p h e", h=8)
                    TT(sq.t[:, 0:768].rearrange("p (h e) -> p h e", h=8), q3, q3, ALU.mult, [KV], [sq])
                    RED(s.t[:, 0:8], sq.t[:, 0:768].rearrange("p (h e) -> p h e", h=8), [sq], [s])
                    rstd_of(s.t[:, 16:24], s.t[:, 0:8], 96, [s], s, s.t[:, 8:16])
                    Q3 = kk[k].t
                    TT(tmp8.t[:, :, :], q3[:, :, 0:64], bc(s.t[:, 16:24].unsqueeze(2), [128, 8, 64]), ALU.mult,
                       [KV, s], [tmp8])
                    TT(Q3[:, :, 0:64], tmp8.t[:, :, :], bc(gqs.t[:, 0:64].unsqueeze(1), [128, 8, 64]), ALU.mult,
                       [tmp8, gqs], [kk[k]])
                    tq = krg[k].t
                    TT(tq[:, :, :], q3[:, :, 64:96], bc(s.t[:, 16:24].unsqueeze(2), [128, 8, 32]), ALU.mult,
                       [KV, s], [krg[k]])
                    TT(tq[:, :, :], tq[:, :, :], bc(gqs.t[:, 64:96].unsqueeze(1), [128, 8, 32]), ALU.mult,
                       [krg[k], gqs], [krg[k]])
                    co = bc(C.t[:, 0:16].unsqueeze(1), [128, 8, 16])
                    si = bc(C.t[:, 16:32].unsqueeze(1), [128, 8, 16])
                    a1, a2 = rr[k].t[:, :, 0:16], rr[k].t[:, :, 16:32]
                    TT(a1, tq[:, :, 0:16], co, ALU.mult, [krg[k], C], [rr[k]])
                    TT(a2, tq[:, :, 16:32], si, ALU.mult, [krg[k], C], [rr[k]])
                    TT(Q3[:, :, 64:80], a1, a2, ALU.subtract, [rr[k]], [kk[k]])
                    TT(a1, tq[:, :, 0:16], si, ALU.mult, [krg[k], C, kk[k]], [rr[k]])
                    TT(a2, tq[:, :, 16:32], co, ALU.mult, [krg[k], C, kk[k]], [rr[k]])
                    TT(Q3[:, :, 80:96], a1, a2, ALU.add, [rr[k]], [kk[k]])
                    for h in range(8):
                        TR(TK.t[0:96, h, :], Q3[:, h, :], identb.t[:, :], [kk[k], identb], [TK])
                    CP("act", ktm[k].t[0:96, :, :], TK.t[0:96, 0:8, :], [TK], [ktm[k]])
                    DMA("sp", QTm[blk], ktm[k].t[0:96, :, :], [ktm[k]], [dQTm[blk]])
                    d3 = dks[k].t[:, :].rearrange("p (g e) -> p g e", g=8)
                    TT(sq.t[:, 512:1024].rearrange("p (g e) -> p g e", g=8), d3, d3, ALU.mult, [dks[k]], [sq])
                    RED(s.t[:, 24:32], sq.t[:, 512:1024].rearrange("p (g e) -> p g e", g=8), [sq], [s])
                    rstd_of(s.t[:, 24:32], s.t[:, 24:32], 64, [s], s, s.t[:, 8:16])
                    TT(tmp8.t[:, :, :], d3, bc(s.t[:, 24:32].unsqueeze(2), [128, 8, 64]), ALU.mult, [dks[k], s], [tmp8])
                    TT(kd[k].t[:, :].rearrange("p (g e) -> p g e", g=8), tmp8.t[:, :, :],
                       bc(gdqs.t[:, :].unsqueeze(1), [128, 8, 64]), ALU.mult, [tmp8, gdqs], [kd[k]])
                    for h in range(4):
                        TR(TK.t[:, 8 + h, :], kd[k].t[:, h * 128:(h + 1) * 128], identb.t[:, :], [kd[k], identb], [TK])
                    CP("act", ktd[k].t[:, :, :], TK.t[:, 8:12, :], [TK], [ktd[k]])
                    DMA("sp", QTd[blk], ktd[k].t[:, :, :], [ktd[k]], [dQTd[blk]])

            seq = [(True, blk) for blk in range(NKB)] + [(False, blk) for blk in range(NOWN)]
            p1(seq[0][0], seq[0][1], 0)
            for n in range(len(seq)):
                if n + 1 < len(seq):
                    p1(seq[n + 1][0], seq[n + 1][1], n + 1)
                p2(seq[n][0], seq[n][1], n)
            S.barrier()
            S.flush()

        for es in phase("B"):
            S.defer = True
            stage = [sb(es, [128, 1024]) for _ in range(2)]
            w_out_b = load_w_bf16(es, w_out, 8, 1024, stage)
            mk = sb(es, [128, 2, 128])
            bd = sb(es, [128, 12, 128])
            b31c = V("b31")
            for t in range(2):
                DMA("sp", mk.t[:, t, :], maskm[t], [], [mk])
            for t in range(3):
                for h in range(4):
                    DMA("sp", bd.t[:, t * 4 + h, :], biasd[t, h], [], [bd])
            for t in range(3):
                for h in range(4):
                    TS(bd.t[:, t * 4 + h, :], bd.t[:, t * 4 + h, :], b31c[:, h:h + 1], None, ALU.subtract, None,
                       [bd, vec], [bd])
            mkb = sb(es, [128, 2, 128], BF16)
            bdh = sb(es, [128, 12, 128], BF16)
            bdl = sb(es, [128, 12, 128], BF16)
            bdr = sb(es, [128, 12, 128])
            CP("dve", mkb.t[:, :, :], mk.t[:, :, :], [mk], [mkb])
            CP("dve", bdh.t[:, :, :], bd.t[:, :, :], [bd], [bdh])
            TT(bdr.t[:, :, :], bd.t[:, :, :], bdh.t[:, :, :], ALU.subtract, [bd, bdh], [bdr])
            CP("dve", bdl.t[:, :, :], bdr.t[:, :, :], [bdr], [bdl])
            NKV = 6
            kvt = [sb(es, [128, KVW], BF16) for _ in range(NKV)]
            qtm = [sb(es, [128, 8, 128], BF16) for _ in range(2)]
            qtd = [sb(es, [128, 4, 128], BF16) for _ in range(2)]
            ptm = [sb(es, [128, 4, 128], BF16) for _ in range(6)]
            ptd = [sb(es, [128, 4, 128], BF16) for _ in range(6)]
            hb = [sb(es, [128, D]) for _ in range(2)]
            mixb = sb(es, [128, D], BF16)
            mixT = sb(es, [128, 8, 128], BF16)
            h2 = [sb(es, [128, D]) for _ in range(2)]
            rec = sb(es, [128, 16])
            od = sb(es, [128, 8, 128])
            odd = sb(es, [128, 4, 128])
            sq = sb(es, [128, 4, 128])
            s4 = sb(es, [128, 12])

            AM = [ps(es, [128, 4, 65]) for _ in range(2)]
            AD = [ps(es, [128, 3, 129]), ps(es, [128, 3, 129]), ps(es, [128, 2, 129])]
            SS = [ps(es, [128, 4, 128]) for _ in range(3)]
            sidx = [0]

            def next_s():
                t = SS[sidx[0] % 3]
                sidx[0] += 1
                return t

            if "C" in phases:
                cst = [sb(es, [128, 4096]) for _ in range(2)]
                cbf = [sb(es, [128, 4096], BF16) for _ in range(2)]
                for gq in range(32):
                    for which in range(2):
                        j = (gq * 2 + which) % 2
                        if which == 0:
                            src_ap = uT_d[:, :, gq * 512:(gq + 1) * 512].rearrange("k d e -> d k e")
                            dst_ap, dbuf = UB[gq].rearrange("d k e -> d (k e)"), dUB[gq]
                            stv = cst[j].t[:, :].rearrange("p (k e) -> p k e", k=8)
                        else:
                            src_ap = v_d[gq * 512:(gq + 1) * 512, :].rearrange("(c e) d -> e c d", c=4)
                            dst_ap, dbuf = VB[gq].rearrange("e c d -> e (c d)"), dVB[gq]
                            stv = cst[j].t[:, :].rearrange("p (c d) -> p c d", c=4)
                        DMA("pool", stv, src_ap, [], [cst[j]])
                        CP("pool", cbf[j].t[:, :], cst[j].t[:, :], [cst[j]], [cbf[j]])
                        DMA("pool", dst_ap, cbf[j].t[:, :], [cbf[j]], [dbuf])
            pcount = [0]
            kvit = 0
            for i in range(NOWN):
                Qm, Qd = qtm[i % 2], qtd[i % 2]
                DMA("sp", Qm.t[0:96, :, :], QTm[i], [dQTm[i]], [Qm])
                DMA("sp", Qd.t[:, :, :], QTd[i], [dQTd[i]], [Qd])
                H = hb[i % 2]
                DMA("sp", H.t[:, :], hown[i], [], [H])
                last = 2 * i + 1
                pend = []
                for kb in range(0, last + 1):
                    kq = kvit % NKV
                    kvit += 1
                    kvb = kvt[kq]
                    DMA("sp", kvb.t[:, :], KVD[kb], [dKTm[kb], dVm[kb], dKTd[kb], dVd[kb]], [kvb])

                    def _v(c0, a, b, kvb=kvb):
                        t_ = Tile(kvb.t[:, c0:c0 + a * b].rearrange("p (a b) -> p a b", a=a))
                        t_.b = kvb.b
                        return t_

                    Km, Vmt, Kd, Vdt = _v(0, 8, 128), _v(1024, 8, 65), _v(1544, 4, 128), _v(2056, 4, 129)
                    t = kb - 2 * i
                    first, fin = (kb == 0), (kb == last)
                    def mk_mla(half, t=t, first=first, fin=fin, Km=Km, Vmt=Vmt):
                        box = {}

                        def fS():
                            St = next_s()
                            for hh in range(4):
                                h = half * 4 + hh
                                sp_ = t in (0, 1)
                                MM(St.t[:, hh, :], Km.t[0:96, h, :], Qm.t[0:96, h, :], True, not sp_, [Km, Qm], [St])
                                if sp_:
                                    MM(St.t[:, hh, :], identb.t[:, :], mkb.t[:, t, :], False, True, [identb, mkb], [St])
                            P = ptm[pcount[0] % 6]
                            pcount[0] += 1
                            A(P.t[:, :, :], St.t[:, :, :], AF.Exp, [St], [P])
                            box["P"] = P

                        def fPV():
                            P = box["P"]
                            for hh in range(4):
                                h = half * 4 + hh
                                MM(AM[half].t[:, hh, :], P.t[:, hh, :], Vmt.t[:, h, :], first and hh == 0, fin and hh == 3,
                                   [P, Vmt], [AM[half]])
                        return fS, fPV

                    def mk_diff(t=t, first=first, fin=fin, Kd=Kd, Vdt=Vdt):
                        box = {}

                        def fS():
                            sp_ = t in (-1, 0, 1)
                            Sm = [next_s(), next_s()]
                            for h in range(4):
                                for m in range(2):
                                    MM(Sm[m].t[:, h, :], Kd.t[m * 64:(m + 1) * 64, h, :], Qd.t[m * 64:(m + 1) * 64, h, :],
                                       True, not sp_, [Kd, Qd], [Sm[m]])
                                    if sp_:
                                        MM(Sm[m].t[:, h, :], identb.t[:, :], bdh.t[:, (t + 1) * 4 + h, :], False, False,
                                           [identb, bdh], [Sm[m]])
                                        MM(Sm[m].t[:, h, :], identb.t[:, :], bdl.t[:, (t + 1) * 4 + h, :], False, True,
                                           [identb, bdl], [Sm[m]])
                            Pm = []
                            for m in range(2):
                                P = ptd[pcount[0] % 6]
                                pcount[0] += 1
                                A(P.t[:, :, :], Sm[m].t[:, :, :], AF.Exp, [Sm[m]], [P])
                                Pm.append(P)
                            box["Pm"] = Pm

                        def fPV():
                            Pm = box["Pm"]
                            for h in range(4):
                                for m in range(2):
                                    g = h * 2 + m
                                    MM(AD[g // 3].t[:, g % 3, :], Pm[m].t[:, h, :], Vdt.t[:, h, :], first and g in (0, 3, 6),
                                       fin and g in (2, 5, 7), [Pm[m], Vdt], [AD[g // 3]])
                        return fS, fPV

                    for stg in (mk_mla(0), mk_mla(1), mk_diff()):
                        stg[0]()
                        if pend:
                            pend.pop(0)()
                        pend.append(stg[1])
                while pend:
                    pend.pop(0)()
                for half in range(2):
                    dve(lambda e, half=half: e.reciprocal(out=rec.t[:, half * 4:half * 4 + 4],
                                                          in_=AM[half].t[:, :, 64]), [AM[half]], [rec])
                    TT(mixb.t[:, half * 256:(half + 1) * 256].rearrange("p (h e) -> p h e", h=4), AM[half].t[:, :, 0:64],
                       bc(rec.t[:, half * 4:half * 4 + 4].unsqueeze(2), [128, 4, 64]), ALU.mult, [AM[half], rec], [mixb])
                for a, n0, n in ((0, 0, 3), (1, 3, 3), (2, 6, 2)):
                    dve(lambda e, a=a, n0=n0, n=n: e.reciprocal(out=rec.t[:, 8 + n0:8 + n0 + n], in_=AD[a].t[:, :, 128]),
                        [AD[a]], [rec])
                    TT(od.t[:, n0:n0 + n, :], AD[a].t[:, :, 0:128], bc(rec.t[:, 8 + n0:8 + n0 + n].unsqueeze(2), [128, n, 128]),
                       ALU.mult, [AD[a], rec], [od])
                o4 = od.t[:, :, :].rearrange("p (h m) e -> p h m e", m=2)
                STT(odd.t[:, :, :], o4[:, :, 1, :], neglam.t[:, 0:1], o4[:, :, 0, :], ALU.mult, ALU.add, [od, neglam], [odd])
                TT(sq.t[:, :, :], odd.t[:, :, :], odd.t[:, :, :], ALU.mult, [odd], [sq])
                RED(s4.t[:, 0:4], sq.t[:, :, :], [sq], [s4])
                A(s4.t[:, 4:8], s4.t[:, 0:4], AF.Ln, [s4, epsc], [s4], scale=1.0 / 128, bias=epsc.t[:, 0:1])
                A(s4.t[:, 8:12], s4.t[:, 4:8], AF.Exp, [s4], [s4], scale=-0.5)
                TT(sq.t[:, :, :], odd.t[:, :, :], bc(s4.t[:, 8:12].unsqueeze(2), [128, 4, 128]), ALU.mult, [odd, s4], [sq])
                TT(mixb.t[:, 512:1024].rearrange("p (h e) -> p h e", h=4), sq.t[:, :, :],
                   bc(subl8.t[:, :].unsqueeze(1), [128, 4, 128]), ALU.mult, [sq, subl8], [mixb])
                Ta = next_s()
                Tv = Ta.t[:, :, :].rearrange("p a b -> p (a b)").bitcast(BF16).rearrange("p (c q) -> p c q", c=8)
                for c in range(8):
                    TR(Tv[:, c, :], mixb.t[:, c * 128:(c + 1) * 128], identb.t[:, :], [mixb, identb], [Ta])
                CP("act", mixT.t[:, :, :], Tv, [Ta], [mixT])
                Hh = h2[i % 2]
                for half in range(2):
                    Po = next_s()
                    Pf = Po.t[:, :, :].rearrange("p a b -> p (a b)")
                    for c in range(8):
                        MM(Pf, mixT.t[:, c, :], w_out_b.t[:, c, half * 512:(half + 1) * 512], c == 0, c == 7,
                           [mixT, w_out_b], [Po])
                    TT(Hh.t[:, half * 512:(half + 1) * 512], Pf, H.t[:, half * 512:(half + 1) * 512], ALU.add,
                       [Po, H], [Hh])
                DMA("sp", H2[i], Hh.t[:, :], [Hh], [dH2[i]])
            S.barrier()
            S.flush()

        for es in phase("C"):
          T = NOWN * 128
          E1T = sb(es, [128, T], BF16)
          E2T = sb(es, [128, T], BF16)
          GGT = sb(es, [128, T], BF16)
          iotaf = sb(es, [128, 128])
          S.op("pool", lambda e: e.iota(iotaf.t[:, :], pattern=[[1, 128]], base=0, channel_multiplier=0,
                                        allow_small_or_imprecise_dtypes=True), [], [iotaf])
          with ExitStack() as es1:
            es_outer = es
            es = es1
            S.defer = True
            stage = [sb(es, [128, 2048]) for _ in range(2)]
            w_q_b = load_w_bf16(es, w_query, 8, 2048, stage)
            skb = sb(es, [128, 16, 128], BF16)
            for c in range(16):
                st = stage[c % 2]
                DMA("sp", st.t[:, 0:128], skT[c], [], [st])
                CP("dve", skb.t[:, c, :], st.t[:, 0:128], [st], [skb])
            thr = sb(es, [128, 15])
            io16 = sb(es, [128, 16])
            for m in range(15):
                S.op("pool", lambda e, m=m: e.memset(thr.t[:, m:m + 1], 16.0 * (m + 1) - 0.5), [], [thr])
            for m in range(16):
                S.op("pool", lambda e, m=m: e.memset(io16.t[:, m:m + 1], float(m)), [], [io16])
            hh2 = [sb(es, [128, D]) for _ in range(2)]
            xn = [sb(es, [128, D]) for _ in range(2)]
            xb2 = [sb(es, [128, D], BF16) for _ in range(2)]
            xT2 = [sb(es, [128, 8, 128], BF16) for _ in range(2)]
            junk2_ = [sb(es, [128, D]) for _ in range(2)]
            st42 = [sb(es, [128, 4]) for _ in range(2)]
            qpT2 = [sb(es, [128, 16, 128], BF16) for _ in range(2)]
            sc2 = [sb(es, [128, 16, 128]) for _ in range(2)]
            t16 = sb(es, [128, 8, 2, 16])
            ix = sb(es, [128, 8, 2, 16], U32)
            ixf = sb(es, [128, 8, 2, 16])
            scr = sb(es, [128, 128])
            cand = sb(es, [128, 8, 256])
            scr2 = sb(es, [128, 256])
            best = sb(es, [128, 8, 16])
            fx = sb(es, [128, 8, 16], U32)
            fxf = sb(es, [128, 128])
            cmp = sb(es, [128, 128, 16])
            fi = sb(es, [128, 128])
            fj = sb(es, [128, 128])
            e1 = sb(es, [128, 128])
            e2 = sb(es, [128, 128])
            ge = sb(es, [128, 8, 16])
            gs = sb(es, [128, 16])
            gg = sb(es, [128, 128])
            TPx = ps(es, [128, 8, 128], BF16)
            QS = ps(es, [128, 16, 128])
            t16s = [[Tile(t16.t[:, h, p, :]) for p in range(2)] for h in range(8)]
            ixs = [[Tile(ix.t[:, h, p, :]) for p in range(2)] for h in range(8)]
            scrs = [[sb(es, [128, 128]) for p in range(2)] for h in range(8)]
            bests = [Tile(best.t[:, h, :]) for h in range(8)]
            fxs = [Tile(fx.t[:, h, :]) for h in range(8)]
            scr2s = [sb(es, [128, 256]) for h in range(8)]

            def stage1(i):
                k = i % 2
                xb, xT, junk, st4, qpT, sc = xb2[k], xT2[k], junk2_[k], st42[k], qpT2[k], sc2[k]
                Hh, X = hh2[k], xn[k]
                DMA("sp", Hh.t[:, :], H2[i], [dH2[i]], [Hh])
                A(junk.t[:, :], Hh.t[:, :], AF.Square, [Hh], [junk, st4], accum_out=st4.t[:, 0:1])
                A(st4.t[:, 1:2], st4.t[:, 0:1], AF.Ln, [st4, epsc], [st4], scale=1.0 / D, bias=epsc.t[:, 0:1])
                A(st4.t[:, 2:3], st4.t[:, 1:2], AF.Exp, [st4], [st4], scale=-0.5)
                STT(X.t[:, :], Hh.t[:, :], st4.t[:, 2:3], V("ffn_norm"), ALU.mult, ALU.mult, [Hh, st4, vec], [X])
                CP("pool", xb.t[:, :], X.t[:, :], [X], [xb])
                for c in range(8):
                    TR(TPx.t[:, c, :], xb.t[:, c * 128:(c + 1) * 128], identb.t[:, :], [xb, identb], [TPx])
                CP("act", xT.t[:, :, :], TPx.t[:, :, :], [TPx], [xT])
                for c in range(16):
                    for kc in range(8):
                        MM(QS.t[:, c, :], w_q_b.t[:, kc, c * 128:(c + 1) * 128], xT.t[:, kc, :], kc == 0, kc == 7,
                           [w_q_b, xT], [QS])
                CP("act", qpT.t[:, 0:8, :], QS.t[:, 0:8, :], [QS], [qpT])
                CP("dve", qpT.t[:, 8:16, :], QS.t[:, 8:16, :], [QS], [qpT])
                for c in range(16):
                    MM(QS.t[:, c, :], qpT.t[:, c, :], skb.t[:, c, :], True, True, [qpT, skb], [QS])
                CP("act", sc.t[:, 0:8, :], QS.t[:, 0:8, :], [QS], [sc])
                CP("dve", sc.t[:, 8:16, :], QS.t[:, 8:16, :], [QS], [sc])
                HP = [(h, p) for h in range(8) for p in range(2)]
                for h, p in HP:
                    dve(lambda e, h=h, p=p: e.max(out=t16.t[:, h, p, 0:8], in_=sc.t[:, 2 * h + p, :]), [sc], [t16s[h][p]])
                for h, p in HP:
                    dve(lambda e, h=h, p=p: e.match_replace(out=scrs[h][p].t[:, :], in_to_replace=t16.t[:, h, p, 0:8],
                                                            in_values=sc.t[:, 2 * h + p, :], imm_value=-1e30),
                        [sc, t16s[h][p]], [scrs[h][p]])
                for h, p in HP:
                    dve(lambda e, h=h, p=p: e.max(out=t16.t[:, h, p, 8:16], in_=scrs[h][p].t[:, :]), [scrs[h][p]], [t16s[h][p]])
                for h, p in HP:
                    dve(lambda e, h=h, p=p: e.max_index(out=ix.t[:, h, p, 0:8], in_max=t16.t[:, h, p, 0:8],
                                                        in_values=sc.t[:, 2 * h + p, :]), [sc, t16s[h][p]], [ixs[h][p]])
                for h, p in HP:
                    dve(lambda e, h=h, p=p: e.max_index(out=ix.t[:, h, p, 8:16], in_max=t16.t[:, h, p, 8:16],
                                                        in_values=sc.t[:, 2 * h + p, :]), [sc, t16s[h][p]], [ixs[h][p]])
                all_t16 = [t16s[h][p] for h, p in HP]
                all_ix = [ixs[h][p] for h, p in HP]
                CP("dve", ixf.t[:, :, :, :], ix.t[:, :, :, :], all_ix, [ixf])
                TT(cand.t[:, :, :].rearrange("p h (a b) -> p h a b", a=16), bc(t16.t[:, :, 0, :].unsqueeze(3), [128, 8, 16, 16]),
                   bc(t16.t[:, :, 1, :].unsqueeze(2), [128, 8, 16, 16]), ALU.add, all_t16, [cand])
                for h in range(8):
                    dve(lambda e, h=h: e.max(out=best.t[:, h, 0:8], in_=cand.t[:, h, :]), [cand], [bests[h]])
                for h in range(8):
                    dve(lambda e, h=h: e.match_replace(out=scr2s[h].t[:, :], in_to_replace=best.t[:, h, 0:8],
                                                       in_values=cand.t[:, h, :], imm_value=-1e30), [cand, bests[h]], [scr2s[h]])
                for h in range(8):
                    dve(lambda e, h=h: e.max(out=best.t[:, h, 8:16], in_=scr2s[h].t[:, :]), [scr2s[h]], [bests[h]])
                for h in range(8):
                    dve(lambda e, h=h: e.max_index(out=fx.t[:, h, 0:8], in_max=best.t[:, h, 0:8],
                                                   in_values=cand.t[:, h, :]), [cand, bests[h]], [fxs[h]])
                for h in range(8):
                    dve(lambda e, h=h: e.max_index(out=fx.t[:, h, 8:16], in_max=best.t[:, h, 8:16],
                                                   in_values=cand.t[:, h, :]), [cand, bests[h]], [fxs[h]])
                best_all = bests
                CP("dve", fxf.t[:, :], fx.t[:, :, :].rearrange("p h k -> p (h k)"), fxs, [fxf])
                TT(cmp.t[:, :, 0:15], bc(fxf.t[:, :].unsqueeze(2), [128, 128, 15]), bc(thr.t[:, :].unsqueeze(1), [128, 128, 15]),
                   ALU.is_ge, [fxf, thr], [cmp])
                RED(fi.t[:, :], cmp.t[:, :, 0:15], [cmp], [fi])
                STT(fj.t[:, :], fi.t[:, :], -16.0, fxf.t[:, :], ALU.mult, ALU.add, [fi, fxf], [fj])
                c4 = cmp.t[:, :, :].rearrange("p (h k) i -> p h k i", h=8)
                for (fsel, pidx, eo) in ((fi, 0, e1), (fj, 1, e2)):
                    TT(cmp.t[:, :, :], bc(io16.t[:, :].unsqueeze(1), [128, 128, 16]), bc(fsel.t[:, :].unsqueeze(2), [128, 128, 16]),
                       ALU.is_equal, [io16, fsel], [cmp])
                    TT(c4, c4, bc(ixf.t[:, :, pidx, :].unsqueeze(2), [128, 8, 16, 16]), ALU.mult, [cmp, ixf], [cmp])
                    RED(eo.t[:, :], cmp.t[:, :, :], [cmp], [eo])
                TT(ge.t[:, :, :], best.t[:, :, :], bc(best.t[:, :, 0:1], [128, 8, 16]), ALU.subtract, bests, [ge])
                A(ge.t[:, :, :], ge.t[:, :, :], AF.Exp, [ge], [ge])
                RED(gs.t[:, 0:8], ge.t[:, :, :], [ge], [gs])
                dve(lambda e: e.reciprocal(out=gs.t[:, 8:16], in_=gs.t[:, 0:8]), [gs], [gs])
                TT(gg.t[:, :].rearrange("p (h k) -> p h k", h=8), ge.t[:, :, :], bc(gs.t[:, 8:16].unsqueeze(2), [128, 8, 16]),
                   ALU.mult, [ge, gs], [gg])


            for i in range(NOWN):
                stage1(i)
                DMA("sp", XT[i], xT2[i % 2].t[:, :, :], [xT2[i % 2]], [dXT[i]])
                for j, (srcT, dstT) in enumerate(((e1, E1T), (e2, E2T), (gg, GGT))):
                    MM(QS.t[:, j, :], srcT.t[:, :], ident.t[:, :], True, True, [srcT, ident], [QS])
                    CP("act", dstT.t[:, i * 128:(i + 1) * 128], QS.t[:, j, :], [QS], [dstT])
            S.barrier()
            S.flush()
            es = es_outer
          S.defer = True
          GB = 3
          NGRP = 0 if skip_c2 else -(-NOWN // GB)
          Wg = sb(es, [128, 128, GB * 128], BF16)
          xg = sb(es, [128, 8, GB * 128], BF16)
          hg = [sb(es, [128, D]) for _ in range(GB)]
          ust = [sb(es, [128, 8, 512], BF16) for _ in range(2)]
          vst = [sb(es, [128, 4, 1024], BF16) for _ in range(2)]
          gl = [sb(es, [128, GB * 128], BF16) for _ in range(3)]
          wa = [sb(es, [128, GB * 128], BF16) for _ in range(3)]
          At = [sb(es, [128, 128], BF16) for _ in range(4)]
          Bt = [sb(es, [128, 128], BF16) for _ in range(4)]
          yo = sb(es, [128, D])
          ACC = [ps(es, [128, D]) for _ in range(GB)]
          BK = [ps(es, [128, 512]) for _ in range(2)]
          ldn = [0]
          for grp in range(NGRP):
              blks = list(range(grp * GB, min(NOWN, (grp + 1) * GB)))
              nb_ = len(blks)
              G = nb_ * 128
              for j, bi in enumerate(blks):
                  DMA("sp", xg.t[:, :, j * 128:(j + 1) * 128], XT[bi], [dXT[bi]], [xg])
                  DMA("sp", hg[j].t[:, :], H2[bi], [dH2[bi]], [hg[j]])
              for t0 in range(0, G, 4):
                  bk = BK[(t0 // 4) % 2]
                  bkv = bk.t[:, :].rearrange("p (a b) -> p a b", a=4)
                  for tt in range(4):
                      tg = grp * GB * 128 + t0 + tt
                      a_, b_ = At[(t0 + tt) % 4], Bt[(t0 + tt) % 4]
                      TS(a_.t[:, :], iotaf.t[:, :], E1T.t[:, tg:tg + 1], GGT.t[:, tg:tg + 1], ALU.is_equal, ALU.mult,
                         [iotaf, E1T, GGT], [a_])
                      TS(b_.t[:, :], iotaf.t[:, :], E2T.t[:, tg:tg + 1], None, ALU.is_equal, None, [iotaf, E2T], [b_])
                      MM(bkv[:, tt, :], b_.t[:, :], a_.t[:, :], True, True, [a_, b_], [bk])
                  CP("act", Wg.t[:, :, t0:t0 + 4].rearrange("p n t -> p t n"), bkv, [bk], [Wg])
              def load_w(gq):
                  DMA("sp", ust[gq % 2].t[:, :, :], UB[gq], [dUB[gq]], [ust[gq % 2]])
                  DMA("pool", vst[gq % 2].t[:, :, :], VB[gq], [dVB[gq]], [vst[gq % 2]])

              def h_mm(c):
                  gq, cc = divmod(c, 4)
                  k_ = gq % 2
                  bk = BK[c % 2]
                  for kc in range(8):
                      MM(bk.t[:, 0:G], ust[k_].t[:, kc, cc * 128:(cc + 1) * 128], xg.t[:, kc, 0:G], kc == 0, kc == 7,
                         [ust[k_], xg], [bk])
                  A(gl[c % 3].t[:, 0:G], bk.t[:, 0:G], AF.Gelu, [bk], [gl[c % 3]])
                  TT(wa[c % 3].t[:, 0:G], gl[c % 3].t[:, 0:G], Wg.t[:, c, 0:G], ALU.mult, [gl[c % 3], Wg], [wa[c % 3]])
                  return k_

              def v_mm(c, k_):
                  cc = c % 4
                  for j in range(nb_):
                      for half in range(2):
                          MM(ACC[j].t[:, half * 512:(half + 1) * 512], wa[c % 3].t[:, j * 128:(j + 1) * 128],
                             vst[k_].t[:, cc, half * 512:(half + 1) * 512], c == 0, c == 127, [wa[c % 3], vst[k_]], [ACC[j]])

              load_w(0)
              load_w(1)
              pendv = []
              for c in range(128):
                  k_ = h_mm(c)
                  pendv.append((c, k_))
                  if len(pendv) > 2:
                      v_mm(*pendv.pop(0))
                  if c % 4 == 1 and c > 4 and c // 4 + 1 < 32:
                      load_w(c // 4 + 1)
              while pendv:
                  v_mm(*pendv.pop(0))
              for j, bi in enumerate(blks):
                  TT(yo.t[:, :], ACC[j].t[:, :], hg[j].t[:, :], ALU.add, [ACC[j], hg[j]], [yo])
                  DMA("sp", y[bi], yo.t[:, :], [yo], [])
          S.barrier()
          S.flush()
    return nc


def _t5_bucket_np(n):
    n = np.maximum(n, 0)
    nf = np.maximum(n, 1).astype(np.float32)
    large = 16 + (np.log(nf / 16) / math.log(128 / 16) * 16).astype(np.int32)
    large = np.minimum(large, 31)
    return np.where(n < 16, n, large)


def _bias_index_tables():
    k = np.arange(128)[:, None]
    q = np.arange(128)[None, :]
    diag = np.where(k <= q, _t5_bucket_np(q - k), 32)
    sub = _t5_bucket_np(128 + q - k)
    far = np.full((128, 128), 31)
    allm = np.full((128, 128), 32)
    return {"diag": diag, "sub": sub, "far": far, "allm": allm}


def prepare_inputs(inputs, NOWN, seq_pad_blocks=None):
    x = np.asarray(inputs["x"], np.float32)
    Bn, Sn, _ = x.shape
    NKB = 2 * NOWN
    L = NKB * 128
    meta = np.asarray(inputs["meta_tokens"], np.float32)
    rel_bias = np.asarray(inputs["rel_bias"], np.float32)
    half = 16
    inv_freq = (10000.0 ** (-np.arange(half, dtype=np.float32) / half)).astype(np.float32)
    pos = np.arange(L, dtype=np.float32)
    ang = pos[:, None] * inv_freq[None, :]
    cs_all = np.concatenate([np.cos(ang), np.sin(ang)], axis=1).astype(np.float32)

    def g(name):
        return np.asarray(inputs[name], np.float32)[0]

    vec_parts = {
        "attn_norm": g("attn_norm"), "ffn_norm": g("ffn_norm"), "q_norm": g("mla_q_norm"), "kv_norm": g("mla_kv_norm"),
        "gq": g("mla_qk_norm_q"), "gk": g("mla_qk_norm_k"), "gdq": g("diff_q_norm"), "gdk": g("diff_k_norm"),
        "lq1": g("diff_lambda_q1"), "lk1": g("diff_lambda_k1"), "lq2": g("diff_lambda_q2"), "lk2": g("diff_lambda_k2"),
        "subln": g("diff_subln"), "b31": rel_bias[31, :],
    }
    vecs = np.concatenate([vec_parts[n] for n, _ in VEC_LAYOUT])[None, :].astype(np.float32)
    tabs = _bias_index_tables()
    ext = np.concatenate([rel_bias, np.full((1, 4), NEG, np.float32)], axis=0)
    zero_neg = np.array([0.0, NEG], np.float32)
    tri = zero_neg[(tabs["diag"] == 32).astype(np.int64)]
    allneg = zero_neg[np.ones((128, 128), np.int64)]
    zeros = zero_neg[np.zeros((128, 128), np.int64)]
    common = {
        "w_in": g("w_in").reshape(8, 128, IN_W), "w_uq": g("mla_w_uq").reshape(2, 128, 768),
        "w_ukv": g("mla_w_ukv").reshape(2, 128, 1024), "w_out": g("w_out").reshape(8, 128, 1024),
        "w_query": g("peer_w_query").reshape(8, 128, 2048),
        "skT": np.ascontiguousarray(g("peer_sub_keys").reshape(16, 128, 128).transpose(0, 2, 1)),
        "peer_uT": np.ascontiguousarray(g("peer_u").T).reshape(8, 128, 16384), "peer_v": g("peer_v"), "vecs": vecs,
    }
    in_maps = []
    for b in range(Bn):
        hfull = np.zeros((L, D), np.float32)
        hfull[:NMETA] = meta
        hfull[NMETA:NMETA + Sn] = x[b]
        hseq = hfull.reshape(NKB, 128, D)
        for par in range(2):
            own = np.arange(NOWN) * 2 + par
            if par == 0:
                types = ["sub", "diag", "allm"]
                mm = np.stack([tri, allneg])
            else:
                types = ["far", "sub", "diag"]
                mm = np.stack([zeros, tri])
            bd = np.stack([np.stack([ext[tabs[t], h] for h in range(4)]) for t in types]).astype(np.float32)
            m = dict(common)
            m.update({
                "hseq": hseq, "hown": np.ascontiguousarray(hseq[own]),
                "csk": cs_all.reshape(NKB, 128, 32), "csq": np.ascontiguousarray(cs_all.reshape(NKB, 128, 32)[own]),
                "maskm": mm.astype(np.float32), "biasd": bd,
            })
            in_maps.append(m)
    return in_maps


def assemble(results, Bn, Sn, NOWN):
    NKB = 2 * NOWN
    out = np.zeros((Bn, NKB * 128, D), np.float32)
    for b in range(Bn):
        for par in range(2):
            yv = np.asarray(results[b * 2 + par]["y"]).reshape(NOWN, 128, D)
            full = out[b].reshape(NKB, 128, D)
            full[par::2] = yv
    return np.ascontiguousarray(out[:, NMETA:NMETA + Sn])


_NC_CACHE = {}


def kernel(**inputs):
    x = np.asarray(inputs["x"])
    Bn, Sn, _ = x.shape
    nblocks = -(-(NMETA + Sn) // 128)
    NOWN = (nblocks + 1) // 2
    if NOWN not in _NC_CACHE:
        _NC_CACHE[NOWN] = build(NOWN)
    nc = _NC_CACHE[NOWN]
    in_maps = prepare_inputs(inputs, NOWN)
    res = run_bass_kernel_spmd(nc, in_maps, core_ids=list(range(len(in_maps))))
    return assemble(res.results, Bn, Sn, NOWN).astype(np.float32)
```
